# Optimizing a Trainium2 kernel written in Bass

```python
import jax, jax.numpy as jnp
from jax import lax
import numpy as np

D_MODEL = 1024
BATCH = 8
SEQ = 4096
DEPTH = 2

CTX_LEN = 256
GRID_W = 64
NORM_EPS = 1e-6

GLA_HEADS = 4
GLA_DK = 64
GLA_DV = 128
GLA_RANK = 16
GLA_TAU = 16.0
GLA_CHUNK = 64
GLA_QK = GLA_HEADS * GLA_DK
GLA_V = GLA_HEADS * GLA_DV

ATT_HEADS = 8
ATT_KV_HEADS = 2
ATT_DH = 64
ATT_BLOCK = 128
ROPE_THETA = 10000.0
ROPE_AXIS_DIM = ATT_DH // 2
ATT_Q = ATT_HEADS * ATT_DH
ATT_KV = ATT_KV_HEADS * ATT_DH

POOL_WINDOWS = (2, 4, 8, 16)
POOL_GROUP = 128
POOL_WIDTH = POOL_GROUP * len(POOL_WINDOWS)

N_BRANCH = 3
BRANCH_WIDTH = 512

N_GROUPS = 4
EXP_PER_GROUP = 8
N_EXPERTS = N_GROUPS * EXP_PER_GROUP
TOP_K = 2
D_EXPERT = 512
MOE_BLOCK = 128

IN_SIZES = (GLA_QK, GLA_QK, GLA_V, GLA_V, GLA_RANK, GLA_RANK, ATT_Q, ATT_KV, ATT_KV, POOL_WIDTH, N_BRANCH * D_MODEL)
IN_WIDTH = int(sum(IN_SIZES))
IN_SPLITS = tuple(int(s) for s in np.cumsum(IN_SIZES)[:-1])

kernel_name = 'hybrid_gla_gqa_pool_hmoe_dit'

F32 = jnp.float32


def rmsnorm(x, g):
    xf = x.astype(F32)
    y = xf * lax.rsqrt(jnp.mean(xf * xf, axis=-1, keepdims=True) + NORM_EPS)
    return (y * g.astype(F32)).astype(x.dtype)


def modulate(h, shift, scale):
    return h * (1 + scale[:, None]) + shift[:, None]


def axial_rope_angles(rows):
    row = jnp.repeat(jnp.arange(rows), GRID_W).astype(F32)
    col = jnp.tile(jnp.arange(GRID_W), rows).astype(F32)
    inv = ROPE_THETA ** (-jnp.arange(0, ROPE_AXIS_DIM, 2, dtype=F32) / ROPE_AXIS_DIM)
    return row[:, None] * inv, col[:, None] * inv


def _rotate(x, ang):
    x1, x2 = jnp.split(x, 2, axis=-1)
    cos = jnp.cos(ang)[None, :, None, :]
    sin = jnp.sin(ang)[None, :, None, :]
    return jnp.concatenate([x1 * cos - x2 * sin, x1 * sin + x2 * cos], axis=-1)


def rope_2d(x, ang_row, ang_col):
    xf = x.astype(F32)
    out = jnp.concatenate([_rotate(xf[..., :ROPE_AXIS_DIM], ang_row),
                           _rotate(xf[..., ROPE_AXIS_DIM:], ang_col)], axis=-1)
    return out.astype(x.dtype)


def gla_direction(q, k, v, log_a, s0):
    B, T, H, DK = k.shape
    DV = v.shape[-1]
    L = GLA_CHUNK
    n = T // L
    kc = k.astype(F32).reshape(B, n, L, H, DK)
    vc = v.astype(F32).reshape(B, n, L, H, DV)
    b = jnp.cumsum(log_a.astype(F32).reshape(B, n, L, H, DK), axis=2)
    b_last = b[:, :, -1:]
    d_state = jnp.einsum('bnshd,bnshv->bnhdv', kc * jnp.exp(b_last - b), vc)
    a_chunk = jnp.exp(b_last[:, :, 0])
    emit = q is not None

    def step(s, inp):
        a, d = inp
        return a[..., None] * s + d, (s if emit else None)

    s_fin, s_start = lax.scan(step, s0, (jnp.moveaxis(a_chunk, 1, 0), jnp.moveaxis(d_state, 1, 0)))
    if not emit:
        return None, s_fin
    s_start = jnp.moveaxis(s_start, 0, 1)
    qc = q.astype(F32).reshape(B, n, L, H, DK)
    b_ref = b[:, :, L // 2:L // 2 + 1]
    scores = jnp.einsum('bnthd,bnshd->bnhts', qc * jnp.exp(b - b_ref), kc * jnp.exp(b_ref - b))
    lower = jnp.tril(jnp.ones((L, L), dtype=bool))
    scores = jnp.where(lower, scores, 0.0)
    o = (jnp.einsum('bnhts,bnshv->bnthv', scores, vc)
         + jnp.einsum('bnthd,bnhdv->bnthv', qc * jnp.exp(b), s_start))
    return o.reshape(B, T, H, DV).astype(v.dtype), s_fin


def gla_bidir(q, k, v, la_f, la_b, s0_f, s0_b, want_out):
    flip = lambda t: jnp.flip(t, axis=1)
    o_f, s_f = gla_direction(q if want_out else None, k, v, la_f, s0_f)
    o_b, s_b = gla_direction(flip(q) if want_out else None, flip(k), flip(v), flip(la_b), s0_b)
    o = (o_f + flip(o_b)) if want_out else None
    return o, s_f, s_b


def gla_inputs(gq, gk, gv, glf, glb, up_f, bias_f, up_b, bias_b):
    B, T, _ = gq.shape
    shp = (B, T, GLA_HEADS, GLA_DK)
    q = gq.reshape(shp) * GLA_DK ** -0.5
    k = gk.reshape(shp)
    v = gv.reshape(B, T, GLA_HEADS, GLA_DV)
    la_f = (jax.nn.log_sigmoid((glf @ up_f + bias_f).astype(F32)) / GLA_TAU).reshape(shp)
    la_b = (jax.nn.log_sigmoid((glb @ up_b + bias_b).astype(F32)) / GLA_TAU).reshape(shp)
    return q, k, v, la_f, la_b


def gla_output(o, r, norm_g):
    B, T, H, DV = o.shape
    return rmsnorm(o, norm_g).reshape(B, T, H * DV) * jax.nn.silu(r)


def gqa_blocks(q, k, v):
    B, T, H, DH = q.shape
    HKV = k.shape[2]
    G = H // HKV
    nb = T // ATT_BLOCK
    qb = jnp.moveaxis(q.reshape(B, nb, ATT_BLOCK, HKV, G, DH), 1, 0)

    def one(qblk):
        s = jnp.einsum('bqkgd,bskd->bkgqs', qblk, k).astype(F32) * DH ** -0.5
        p = jax.nn.softmax(s, axis=-1).astype(v.dtype)
        return jnp.einsum('bkgqs,bskd->bqkgd', p, v)

    o = lax.map(one, qb)
    return jnp.moveaxis(o, 0, 1).reshape(B, T, H * DH)


def pool_mix(u, pool_w, pool_scale):
    B, T, W = u.shape
    uf = u.astype(F32)
    cs = jnp.concatenate([jnp.zeros((B, 1, W), F32), jnp.cumsum(uf, axis=1)], axis=1)
    t = jnp.arange(T)
    diffs = []
    for gi, win in enumerate(POOL_WINDOWS):
        sl = slice(gi * POOL_GROUP, (gi + 1) * POOL_GROUP)
        lo = jnp.clip(t - win // 2, 0, T)
        hi = jnp.clip(t + win // 2, 0, T)
        csg = cs[..., sl]
        mean = (csg[:, hi] - csg[:, lo]) / (hi - lo).astype(F32)[None, :, None]
        diffs.append(mean - uf[..., sl])
    d = jnp.stack(diffs, axis=2)
    y = jnp.einsum('btgc,gce->btge', d, pool_w.astype(F32)).reshape(B, T, W) * pool_scale.astype(F32)
    return y.astype(u.dtype)


def branch_merge(y_gla, y_att, y_pool, gt, w_branch, w_out):
    g = jax.nn.sigmoid(gt.reshape(gt.shape[:-1] + (N_BRANCH, -1)))
    z = (g[..., 0, :] * (y_gla @ w_branch[0])
         + g[..., 1, :] * (y_att @ w_branch[1])
         + g[..., 2, :] * (y_pool @ w_branch[2]))
    return z @ w_out


def mixing(h, hc, ang_row, ang_col, w_in, gla_a_up_f, gla_a_bias_f, gla_a_up_b, gla_a_bias_b,
           gla_norm_g, att_qn_g, att_kn_g, pool_w, pool_scale, w_branch, w_out, want_ctx):
    B, T, _ = h.shape
    C = hc.shape[1]
    (gq, gk, gv, gr, glf, glb, aq, ak, av, pu, gt) = jnp.split(h @ w_in, IN_SPLITS, axis=-1)
    (gq_c, gk_c, gv_c, gr_c, glf_c, glb_c, aq_c, ak_c, av_c, pu_c, gt_c) = jnp.split(hc @ w_in, IN_SPLITS, axis=-1)
    q_c, k_c, v_c, laf_c, lab_c = gla_inputs(gq_c, gk_c, gv_c, glf_c, glb_c, gla_a_up_f, gla_a_bias_f, gla_a_up_b, gla_a_bias_b)
    s0 = jnp.zeros((B, GLA_HEADS, GLA_DK, GLA_DV), F32)
    o_c, sf_c, sb_c = gla_bidir(q_c, k_c, v_c, laf_c, lab_c, s0, s0, want_ctx)
    q, k, v, laf, lab = gla_inputs(gq, gk, gv, glf, glb, gla_a_up_f, gla_a_bias_f, gla_a_up_b, gla_a_bias_b)
    o, _, _ = gla_bidir(q, k, v, laf, lab, sf_c, sb_c, True)
    y_gla = gla_output(o, gr, gla_norm_g)
    ka_c = rmsnorm(ak_c.reshape(B, C, ATT_KV_HEADS, ATT_DH), att_kn_g)
    va_c = av_c.reshape(B, C, ATT_KV_HEADS, ATT_DH)
    qa = rope_2d(rmsnorm(aq.reshape(B, T, ATT_HEADS, ATT_DH), att_qn_g), ang_row, ang_col)
    ka = rope_2d(rmsnorm(ak.reshape(B, T, ATT_KV_HEADS, ATT_DH), att_kn_g), ang_row, ang_col)
    va = av.reshape(B, T, ATT_KV_HEADS, ATT_DH)
    y_att = gqa_blocks(qa, jnp.concatenate([ka, ka_c], axis=1), jnp.concatenate([va, va_c], axis=1))
    y_pool = pool_mix(pu, pool_w, pool_scale)
    y = branch_merge(y_gla, y_att, y_pool, gt, w_branch, w_out)
    if not want_ctx:
        return y, None
    qa_c = rmsnorm(aq_c.reshape(B, C, ATT_HEADS, ATT_DH), att_qn_g)
    yc = branch_merge(gla_output(o_c, gr_c, gla_norm_g), gqa_blocks(qa_c, ka_c, va_c),
                      pool_mix(pu_c, pool_w, pool_scale), gt_c, w_branch, w_out)
    return y, yc


def routed_ffn(h, w_group, b_group, w_expert, b_expert, w_gate, w_up, w_down):
    B, S, D = h.shape
    N = B * S
    xt = h.reshape(N, D)
    p_group = jax.nn.softmax((xt @ w_group).astype(F32) + b_group, axis=-1)
    p_top_group, grp = lax.top_k(p_group, 1)
    grp = grp[:, 0]
    logits_e = ((xt @ w_expert).astype(F32) + b_expert).reshape(N, N_GROUPS, EXP_PER_GROUP)
    p_in = jax.nn.softmax(logits_e[jnp.arange(N), grp], axis=-1)
    p_top, idx = lax.top_k(p_in, TOP_K)
    wts = (p_top_group * p_top / jnp.sum(p_top, axis=-1, keepdims=True)).reshape(-1)
    eid = (grp[:, None] * EXP_PER_GROUP + idx).reshape(-1)
    M = N * TOP_K
    tok = jnp.arange(M) // TOP_K
    order = jnp.argsort(eid)
    e_s, tok_s, w_s = eid[order], tok[order], wts[order]
    counts = jnp.bincount(eid, length=N_EXPERTS)
    padded = (counts + MOE_BLOCK - 1) // MOE_BLOCK * MOE_BLOCK
    seg_start = jnp.cumsum(counts) - counts
    pad_end = jnp.cumsum(padded)
    pad_start = pad_end - padded
    dest = pad_start[e_s] + jnp.arange(M) - seg_start[e_s]
    n_blocks = -(-(M + N_EXPERTS * (MOE_BLOCK - 1)) // MOE_BLOCK)
    P = n_blocks * MOE_BLOCK
    buf_tok = jnp.full((P,), N, jnp.int32).at[dest].set(tok_s)
    buf_w = jnp.zeros((P,), F32).at[dest].set(w_s)
    blk_expert = jnp.minimum(jnp.searchsorted(pad_end, jnp.arange(n_blocks) * MOE_BLOCK, side='right'), N_EXPERTS - 1)
    x_pad = jnp.concatenate([xt, jnp.zeros((1, D), xt.dtype)], axis=0)

    def expert_block(args):
        tb, e = args
        xb = x_pad[tb]
        return (jax.nn.silu(xb @ w_gate[e]) * (xb @ w_up[e])) @ w_down[e]

    yb = lax.map(expert_block, (buf_tok.reshape(n_blocks, MOE_BLOCK), blk_expert))
    yb = yb.reshape(P, D) * buf_w[:, None].astype(yb.dtype)
    out = jnp.zeros((N + 1, D), yb.dtype).at[buf_tok].add(yb)[:N]
    return out.reshape(B, S, D).astype(h.dtype)


def setup_inputs(seed: int = 0) -> dict:
    key = jax.random.key(seed)
    ks = jax.random.split(key, 32)
    L = DEPTH
    D = D_MODEL
    nrm = lambda k, shape, s: jax.random.normal(k, shape, F32) * s
    return {
        'x': nrm(ks[0], (BATCH, SEQ, D), 1.0),
        'c': nrm(ks[1], (BATCH, D), 1.0),
        'ctx': nrm(ks[2], (BATCH, CTX_LEN, D), 1.0),
        'c_ctx': nrm(ks[3], (D,), 1.0),
        'w_mod': nrm(ks[4], (L, D, 6 * D), 0.5 * D ** -0.5),
        'b_mod': nrm(ks[5], (L, 6 * D), 0.02),
        'norm1_g': 1.0 + nrm(ks[6], (L, D), 0.02),
        'norm2_g': 1.0 + nrm(ks[7], (L, D), 0.02),
        'w_in': nrm(ks[8], (L, D, IN_WIDTH), D ** -0.5),
        'gla_a_up_f': nrm(ks[9], (L, GLA_RANK, GLA_QK), GLA_RANK ** -0.5),
        'gla_a_bias_f': nrm(ks[10], (L, GLA_QK), 0.1),
        'gla_a_up_b': nrm(ks[11], (L, GLA_RANK, GLA_QK), GLA_RANK ** -0.5),
        'gla_a_bias_b': nrm(ks[12], (L, GLA_QK), 0.1),
        'gla_norm_g': 1.0 + nrm(ks[13], (L, GLA_DV), 0.02),
        'att_qn_g': 1.0 + nrm(ks[14], (L, ATT_DH), 0.02),
        'att_kn_g': 1.0 + nrm(ks[15], (L, ATT_DH), 0.02),
        'pool_w': nrm(ks[16], (L, len(POOL_WINDOWS), POOL_GROUP, POOL_GROUP), POOL_GROUP ** -0.5),
        'pool_scale': 1.0 + nrm(ks[17], (L, POOL_WIDTH), 0.02),
        'w_branch': nrm(ks[18], (L, N_BRANCH, BRANCH_WIDTH, D), BRANCH_WIDTH ** -0.5),
        'w_out': nrm(ks[19], (L, D, D), D ** -0.5),
        'moe_w_group': nrm(ks[20], (L, D, N_GROUPS), D ** -0.5),
        'moe_b_group': nrm(ks[21], (L, N_GROUPS), 0.01),
        'moe_w_expert': nrm(ks[22], (L, D, N_EXPERTS), D ** -0.5),
        'moe_b_expert': nrm(ks[23], (L, N_EXPERTS), 0.01),
        'moe_w_gate': nrm(ks[24], (L, N_EXPERTS, D, D_EXPERT), D ** -0.5),
        'moe_w_up': nrm(ks[25], (L, N_EXPERTS, D, D_EXPERT), D ** -0.5),
        'moe_w_down': nrm(ks[26], (L, N_EXPERTS, D_EXPERT, D), D_EXPERT ** -0.5),
        'final_g': 1.0 + nrm(ks[27], (D,), 0.02),
    }


def reference(x, c, ctx, c_ctx, w_mod, b_mod, norm1_g, norm2_g, w_in, gla_a_up_f, gla_a_bias_f,
              gla_a_up_b, gla_a_bias_b, gla_norm_g, att_qn_g, att_kn_g, pool_w, pool_scale,
              w_branch, w_out, moe_w_group, moe_b_group, moe_w_expert, moe_b_expert,
              moe_w_gate, moe_w_up, moe_w_down, final_g):
    B, T, D = x.shape
    rows = T // GRID_W
    ang_row, ang_col = axial_rope_angles(rows)
    xc = ctx
    s_lat = jax.nn.silu(c)
    s_ctx = jax.nn.silu(c_ctx)[None]
    for l in range(DEPTH):
        want_ctx = l < DEPTH - 1
        sh1, sc1, g1, sh2, sc2, g2 = jnp.split(s_lat @ w_mod[l] + b_mod[l], 6, axis=-1)
        csh1, csc1, cg1, csh2, csc2, cg2 = jnp.split(s_ctx @ w_mod[l] + b_mod[l], 6, axis=-1)
        h = modulate(rmsnorm(x, norm1_g[l]), sh1, sc1)
        hc = modulate(rmsnorm(xc, norm1_g[l]), csh1, csc1)
        y, yc = mixing(h, hc, ang_row, ang_col, w_in[l], gla_a_up_f[l], gla_a_bias_f[l],
                       gla_a_up_b[l], gla_a_bias_b[l], gla_norm_g[l], att_qn_g[l], att_kn_g[l],
                       pool_w[l], pool_scale[l], w_branch[l], w_out[l], want_ctx)
        x = x + g1[:, None] * y
        h2 = modulate(rmsnorm(x, norm2_g[l]), sh2, sc2)
        moe_par = (moe_w_group[l], moe_b_group[l], moe_w_expert[l], moe_b_expert[l],
                   moe_w_gate[l], moe_w_up[l], moe_w_down[l])
        if want_ctx:
            xc = xc + cg1[:, None] * yc
            h2c = modulate(rmsnorm(xc, norm2_g[l]), csh2, csc2)
            m = routed_ffn(jnp.concatenate([h2, h2c], axis=1), *moe_par)
            x = x + g2[:, None] * m[:, :T]
            xc = xc + cg2[:, None] * m[:, T:]
        else:
            x = x + g2[:, None] * routed_ffn(h2, *moe_par)
    return rmsnorm(x, final_g)
```

```python
import os
import numpy as np
import concourse.bass as bass
import concourse.mybir as mybir
from concourse.bass_utils import run_bass_kernel_spmd
from concourse.alu_op_type import AluOpType as ALU
from contextlib import ExitStack

F32 = mybir.dt.float32
BF16 = mybir.dt.bfloat16
AF = mybir.ActivationFunctionType

PE, ACT, DVE, POOL, SP = "tensor", "scalar", "vector", "gpsimd", "sync"
ENGINES = [PE, ACT, DVE, POOL, SP]
DMAQ = (SP, ACT, POOL)
NSLOT = 8

D = 1024
T = 4096
C = 256
NT = T + C
NTILE = NT // 128
DEPTH = 2
INW = 5920
NE = 32
DE = 512
EPS = 1e-6
CAP = 1280
NROWS = NE * CAP
I32 = mybir.dt.int32
GROUPS = [(g * 512, 512, 0) for g in range(8)] + [(4096, 256, 1)]


class Tile:
    def __init__(self, t, name, psum=False):
        self.t = t
        self.name = name
        self.parts = {}
        self.psum = psum

    def __getitem__(self, idx):
        return self.t[idx]


class _Rec:
    def __init__(self):
        self.call = None

    def __getattr__(self, name):
        def f(*a, **kw):
            self.call = (name, a, kw)
            return self
        return f


class Prog:
    def __init__(self, nc):
        self.nc = nc
        self.es = ExitStack()
        self.ops = {e: [] for e in ENGINES}
        self.cnt = {e: 0 for e in ENGINES}
        self.dma_cnt = {e: 0 for e in ENGINES}
        self.known = {e: {} for e in ENGINES}
        self.sem = {}
        self.dsem = {}
        for e in ENGINES:
            self.sem[e] = self.es.enter_context(nc.semaphore("s_" + e))
        for e in DMAQ:
            self.dsem[e] = [self.es.enter_context(nc.semaphore("d_%s%d" % (e, i))) for i in range(NSLOT)]
        self.latest = {}
        self.out_events = []

    def sbuf(self, name, shape, dtype):
        return Tile(self.es.enter_context(self.nc.sbuf_tensor(name, list(shape), dtype)), name)

    def psum(self, name, shape, dtype=F32):
        return Tile(self.es.enter_context(self.nc.psum_tensor(name, list(shape), dtype)), name, psum=True)

    @staticmethod
    def _norm(acc):
        out = []
        for a in acc:
            if a is None:
                continue
            if isinstance(a, Tile):
                out.append((a, "*"))
            elif a[0].psum:
                out.append((a[0], "*"))
            else:
                out.append((a[0], a[1]))
        return out

    def _deps(self, eng, reads, writes):
        deps = []
        for (t, pt) in reads:
            states = list(t.parts.values()) if pt == "*" else [t.parts.get(pt), t.parts.get("*")]
            for s in states:
                if s is not None and s[0] is not None:
                    deps.append(s[0])
        for (t, pt) in writes:
            states = list(t.parts.values()) if pt == "*" else [t.parts.get(pt), t.parts.get("*")]
            for s in states:
                if s is not None:
                    if s[0] is not None:
                        deps.append(s[0])
                    deps.extend(s[1].values())
        need = {}
        for (src, val) in deps:
            if src == PE and eng == PE:
                continue
            if self.known[eng].get(src, 0) >= val:
                continue
            if need.get(src, 0) < val:
                need[src] = val
        for src, val in need.items():
            self.known[eng][src] = val
        return list(need.items())

    def _commit(self, ev, reads, writes):
        self.latest[ev[0]] = ev[1]
        for (t, pt) in reads:
            s = t.parts.setdefault(pt, [None, {}])
            s[1][ev[0]] = ev
        for (t, pt) in writes:
            if pt == "*":
                t.parts = {"*": [ev, {}]}
            else:
                t.parts[pt] = [ev, {}]

    def op(self, eng, fn, reads=(), writes=()):
        reads = self._norm(reads)
        writes = self._norm(writes)
        waits = self._deps(eng, reads, writes)
        self.cnt[eng] += 1
        ev = (eng, self.cnt[eng])
        r = _Rec()
        fn(r)
        self.ops[eng].append((r.call, waits, None))
        self._commit(ev, reads, writes)

    def dma(self, q, out, in_, reads=(), writes=(), is_output=False, **kw):
        reads = self._norm(reads)
        writes = self._norm(writes)
        j = self.dma_cnt[q]
        self.dma_cnt[q] += 1
        slot = j % NSLOT
        val = 16 * (j // NSLOT + 1)
        src = (q, slot)
        waits = self._deps(q, reads, writes)
        if val > 16 and self.known[q].get(src, 0) < val - 16:
            waits.append((src, val - 16))
            self.known[q][src] = val - 16
        ev = (src, val)

        self.ops[q].append((("dma_start", (), dict(out=out, in_=in_, **kw)), waits, slot))
        self._commit(ev, reads, writes)
        if is_output:
            self.out_events.append(ev)
        return ev

    def dma_call(self, q, method, kw, reads=(), writes=()):
        reads = self._norm(reads)
        writes = self._norm(writes)
        j = self.dma_cnt[q]
        self.dma_cnt[q] += 1
        slot = j % NSLOT
        val = 16 * (j // NSLOT + 1)
        src = (q, slot)
        waits = self._deps(q, reads, writes)
        if val > 16 and self.known[q].get(src, 0) < val - 16:
            waits.append((src, val - 16))
            self.known[q][src] = val - 16
        ev = (src, val)
        self.ops[q].append(((method, (), kw), waits, slot))
        self._commit(ev, reads, writes)
        return ev

    def barrier(self):
        for eng in ENGINES:
            waits = []
            for src, val in self.latest.items():
                if src == eng:
                    continue
                if self.known[eng].get(src, 0) < val:
                    waits.append((src, val))
                    self.known[eng][src] = val
            if waits:
                self.ops[eng].append((None, waits, None))

    def _semof(self, src):
        if isinstance(src, tuple):
            return self.dsem[src[0]][src[1]]
        return self.sem[src]

    def emit(self):
        nc = self.nc
        fin = []
        for src, val in self.latest.items():
            if self.known[SP].get(src, 0) < val and src != SP:
                fin.append((src, val))
        self.ops[SP].append((None, fin, None))
        with nc.Block() as block:
            for eng in ENGINES:
                def body(e, ops=self.ops[eng], eng=eng):
                    breg = None
                    for (fn, waits, slot) in ops:
                        for (src, val) in waits:
                            e.wait_ge(self._semof(src), val)
                        if fn is None:
                            continue
                        if fn[2].get('bounds_check') == 'NROWS_REG':
                            if breg is None:
                                breg = e.alloc_register()
                                e.reg_mov(breg, NROWS - 1)
                            fn = (fn[0], fn[1], dict(fn[2], bounds_check=breg))
                        try:
                            inst = getattr(e, fn[0])(*fn[1], **fn[2])
                        except Exception:
                            import traceback
                            traceback.print_exc()
                            for k_, v_ in fn[2].items():
                                if hasattr(v_, 'ap') and hasattr(v_, 'shape'):
                                    print('  AP', k_, v_.shape, v_.offset, v_.ap, v_.dtype, v_.tensor.name if hasattr(v_, 'tensor') else None)
                            print('EMIT FAIL', eng, fn[0], {k_: (getattr(v_, 'shape', v_), getattr(v_, 'dtype', None)) for k_, v_ in fn[2].items()})
                            raise
                        if slot is not None:
                            inst.then_inc(self.dsem[eng][slot], 16)
                        else:
                            inst.then_inc(self.sem[eng], 1)
                getattr(block, eng)(body)
        self.es.close()


class Arena:
    def __init__(self, p, nfloat):
        self.p = p
        self.base = p.sbuf("arena", [128, nfloat], F32)
        self.n = nfloat
        self.n0 = nfloat
        self.off = 0

    def reset(self):
        self.off = 0

    def reset_top(self):
        self.n = self.n0

    def alloc(self, name, shape, dtype, top=False):
        free = int(np.prod(shape[1:]))
        nf = free if dtype == F32 else (free + 1) // 2
        nf = (nf + 7) // 8 * 8
        assert self.off + nf <= self.n, "arena overflow %s %d+%d>%d" % (name, self.off, nf, self.n)
        if top:
            self.n -= nf
            v = self.base.t[0:shape[0], self.n:self.n + nf]
        else:
            v = self.base.t[0:shape[0], self.off:self.off + nf]
            self.off += nf
        if dtype != F32:
            v = v.bitcast(dtype)
        v = v[:, 0:free]
        if len(shape) == 3:
            v = v.rearrange("p (a b) -> p a b", a=shape[1])
        elif len(shape) == 4:
            v = v.rearrange("p (a b c) -> p a b c", a=shape[1], b=shape[2])
        return Tile(v, name)


class Rot:
    def __init__(self, tiles):
        self.tiles = tiles
        self.i = 0

    def next(self):
        t = self.tiles[self.i % len(self.tiles)]
        self.i += 1
        return t


def _consts():
    c = {}
    c["ident"] = np.eye(128, dtype=np.float32)
    c["ones"] = np.ones((128, 128), np.float32)
    blk = np.zeros((128, 128), np.float32)
    blk[:64, :64] = 1
    blk[64:, 64:] = 1
    c["blk64"] = blk
    rm = np.zeros((64, 64), np.float32)
    for a in range(2):
        for f in range(16):
            rm[a * 32 + f, a * 32 + 16 + f] = -1.0
            rm[a * 32 + 16 + f, a * 32 + f] = 1.0
    rot = np.zeros((128, 128), np.float32)
    rot[:64, :64] = rm.T
    rot[64:, 64:] = rm.T
    c["rotT"] = rot
    inv = (np.float32(10000.0) ** (-np.arange(0, 32, 2, dtype=np.float32) / np.float32(32))).astype(np.float32)
    t = np.arange(T)
    row = (t // 64).astype(np.float32)
    col = (t % 64).astype(np.float32)
    cos = np.ones((128, NT), np.float32)
    sin = np.zeros((128, NT), np.float32)
    for pp in range(128):
        d = pp % 64
        a = d // 32
        f = d % 16
        ang = ((row if a == 0 else col) * inv[f]).astype(np.float32)
        cos[pp, :T] = np.cos(ang)
        sin[pp, :T] = np.sin(ang)
    c["cos"] = cos
    c["sin"] = sin
    s = np.arange(128)[:, None]
    tt = np.arange(128)[None, :]
    same = (s // 64) == (tt // 64)
    c["ti_f"] = (same & (s <= tt)).astype(np.float32)
    c["ti_b"] = (same & (s >= tt)).astype(np.float32)
    c["te_f"] = (same & (s > tt)).astype(np.float32)
    c["te_b"] = (same & (s < tt)).astype(np.float32)
    band = np.zeros((4, 5, 128, 128), np.float32)
    wins = (2, 4, 8, 16)
    nseq = 3 * 128
    for gi, w in enumerate(wins):
        tg = np.arange(nseq)
        lo = np.clip(tg - w // 2, 0, nseq)
        hi = np.clip(tg + w // 2, 0, nseq)
        M = np.zeros((nseq, nseq), np.float32)
        for t_ in range(nseq):
            M[lo[t_]:hi[t_], t_] = np.float32(1.0) / np.float32(hi[t_] - lo[t_])
            M[t_, t_] -= 1.0
        band[gi, 0] = M[0:128, 0:128]
        band[gi, 1] = M[128:256, 128:256]
        band[gi, 2] = M[256:384, 256:384]
        band[gi, 3] = M[0:128, 128:256]
        band[gi, 4] = M[256:384, 128:256]
    c["band"] = band.transpose(2, 0, 1, 3).reshape(128, 20 * 128).copy()
    c["triu"] = (s < tt).astype(np.float32)
    eo = np.zeros((128, 64), np.float32)
    eo[:, 0:32] = np.arange(NE, dtype=np.float32)[None, :] * CAP
    eo[:, 32:64] = (np.arange(NE, dtype=np.float32)[None, :] + 1) * CAP
    c["eoff"] = eo
    return c


CONST_SHAPES = {"ident": [128, 128], "ones": [128, 128], "blk64": [128, 128], "rotT": [128, 128],
                "cos": [128, NT], "sin": [128, NT], "ti_f": [128, 128], "ti_b": [128, 128],
                "te_f": [128, 128], "te_b": [128, 128], "band": [128, 2560], "triu": [128, 128], "eoff": [128, 64]}


def _col(v, nch):
    return np.ascontiguousarray(np.asarray(v, np.float32).reshape(nch, 128).T)


def _prep_weights(inp):
    w = {}
    L = DEPTH
    w["w_mod"] = np.ascontiguousarray(inp["w_mod"], np.float32)
    w["w_in"] = np.ascontiguousarray(inp["w_in"], np.float32)
    w["w_branch"] = np.ascontiguousarray(inp["w_branch"], np.float32)
    w["w_out"] = np.ascontiguousarray(inp["w_out"], np.float32)
    w["moe_w_gate"] = np.ascontiguousarray(inp["moe_w_gate"], np.float32)
    w["moe_w_up"] = np.ascontiguousarray(inp["moe_w_up"], np.float32)
    w["moe_w_down"] = np.ascontiguousarray(inp["moe_w_down"], np.float32)
    w["pool_w"] = np.ascontiguousarray(inp["pool_w"], np.float32)
    w["b_mod_c"] = np.stack([_col(inp["b_mod"][l], 48) for l in range(L)])
    w["n1g_c"] = np.stack([_col(inp["norm1_g"][l], 8) for l in range(L)])
    w["n2g_c"] = np.stack([_col(inp["norm2_g"][l], 8) for l in range(L)])
    w["fing_c"] = _col(inp["final_g"], 8)
    w["psc_c"] = np.stack([_col(inp["pool_scale"][l], 4) for l in range(L)])
    up = np.zeros((L, 33, 512), np.float32)
    up[:, 0:16, 0:256] = inp["gla_a_up_f"]
    up[:, 16:32, 256:512] = inp["gla_a_up_b"]
    up[:, 32, 0:256] = inp["gla_a_bias_f"]
    up[:, 32, 256:512] = inp["gla_a_bias_b"]
    w["upcat"] = up
    qk = np.zeros((L, 128, 2), np.float32)
    qk[:, :, 0] = np.tile(np.asarray(inp["att_qn_g"], np.float32), (1, 2))
    qk[:, :, 1] = np.tile(np.asarray(inp["att_kn_g"], np.float32), (1, 2))
    w["qkg_c"] = qk
    w["glag_b"] = np.ascontiguousarray(np.broadcast_to(np.tile(np.asarray(inp["gla_norm_g"], np.float32), (1, 4))[:, None, :], (L, 128, 512)))
    w["w_rt"] = np.ascontiguousarray(np.concatenate([inp["moe_w_group"], inp["moe_w_expert"]], axis=2), np.float32)
    brt = np.concatenate([inp["moe_b_group"], inp["moe_b_expert"]], axis=1).astype(np.float32)
    w["b_rt"] = np.ascontiguousarray(np.broadcast_to(brt[:, None, :], (L, 128, 36)))
    return w


WEIGHT_SHAPES = {"w_mod": [2, 1024, 6144], "w_in": [2, 1024, INW], "w_branch": [2, 3, 512, 1024], "w_out": [2, 1024, 1024],
                 "moe_w_gate": [2, 32, 1024, 512], "moe_w_up": [2, 32, 1024, 512], "moe_w_down": [2, 32, 512, 1024],
                 "pool_w": [2, 4, 128, 128], "b_mod_c": [2, 128, 48], "n1g_c": [2, 128, 8], "n2g_c": [2, 128, 8],
                 "fing_c": [128, 8], "psc_c": [2, 128, 4], "upcat": [2, 33, 512], "qkg_c": [2, 128, 2],
                 "glag_b": [2, 128, 512], "w_rt": [2, 1024, 36], "b_rt": [2, 128, 36]}


def roundrobin(gens):
    gens = list(gens)
    while gens:
        nxt_ = []
        for g_ in gens:
            try:
                next(g_)
                nxt_.append(g_)
            except StopIteration:
                pass
        gens = nxt_


class Ctx:
    pass


def build_program(stop_after=None, dbg_outs=(), layers=DEPTH):
    nc = bass.Bass("TRN2", target_bir_lowering=False)
    p = Prog(nc)
    k = Ctx()
    k.nc, k.p = nc, p
    k.IN = {}

    def din(name, shape):
        k.IN[name] = nc.dram_tensor(name, list(shape), F32, kind="ExternalInput").ap()

    din("xin", [NT, D])
    din("cvec", [128, 8, 2])
    for n_, s_ in CONST_SHAPES.items():
        din(n_, s_)
    for n_, s_ in WEIGHT_SHAPES.items():
        din(n_, s_)
    k.out = nc.dram_tensor("out", [T, D], F32, kind="ExternalOutput").ap()

    def scratch(name, shape, dt):
        kind = "ExternalOutput" if name in dbg_outs else "Internal"
        return nc.dram_tensor(name, list(shape), dt, kind=kind).ap()

    k.xT = scratch("xT", [D, NT], F32)
    k.hT = scratch("hT", [D, NT], BF16)
    k.gqkT = scratch("gqkT", [512, NT], BF16)
    k.gkvr = scratch("gkvr", [NT, 1280], BF16)
    k.la = scratch("la", [NT, 512], F32)
    k.qaT = scratch("qaT", [512, NT], BF16)
    k.kaT = scratch("kaT", [128, NT], BF16)
    k.vaug = scratch("vaug", [NT, 130], BF16)
    k.pu = scratch("pu", [NT, 512], BF16)
    k.of = scratch("of", [NT, 512], F32)
    k.yglaT = scratch("yglaT", [512, NT], BF16)
    k.yattT = scratch("yattT", [512, NT], BF16)
    k.ypoolT = scratch("ypoolT", [512, NT], BF16)
    k.h2T = scratch("h2T", [D, NT], BF16)
    k.wdT = scratch("wdT", [128, NT], F32)
    k.Xg = scratch("Xg", [NROWS, D], BF16)
    k.Yg = scratch("Yg", [NROWS, D], BF16)
    k.dbg = {n_: scratch(n_, s_, F32) for n_, s_ in (("dbg_mod", [128, 96]),) if n_ in dbg_outs}

    k.psw = [p.psum("psw%d" % i, [128, 1024], F32) for i in range(4)]
    k.ps = []
    for i, w_ in enumerate(k.psw):
        k.ps.append(Tile(w_.t[:, 0:512], "ps%d" % (2 * i), psum=True))
        k.ps.append(Tile(w_.t[:, 512:1024], "ps%d" % (2 * i + 1), psum=True))
    k.ident = p.sbuf("c_ident", [128, 128], F32)
    k.identb = p.sbuf("c_identb", [128, 128], BF16)
    k.ones = p.sbuf("c_ones", [128, 128], F32)
    k.blk64 = p.sbuf("c_blk64", [128, 128], F32)
    k.rotTb = p.sbuf("c_rotTb", [128, 128], BF16)
    k.tri = {n_: p.sbuf("c_" + n_, [128, 128], F32) for n_ in ("ti_f", "ti_b", "te_f", "te_b")}
    k.maskb = {n_: p.sbuf("c_m" + n_, [128, 128], BF16) for n_ in ("ti_f", "ti_b")}
    k.cv = p.sbuf("cv", [128, 8, 2], F32)
    k.scv = p.sbuf("scv", [128, 8, 2], F32)
    k.modT = [p.sbuf("modT%d" % l, [128, 48, 2], F32) for l in range(DEPTH)]
    k.A1 = [p.sbuf("A1_%d" % l, [128, 8, 2], F32) for l in range(DEPTH)]
    k.A2 = [p.sbuf("A2_%d" % l, [128, 8, 2], F32) for l in range(DEPTH)]
    k.ngc = p.sbuf("ngc", [128, 8], F32)
    k.bmc = p.sbuf("bmc", [128, 48], F32)
    k.ridx = p.sbuf("ridx", [128, NTILE, 2], I32)
    k.rw = p.sbuf("rw", [128, NTILE, 2], F32)
    k.triub = p.sbuf("c_triub", [128, 128], BF16)
    k.onesb = p.sbuf("c_onesb", [128, 128], BF16)
    k.tokbuf = [p.sbuf("tokbuf%d" % i, [128, 1024], BF16) for i in range(4)]
    k.zerob = p.sbuf("c_zerob", [128, 1024], BF16)
    k.ar = Arena(p, 46000)

    for n_, t_ in (("ident", k.ident), ("ones", k.ones), ("blk64", k.blk64)):
        p.dma(SP, t_[:], k.IN[n_], writes=[t_])
    for n_ in k.tri:
        p.dma(SP, k.tri[n_][:], k.IN[n_], writes=[k.tri[n_]])
    p.dma(POOL, k.rotTb[:], k.IN["rotT"], writes=[k.rotTb])
    p.dma(POOL, k.identb[:], k.IN["ident"], writes=[k.identb])
    p.dma(POOL, k.triub[:], k.IN["triu"], writes=[k.triub])
    p.dma(POOL, k.onesb[:], k.IN["ones"], writes=[k.onesb])
    for n_ in k.maskb:
        p.dma(POOL, k.maskb[n_][:], k.IN[n_], writes=[k.maskb[n_]])
    p.op(DVE, lambda e: e.memset(k.zerob[:], 0.0), writes=[k.zerob])
    p.dma(SP, k.cv[:], k.IN["cvec"], writes=[k.cv])
    p.op(ACT, lambda e: e.activation(out=k.scv[:], in_=k.cv[:], func=AF.Silu), reads=[k.cv], writes=[k.scv])

    phases = []
    for l in range(layers):
        phases.append(("M%d" % l, lambda l=l: phase_M(k, l)))
    for l in range(layers):
        last = (l == DEPTH - 1)
        phases.append(("A%d" % l, lambda l=l: phase_A(k, l)))
        phases.append(("B%d" % l, lambda l=l, last=last: phase_B(k, l, last)))
        phases.append(("C%d" % l, lambda l=l, last=last: phase_C(k, l, last)))
        phases.append(("D%d" % l, lambda l=l, last=last: phase_D(k, l, last)))
        phases.append(("E%d" % l, lambda l=l, last=last: phase_E(k, l, last)))
        phases.append(("F%d" % l, lambda l=l, last=last: phase_F(k, l, last)))
    only = os.environ.get('PHASES')
    for name, fn in phases:
        if only and name not in only.split(','):
            continue
        p.barrier()
        k.ar.reset()
        if name[0] == 'F':
            k.ar.reset_top()
        if name[0] == 'C':
            prefetch_E(k, int(name[1]))
            k.Ew_l = int(name[1])
        if name[0] == 'E' and getattr(k, 'Ew_l', -1) != int(name[1]):
            prefetch_E(k, int(name[1]))
        fn()
        if stop_after == name:
            break
    p.emit()
    return nc


def phase_M(k, l):
    p, ar = k.p, k.ar
    wpool = Rot([ar.alloc("wm%d" % i, [128, 8, 512], F32) for i in range(2)])
    p.dma(SP, k.bmc[:], k.IN["b_mod_c"][l], writes=[k.bmc])
    pm = k.ps[0]
    for fg in range(12):
        wst = wpool.next()
        p.dma(SP if fg % 2 == 0 else ACT, wst[:], k.IN["w_mod"][l, :, fg * 512:(fg + 1) * 512].rearrange("(c p) n -> p c n", p=128), writes=[wst])
        for fc in range(4):
            for c in range(8):
                p.op(PE, lambda e, fc=fc, c=c, wst=wst: e.matmul(pm[:, fc * 2:fc * 2 + 2], lhsT=wst[:, c, fc * 128:(fc + 1) * 128],
                                                               rhs=k.scv[:, c, :], start=(c == 0), stop=(c == 7)),
                     reads=[wst, k.scv], writes=[pm])
        p.op(DVE, lambda e, fg=fg: e.tensor_tensor(out=k.modT[l][:, fg * 4:(fg + 1) * 4, :],
                                                  in0=pm[:, 0:8].rearrange("p (a b) -> p a b", a=4),
                                                  in1=k.bmc[:, fg * 4:(fg + 1) * 4].unsqueeze(2).broadcast_to([128, 4, 2]), op=ALU.add),
             reads=[pm, k.bmc], writes=[(k.modT[l], fg)])
    for (A, gname, c0) in ((k.A1[l], "n1g_c", 8), (k.A2[l], "n2g_c", 32)):
        p.dma(SP, k.ngc[:], k.IN[gname][l], writes=[k.ngc])
        p.op(DVE, lambda e, A=A, c0=c0: e.tensor_scalar(out=A[:], in0=k.modT[l][:, c0:c0 + 8, :], scalar1=1.0, scalar2=None, op0=ALU.add),
             reads=[k.modT[l]], writes=[A])
        p.op(DVE, lambda e, A=A: e.tensor_tensor(out=A[:], in0=A[:], in1=k.ngc[:].unsqueeze(2).broadcast_to([128, 8, 2]), op=ALU.mult),
             reads=[A, k.ngc], writes=[A])
    if "dbg_mod" in k.dbg:
        p.dma(POOL, k.dbg["dbg_mod"], k.modT[l][:].rearrange("p a b -> p (a b)"), reads=[k.modT[l]])


def phase_T0(k):
    p, ar = k.p, k.ar
    xin = k.IN["xin"]
    xl = Rot([ar.alloc("xl%d" % i, [128, 1024], F32) for i in range(3)])
    st = Rot([ar.alloc("xst%d" % i, [128, 8, 512], F32) for i in range(2)])
    pb = Rot([(k.ps[0], k.ps[1]), (k.ps[2], k.ps[3])])
    for (tok0, W, j) in GROUPS:
        stg = st.next()
        for s in range(W // 128):
            xt = xl.next()
            p.dma(SP, xt[:], xin[tok0 + s * 128: tok0 + (s + 1) * 128, :], writes=[xt])
            pa, pbk = pb.next()
            for c in range(8):
                pt = pa if c < 4 else pbk
                p.op(PE, lambda e, c=c, xt=xt, pt=pt: e.transpose(out=pt[:, (c % 4) * 128:(c % 4 + 1) * 128], in_=xt[:, c * 128:(c + 1) * 128], identity=k.ident[:]),
                     reads=[xt, k.ident], writes=[(pt, c % 4)])
            p.op(ACT, lambda e, s=s, stg=stg, pa=pa: e.activation(out=stg[:, 0:4, s * 128:(s + 1) * 128], in_=pa[:].rearrange("p (c n) -> p c n", c=4), func=AF.Copy),
                 reads=[pa], writes=[(stg, (s, 0))])
            p.op(DVE, lambda e, s=s, stg=stg, pbk=pbk: e.tensor_copy(out=stg[:, 4:8, s * 128:(s + 1) * 128], in_=pbk[:].rearrange("p (c n) -> p c n", c=4)),
                 reads=[pbk], writes=[(stg, (s, 1))])
        p.dma(POOL, k.xT[:, tok0:tok0 + W].rearrange("(c p) n -> p c n", p=128), stg[:, :, 0:W], reads=[stg])


def rms_group(k, xg, W, A, B, boff, j, hT32=None, hTb=None, sqt=None, pss=None, tmp=None):
    p = k.p
    p.op(ACT, lambda e: e.activation(out=sqt[:, :, 0:W], in_=xg[:, :, 0:W], func=AF.Square), reads=[xg], writes=[sqt])
    for c in range(8):
        p.op(PE, lambda e, c=c: e.matmul(pss[:, 0:W], lhsT=k.ones[:], rhs=sqt[:, c, 0:W], start=(c == 0), stop=(c == 7)),
             reads=[sqt, k.ones], writes=[pss])
    rs, rstd = tmp
    p.op(ACT, lambda e: e.activation(out=rs[:, 0:W], in_=pss[:, 0:W], func=AF.Sqrt, bias=EPS, scale=1.0 / D), reads=[pss], writes=[rs])
    p.op(DVE, lambda e: e.reciprocal(out=rstd[:, 0:W], in_=rs[:, 0:W]), reads=[rs], writes=[rstd])
    p.op(DVE, lambda e: e.tensor_tensor(out=sqt[:, :, 0:W], in0=xg[:, :, 0:W], in1=rstd[:, 0:W].unsqueeze(1).broadcast_to([128, 8, W]), op=ALU.mult),
         reads=[xg, rstd], writes=[sqt])
    for c in range(8):
        if hT32 is not None:
            p.op(ACT, lambda e, c=c: e.activation(out=hT32[:, c, 0:W], in_=sqt[:, c, 0:W], func=AF.Identity, scale=A[:, c, j:j + 1], bias=B[:, boff + c, j:j + 1]),
                 reads=[sqt, A, B], writes=[(hT32, c)])
            if hTb is not None:
                p.op(POOL, lambda e, c=c: e.tensor_copy(out=hTb[:, c, 0:W], in_=hT32[:, c, 0:W]), reads=[(hT32, c)], writes=[(hTb, c)])
        else:
            p.op(ACT, lambda e, c=c: e.activation(out=hTb[:, c, 0:W], in_=sqt[:, c, 0:W], func=AF.Identity, scale=A[:, c, j:j + 1], bias=B[:, boff + c, j:j + 1]),
                 reads=[sqt, A, B], writes=[(hTb, c)])


def phase_A(k, l):
    p, ar = k.p, k.ar
    w_in = k.IN["w_in"]
    NCOL = 2848
    wA = ar.alloc("wA", [128, 8, NCOL], BF16)
    for c0 in range(0, NCOL, 512):
        c1 = min(NCOL, c0 + 512)
        p.dma(POOL, wA[:, :, c0:c1], w_in[l, :, c0:c1].rearrange("(c p) n -> p c n", p=128), writes=[(wA, c0)])
    upc = ar.alloc("upc", [33, 512], F32)
    p.dma(SP, upc[:], k.IN["upcat"][l], writes=[upc])
    qkg = ar.alloc("qkg", [128, 2], F32)
    p.dma(SP, qkg[:], k.IN["qkg_c"][l], writes=[qkg])
    xgp = Rot([ar.alloc("xg%d" % i, [128, 8, 512], F32) for i in range(2)])
    sqt = ar.alloc("sqt", [128, 8, 512], F32)
    hTp = Rot([ar.alloc("hTb%d" % i, [128, 8, 512], BF16) for i in range(2)])
    rs = ar.alloc("rs", [128, 512], F32)
    rstd = ar.alloc("rstd", [128, 512], F32)
    nb_ = 1 if l == 0 else 2
    if l == 0:
        xlp = Rot([ar.alloc("xl%d" % i, [128, 1024], F32) for i in range(2)])
    cosp = Rot([ar.alloc("cos%d" % i, [128, 512], F32) for i in range(nb_)])
    sinp = Rot([ar.alloc("sin%d" % i, [128, 512], F32) for i in range(nb_)])
    stq = Rot([ar.alloc("stq%d" % i, [128, 4, 512], BF16) for i in range(2)])
    sta = Rot([ar.alloc("sta%d" % i, [128, 5, 512], BF16) for i in range(2)])
    glT = Rot([ar.alloc("glT%d" % i, [33, 512], F32) for i in range(2)])
    for g_ in glT.tiles:
        p.op(DVE, lambda e, g_=g_: e.memset(g_[:], 1.0), writes=[g_])
    sqh = Rot([ar.alloc("sqh%d" % i, [128, 512], F32) for i in range(2)])
    qg = Rot([ar.alloc("qg%d" % i, [128, 512], BF16) for i in range(2)])
    rsh = Rot([ar.alloc("rsh%d" % i, [128, 512], F32) for i in range(2)])
    t1 = Rot([ar.alloc("t1_%d" % i, [128, 512], F32) for i in range(nb_)])
    t2 = Rot([ar.alloc("t2_%d" % i, [128, 512], F32) for i in range(nb_)])
    stkvr = Rot([ar.alloc("stkvr%d" % i, [128, 1280], BF16) for i in range(2)])
    stva = Rot([ar.alloc("stva%d" % i, [128, 2, 65], BF16) for i in range(2)])
    for v_ in stva.tiles:
        p.op(DVE, lambda e, v_=v_: e.memset(v_[:], 1.0), writes=[v_])
    stpu = Rot([ar.alloc("stpu%d" % i, [128, 512], BF16) for i in range(2)])
    e1p = Rot([ar.alloc("e1_%d" % i, [128, 512], F32) for i in range(2)])
    stla = Rot([ar.alloc("stla%d" % i, [128, 512], F32) for i in range(2)])
    pss = k.ps[0]
    pfm = Rot([k.ps[1], k.ps[2]])
    pssh, prq = k.ps[3], k.ps[4]
    ptm = Rot([k.ps[5], k.ps[6], k.ps[7]])

    def fm_proj(col0, M, hTb, W):
        ps_ = pfm.next()
        for c in range(8):
            p.op(PE, lambda e, c=c: e.matmul(ps_[0:M, 0:W], lhsT=wA[:, c, col0:col0 + M], rhs=hTb[:, c, 0:W], start=(c == 0), stop=(c == 7)),
                 reads=[wA, hTb], writes=[ps_])
        return ps_

    def tm_proj(col0, N, hTb, s):
        ps_ = ptm.next()
        for c in range(8):
            p.op(PE, lambda e, c=c: e.matmul(ps_[:, 0:N], lhsT=hTb[:, c, s * 128:(s + 1) * 128], rhs=wA[:, c, col0:col0 + N], start=(c == 0), stop=(c == 7)),
                 reads=[wA, hTb], writes=[ps_])
        return ps_

    def A_prep(tok0, W, j):
        xg = xgp.next()
        if l == 0:
            for s in range(W // 128):
                xt = xlp.next()
                p.dma(SP, xt[:], k.IN["xin"][tok0 + s * 128:tok0 + (s + 1) * 128, :], writes=[xt])
                for half in range(2):
                    pt_ = ptm.next()
                    for c4 in range(4):
                        c = half * 4 + c4
                        p.op(PE, lambda e: e.transpose(out=pt_[:, c4 * 128:(c4 + 1) * 128], in_=xt[:, c * 128:(c + 1) * 128], identity=k.ident[:]),
                             reads=[xt, k.ident], writes=[pt_])
                    if half == 0:
                        p.op(ACT, lambda e: e.activation(out=xg[:, 0:4, s * 128:(s + 1) * 128], in_=pt_[:].rearrange("p (c n) -> p c n", c=4), func=AF.Copy),
                             reads=[pt_], writes=[(xg, (s, 0))])
                    else:
                        p.op(DVE, lambda e: e.tensor_copy(out=xg[:, 4:8, s * 128:(s + 1) * 128], in_=pt_[:].rearrange("p (c n) -> p c n", c=4)),
                             reads=[pt_], writes=[(xg, (s, 1))])
            p.dma(POOL, k.xT[:, tok0:tok0 + W].rearrange("(c p) n -> p c n", p=128), xg[:, :, 0:W], reads=[xg])
        else:
            p.dma(SP, xg[:, :, 0:W], k.xT[:, tok0:tok0 + W].rearrange("(c p) n -> p c n", p=128), writes=[xg])
        hTb = hTp.next()
        rms_group(k, xg, W, k.A1[l], k.modT[l], 0, j, hTb=hTb, sqt=sqt, pss=pss, tmp=(rs, rstd))
        p.dma(POOL, k.hT[:, tok0:tok0 + W].rearrange("(c p) n -> p c n", p=128), hTb[:, :, 0:W], reads=[hTb])
        return hTb

    def A_body(tok0, W, j, hTb):
        cs, sn = cosp.next(), sinp.next()
        p.dma(SP, cs[:, 0:W], k.IN["cos"][:, tok0:tok0 + W], writes=[cs])
        p.dma(SP, sn[:, 0:W], k.IN["sin"][:, tok0:tok0 + W], writes=[sn])
        sq_ = stq.next()
        for i in range(4):
            ps_ = fm_proj(i * 128, 128, hTb, W)
            if i < 2:
                p.op(ACT, lambda e, i=i, ps_=ps_: e.activation(out=sq_[:, i, 0:W], in_=ps_[:, 0:W], func=AF.Copy, scale=0.125), reads=[ps_], writes=[(sq_, i)])
            else:
                p.op(DVE, lambda e, i=i, ps_=ps_: e.tensor_copy(out=sq_[:, i, 0:W], in_=ps_[:, 0:W]), reads=[ps_], writes=[(sq_, i)])
        p.dma(POOL, k.gqkT[:, tok0:tok0 + W].rearrange("(c p) n -> p c n", p=128), sq_[:, :, 0:W], reads=[sq_])
        gl_ = glT.next()
        ps_ = fm_proj(1536, 32, hTb, W)
        p.op(DVE, lambda e, ps_=ps_: e.tensor_copy(out=gl_[0:32, 0:W], in_=ps_[0:32, 0:W]), reads=[ps_], writes=[(gl_, 0)])
        sa_ = sta.next()
        for i in range(5):
            ps_ = fm_proj(1568 + i * 128, 128, hTb, W)
            gcol = qkg[:, 0:1] if i < 4 else qkg[:, 1:2]
            sqh_, qg_, rsh_, t1_, t2_ = sqh.next(), qg.next(), rsh.next(), t1.next(), t2.next()
            p.op(ACT, lambda e, ps_=ps_, sqh_=sqh_: e.activation(out=sqh_[:, 0:W], in_=ps_[:, 0:W], func=AF.Square), reads=[ps_], writes=[sqh_])
            p.op(ACT, lambda e, ps_=ps_, qg_=qg_, gcol=gcol: e.activation(out=qg_[:, 0:W], in_=ps_[:, 0:W], func=AF.Copy, scale=gcol), reads=[ps_, qkg], writes=[qg_])
            p.op(PE, lambda e, sqh_=sqh_: e.matmul(pssh[:, 0:W], lhsT=k.blk64[:], rhs=sqh_[:, 0:W], start=True, stop=True), reads=[sqh_, k.blk64], writes=[pssh])
            p.op(PE, lambda e, qg_=qg_: e.matmul(prq[:, 0:W], lhsT=k.rotTb[:], rhs=qg_[:, 0:W], start=True, stop=True), reads=[qg_, k.rotTb], writes=[prq])
            p.op(ACT, lambda e, rsh_=rsh_: e.activation(out=rsh_[:, 0:W], in_=pssh[:, 0:W], func=AF.Sqrt, bias=EPS, scale=1.0 / 64), reads=[pssh], writes=[rsh_])
            p.op(DVE, lambda e, rsh_=rsh_: e.reciprocal(out=rsh_[:, 0:W], in_=rsh_[:, 0:W]), reads=[rsh_], writes=[rsh_])
            p.op(DVE, lambda e, t2_=t2_: e.tensor_tensor(out=t2_[:, 0:W], in0=prq[:, 0:W], in1=sn[:, 0:W], op=ALU.mult), reads=[prq, sn], writes=[t2_])
            p.op(POOL, lambda e, t1_=t1_, qg_=qg_: e.tensor_tensor(out=t1_[:, 0:W], in0=qg_[:, 0:W], in1=cs[:, 0:W], op=ALU.mult), reads=[qg_, cs], writes=[t1_])
            p.op(POOL, lambda e, t1_=t1_, t2_=t2_: e.tensor_tensor(out=t1_[:, 0:W], in0=t1_[:, 0:W], in1=t2_[:, 0:W], op=ALU.add), reads=[t1_, t2_], writes=[t1_])
            p.op(DVE, lambda e, i=i, t1_=t1_, rsh_=rsh_: e.tensor_tensor(out=sa_[:, i, 0:W], in0=t1_[:, 0:W], in1=rsh_[:, 0:W], op=ALU.mult), reads=[t1_, rsh_], writes=[(sa_, i)])
        p.dma(POOL, k.qaT[:, tok0:tok0 + W].rearrange("(c p) n -> p c n", p=128), sa_[:, 0:4, 0:W], reads=[sa_])
        p.dma(POOL, k.kaT[:, tok0:tok0 + W], sa_[:, 4, 0:W], reads=[sa_])
        for s in range(W // 128):
            r0 = tok0 + s * 128
            sk = stkvr.next()
            psa = tm_proj(256, 512, hTb, s)
            p.op(ACT, lambda e, psa=psa, sk=sk: e.activation(out=sk[:, 0:512], in_=psa[:, 0:512], func=AF.Copy), reads=[psa], writes=[(sk, 0)])
            psb = tm_proj(768, 512, hTb, s)
            p.op(DVE, lambda e, psb=psb, sk=sk: e.tensor_copy(out=sk[:, 512:1024], in_=psb[:, 0:512]), reads=[psb], writes=[(sk, 1)])
            psc = tm_proj(1280, 256, hTb, s)
            p.op(ACT, lambda e, psc=psc, sk=sk: e.activation(out=sk[:, 1024:1280], in_=psc[:, 0:256], func=AF.Copy), reads=[psc], writes=[(sk, 2)])
            p.dma(POOL, k.gkvr[r0:r0 + 128, :], sk[:], reads=[sk])
            sv = stva.next()
            psd = tm_proj(2208, 128, hTb, s)
            p.op(DVE, lambda e, psd=psd, sv=sv: e.tensor_copy(out=sv[:, :, 0:64], in_=psd[:, 0:128].rearrange("p (g d) -> p g d", g=2)), reads=[psd], writes=[(sv, 0)])
            p.dma(POOL, k.vaug[r0:r0 + 128, :], sv[:].rearrange("p g d -> p (g d)"), reads=[sv])
            su = stpu.next()
            pse = tm_proj(2336, 512, hTb, s)
            p.op(ACT, lambda e, pse=pse, su=su: e.activation(out=su[:], in_=pse[:, 0:512], func=AF.Copy), reads=[pse], writes=[su])
            p.dma(POOL, k.pu[r0:r0 + 128, :], su[:], reads=[su])
            psl = ptm.next()
            p.op(PE, lambda e, s=s, psl=psl: e.matmul(psl[:, 0:512], lhsT=gl_[0:33, s * 128:(s + 1) * 128], rhs=upc[0:33, :], start=True, stop=True),
                 reads=[gl_, upc], writes=[psl])
            e1, sl = e1p.next(), stla.next()
            p.op(ACT, lambda e, psl=psl, e1=e1: e.activation(out=e1[:], in_=psl[:, 0:512], func=AF.Exp, scale=-1.0), reads=[psl], writes=[e1])
            p.op(ACT, lambda e, e1=e1: e.activation(out=e1[:], in_=e1[:], func=AF.Ln, bias=1.0, scale=1.0), reads=[e1], writes=[e1])
            p.op(DVE, lambda e, e1=e1, sl=sl: e.tensor_scalar(out=sl[:], in0=e1[:], scalar1=-1.0 / 16, scalar2=None, op0=ALU.mult), reads=[e1], writes=[sl])
            p.dma(POOL, k.la[r0:r0 + 128, :], sl[:], reads=[sl])

    cur = A_prep(*GROUPS[0])
    for gi, g_ in enumerate(GROUPS):
        nxt_h = A_prep(*GROUPS[gi + 1]) if gi + 1 < len(GROUPS) else None
        A_body(*g_, cur)
        cur = nxt_h


def make_in_maps(inputs, cores=range(8)):
    consts = _consts()
    w = _prep_weights(inputs)
    x = np.asarray(inputs["x"], np.float32)
    ctx = np.asarray(inputs["ctx"], np.float32)
    c = np.asarray(inputs["c"], np.float32)
    cc = np.asarray(inputs["c_ctx"], np.float32)
    maps = []
    for b in cores:
        m = {}
        m["xin"] = np.ascontiguousarray(np.concatenate([x[b], ctx[b]], axis=0))
        cv = np.stack([_col(c[b], 8), _col(cc, 8)], axis=2)
        m["cvec"] = np.ascontiguousarray(cv)
        m.update(consts)
        m.update(w)
        maps.append(m)
    return maps


_NC_CACHE = {}


def kernel(**inputs):
    if "nc" not in _NC_CACHE:
        _NC_CACHE["nc"] = build_program()
    nc = _NC_CACHE["nc"]
    maps = make_in_maps(inputs)
    res = run_bass_kernel_spmd(nc, maps, core_ids=list(range(8)))
    return np.stack([np.asarray(r["out"], np.float32) for r in res.results], axis=0)


def phase_B(k, l, last):
    p, ar = k.p, k.ar
    AXX = mybir.AxisListType.X
    glag = ar.alloc("glag", [128, 512], F32)
    p.dma(SP, glag[:], k.IN["glag_b"][l], writes=[glag])
    lap = Rot([ar.alloc("laT%d" % i, [128, 256], F32) for i in range(2)])
    qkp = Rot([ar.alloc("qk%d" % i, [128, 4, 128], BF16) for i in range(2)])
    kvp = Rot([ar.alloc("kvr%d" % i, [128, 1280], BF16) for i in range(2)])
    ofp = Rot([ar.alloc("ofl%d" % i, [128, 512], F32) for i in range(2)])
    Ep = Rot([ar.alloc("E%d" % i, [128, 2, 128], F32) for i in range(2)])
    Eip = Rot([ar.alloc("Ei%d" % i, [128, 2, 128], F32) for i in range(2)])
    Qbp = Rot([ar.alloc("Qb%d" % i, [128, 2, 128], BF16) for i in range(2)])
    Kip = Rot([ar.alloc("Ki%d" % i, [128, 2, 128], BF16) for i in range(2)])
    EXp = Rot([ar.alloc("EX%d" % i, [128, 256], F32) for i in range(2)])
    Klp = Rot([ar.alloc("Kl%d" % i, [128, 256], BF16) for i in range(2)])
    ATp = Rot([ar.alloc("AT%d" % i, [128, 4, 128], BF16) for i in range(2)])
    S32 = [ar.alloc("S32_%d" % h, [128, 128], F32) for h in range(4)]
    Sbp = [Rot([ar.alloc("Sb%d_%d" % (h, i), [128, 128], BF16) for i in range(2)]) for h in range(4)]
    ofst = Rot([ar.alloc("ofst%d" % i, [128, 512], F32) for i in range(2)])
    osum = Rot([ar.alloc("osum%d" % i, [128, 512], F32) for i in range(2)])
    sqo = ar.alloc("sqo", [128, 512], F32)
    ssg = Rot([ar.alloc("ssg%d" % i, [128, 4], F32) for i in range(2)])
    srp = Rot([ar.alloc("sr%d" % i, [128, 512], F32) for i in range(2)])
    ytk = Rot([ar.alloc("ytk%d" % i, [128, 512], BF16) for i in range(2)])
    ystp = Rot([ar.alloc("ystB%d" % i, [128, 4, 512], BF16) for i in range(2)])
    pbT = k.ps[0]
    pA = k.ps[1]
    pAo = k.ps[6]
    pOh = [k.ps[2], k.ps[3], k.ps[4], k.ps[5]]
    pDp = Rot([k.ps[7]])
    pT_ap = k.ps[1].t[:, 0:256].bitcast(BF16)

    for dirn in (0, 1):
        if dirn == 1:
            p.barrier()
        tri_i = k.tri["ti_f" if dirn == 0 else "ti_b"]
        tri_e = k.tri["te_f" if dirn == 0 else "te_b"]
        msk = k.maskb["ti_f" if dirn == 0 else "ti_b"]
        order = [32, 33] + list(range(32)) if dirn == 0 else [33, 32] + list(range(31, -1, -1))
        jorder = (0, 1) if dirn == 0 else (1, 0)
        Sb = []
        for h in range(4):
            p.op(POOL, lambda e, h=h: e.memset(S32[h][:], 0.0), writes=[S32[h]])
            sb = Sbp[h].next()
            p.op(POOL, lambda e, sb=sb: e.memset(sb[:], 0.0), writes=[sb])
            Sb.append(sb)
        yst = None
        for tt in order:
            rows = slice(tt * 128, (tt + 1) * 128)
            is_ctx = tt >= 32
            want_o = not (is_ctx and last)
            laT, qk, kvr = lap.next(), qkp.next(), kvp.next()
            p.dma(SP, laT[:], k.la[rows, dirn * 256:(dirn + 1) * 256], writes=[laT])
            p.dma(SP, qk[:], k.gqkT[:, rows].rearrange("(c p) n -> p c n", p=128), writes=[qk])
            p.dma(SP, kvr[:], k.gkvr[rows, :], writes=[kvr])
            if dirn == 1 and want_o:
                ofl = ofp.next()
                p.dma(SP, ofl[:], k.of[rows, :], writes=[ofl])
            for hp in range(2):
                p.op(PE, lambda e, hp=hp: e.matmul(pbT[:, hp * 128:(hp + 1) * 128], lhsT=laT[:, hp * 128:(hp + 1) * 128], rhs=tri_i[:], start=True, stop=True),
                     reads=[laT, tri_i], writes=[(pbT, hp)])
            p.op(PE, lambda e: e.matmul(pbT[:, 256:512], lhsT=tri_e[:], rhs=laT[:], start=True, stop=True), reads=[laT, tri_e], writes=[pbT])
            E, Ei, Qb, Ki = Ep.next(), Eip.next(), Qbp.next(), Kip.next()
            p.op(ACT, lambda e: e.activation(out=E[:].rearrange("p a b -> p (a b)"), in_=pbT[:, 0:256], func=AF.Exp), reads=[pbT], writes=[E])
            p.op(ACT, lambda e: e.activation(out=Ei[:].rearrange("p a b -> p (a b)"), in_=pbT[:, 0:256], func=AF.Exp, scale=-1.0), reads=[pbT], writes=[Ei])
            p.op(DVE, lambda e: e.tensor_tensor(out=Qb[:], in0=qk[:, 0:2, :], in1=E[:], op=ALU.mult), reads=[qk, E], writes=[Qb])
            p.op(POOL, lambda e: e.tensor_tensor(out=Ki[:], in0=qk[:, 2:4, :], in1=Ei[:], op=ALU.mult), reads=[qk, Ei], writes=[Ki])
            EX, Kl = EXp.next(), Klp.next()
            p.op(ACT, lambda e: e.activation(out=EX[:], in_=pbT[:, 256:512], func=AF.Exp), reads=[pbT], writes=[EX])
            p.op(DVE, lambda e: e.tensor_tensor(out=Kl[:], in0=kvr[:, 0:256], in1=EX[:], op=ALU.mult), reads=[kvr, EX], writes=[Kl])
            if want_o:
                if dirn == 0:
                    ofs = ofst.next()
                else:
                    osm = osum.next()
            HR = [(h // 2, (h % 2) * 64) for h in range(4)]
            if want_o:
                for h in range(4):
                    hp, r0 = HR[h]
                    pAh = pA if r0 == 0 else pAo
                    p.op(PE, lambda e: e.matmul(pAh[:, h * 128:(h + 1) * 128], lhsT=Ki[r0:r0 + 64, hp, :], rhs=Qb[r0:r0 + 64, hp, :], start=True, stop=True),
                         reads=[Ki, Qb], writes=[pAh])
                AT4 = ATp.next()
                for h in range(4):
                    pAh = pA if h % 2 == 0 else pAo
                    p.op(DVE, lambda e: e.tensor_tensor(out=AT4[:, h, :], in0=pAh[:, h * 128:(h + 1) * 128], in1=msk[:], op=ALU.mult), reads=[pAh, msk], writes=[(AT4, h)])
                for h in range(4):
                    p.op(PE, lambda e: e.matmul(pOh[h][:, 0:128], lhsT=AT4[:, h, :], rhs=kvr[:, 256 + h * 128:256 + (h + 1) * 128], start=True, stop=False),
                         reads=[AT4, kvr], writes=[pOh[h]])
            for j in jorder:
                js = slice(j * 64, (j + 1) * 64)
                pD = pDp.next()
                col = j * 64 + 63 if dirn == 0 else j * 64
                for h in range(4):
                    hp, r0 = HR[h]
                    if want_o:
                        p.op(PE, lambda e: e.matmul(pOh[h][js, 0:128], lhsT=Qb[r0:r0 + 64, hp, js], rhs=Sb[h][r0:r0 + 64, :], start=False, stop=True),
                             reads=[Qb, Sb[h]], writes=[pOh[h]])
                    p.op(PE, lambda e: e.matmul(pD[r0:r0 + 64, h * 128:(h + 1) * 128], lhsT=Kl[js, h * 64:(h + 1) * 64], rhs=kvr[js, 256 + h * 128:256 + (h + 1) * 128], start=True, stop=True),
                         reads=[Kl, kvr], writes=[pD])
                for h in range(4):
                    hp, r0 = HR[h]
                    p.op(DVE, lambda e: e.scalar_tensor_tensor(out=S32[h][r0:r0 + 64, :], in0=S32[h][r0:r0 + 64, :], scalar=E[r0:r0 + 64, hp, col:col + 1],
                                                               in1=pD[r0:r0 + 64, h * 128:(h + 1) * 128], op0=ALU.mult, op1=ALU.add),
                         reads=[S32[h], E, pD], writes=[S32[h]])
                for h in range(4):
                    hp, r0 = HR[h]
                    sb = Sbp[h].next()
                    p.op(ACT, lambda e: e.activation(out=sb[r0:r0 + 64, :], in_=S32[h][r0:r0 + 64, :], func=AF.Copy), reads=[S32[h]], writes=[sb])
                    Sb[h] = sb
            if want_o:
                for h in range(4):
                    hs = slice(h * 128, (h + 1) * 128)
                    if dirn == 0:
                        p.op(ACT, lambda e: e.activation(out=ofs[:, hs], in_=pOh[h][:, 0:128], func=AF.Copy), reads=[pOh[h]], writes=[(ofs, h)])
                    else:
                        p.op(DVE, lambda e: e.tensor_tensor(out=osm[:, hs], in0=pOh[h][:, 0:128], in1=ofl[:, hs], op=ALU.add), reads=[pOh[h], ofl], writes=[(osm, h)])
            if not want_o:
                continue
            if dirn == 0:
                p.dma(POOL, k.of[rows, :], ofs[:], reads=[ofs])
                continue
            ss, sr, yt = ssg.next(), srp.next(), ytk.next()
            p.op(POOL, lambda e: e.tensor_tensor(out=sqo[:], in0=osm[:], in1=osm[:], op=ALU.mult), reads=[osm], writes=[sqo])
            p.op(DVE, lambda e: e.reduce_sum(out=ss[:], in_=sqo[:].rearrange("p (h d) -> p h d", h=4), axis=AXX), reads=[sqo], writes=[ss])
            p.op(ACT, lambda e: e.activation(out=ss[:], in_=ss[:], func=AF.Sqrt, bias=EPS, scale=1.0 / 128), reads=[ss], writes=[ss])
            p.op(DVE, lambda e: e.reciprocal(out=ss[:], in_=ss[:]), reads=[ss], writes=[ss])
            p.op(DVE, lambda e: e.tensor_tensor(out=osm[:].rearrange("p (h d) -> p h d", h=4), in0=osm[:].rearrange("p (h d) -> p h d", h=4),
                                                in1=ss[:].unsqueeze(2).broadcast_to([128, 4, 128]), op=ALU.mult), reads=[osm, ss], writes=[osm])
            p.op(POOL, lambda e: e.tensor_tensor(out=osm[:], in0=osm[:], in1=glag[:], op=ALU.mult), reads=[osm, glag], writes=[osm])
            p.op(ACT, lambda e: e.activation(out=sr[:], in_=kvr[:, 768:1280], func=AF.Silu), reads=[kvr], writes=[sr])
            p.op(DVE, lambda e: e.tensor_tensor(out=yt[:], in0=osm[:], in1=sr[:], op=ALU.mult), reads=[osm, sr], writes=[yt])
            grp0 = (tt // 4) * 4
            gsz = 4 if tt < 32 else 2
            pos = tt - grp0
            if yst is None:
                yst = ystp.next()
            for c in range(4):
                p.op(PE, lambda e, c=c: e.transpose(out=pT_ap[:, c * 128:(c + 1) * 128], in_=yt[:, c * 128:(c + 1) * 128], identity=k.identb[:]),
                     reads=[yt, k.identb], writes=[pA])
            p.op(ACT, lambda e: e.activation(out=yst[:, :, pos * 128:(pos + 1) * 128], in_=pT_ap[:].rearrange("p (c n) -> p c n", c=4), func=AF.Copy),
                 reads=[pA], writes=[(yst, pos)])
            done = (pos == 0) if dirn == 1 else (pos == gsz - 1)
            if done:
                p.dma(POOL, k.yglaT[:, grp0 * 128:(grp0 + gsz) * 128].rearrange("(c p) n -> p c n", p=128), yst[:, :, 0:gsz * 128], reads=[yst])
                yst = None


def phase_C(k, l, last):
    p, ar = k.p, k.ar
    kd = ar.alloc("kd", [128, 2, NT], BF16)
    for g in range(2):
        for half in range(2):
            p.dma(SP, kd[half * 64:(half + 1) * 64, g, :], k.kaT[g * 64:(g + 1) * 64, :], writes=[(kd, (g, half))])
    va = ar.alloc("va", [128, NTILE, 130], BF16)
    p.dma(SP, va[:], k.vaug.rearrange("(t p) n -> p t n", p=128), writes=[va])
    qap = Rot([ar.alloc("qa%d" % i, [128, 4, 512], BF16) for i in range(2)])
    ptp = Rot([ar.alloc("pt%d" % i, [128, 1024], BF16) for i in range(3)])
    ytp = Rot([ar.alloc("ytC%d" % i, [128, 4, 512], BF16) for i in range(2)])
    ystp = Rot([ar.alloc("ystC%d" % i, [128, 4, 512], BF16) for i in range(2)])
    recp = Rot([ar.alloc("rec%d" % i, [128, 1], F32) for i in range(4)])
    pSw = Rot([0, 1])
    pO = [k.ps[4], k.ps[5], k.ps[6], k.ps[7]]
    pT_ap = k.psw[1].t[:, 0:256].bitcast(BF16)
    pT_t = k.ps[2]
    for (tok0, W, j) in GROUPS:
        if j == 1 and last:
            continue
        qa = qap.next()
        p.dma(SP, qa[:, :, 0:W], k.qaT[:, tok0:tok0 + W].rearrange("(c p) n -> p c n", p=128), writes=[qa])
        kts = list(range(NTILE)) if j == 0 else [32, 33]
        pairs = [(kts[i], kts[i + 1]) for i in range(0, len(kts), 2)]
        ns = W // 128
        yt = ytp.next()
        pend = []

        def qk(h, pr):
            hp, r0, g = h // 2, (h % 2) * 64, h // 4
            wi = pSw.next()
            banks = (k.ps[2 * wi], k.ps[2 * wi + 1])
            for half, kt in enumerate(pr):
                p.op(PE, lambda e: e.matmul(banks[half][:, 0:W], lhsT=kd[r0:r0 + 64, g, kt * 128:(kt + 1) * 128], rhs=qa[r0:r0 + 64, hp, 0:W], start=True, stop=True),
                     reads=[kd, qa], writes=[banks[half]])
            pt = ptp.next()
            if W == 512:
                p.op(ACT, lambda e: e.activation(out=pt[:, 0:1024], in_=k.psw[wi].t[:, 0:1024], func=AF.Exp, scale=0.125), reads=[banks[0], banks[1]], writes=[pt])
            else:
                for half in range(2):
                    p.op(ACT, lambda e: e.activation(out=pt[:, half * 512:half * 512 + W], in_=banks[half][:, 0:W], func=AF.Exp, scale=0.125), reads=[banks[half]], writes=[(pt, half)])
            pend.append((h, pr, pt))

        def pv():
            h, pr, pt = pend.pop(0)
            g = h // 4
            for half, kt in enumerate(pr):
                for s in range(ns):
                    p.op(PE, lambda e, s=s: e.matmul(pO[s][:, 0:65], lhsT=pt[:, half * 512 + s * 128:half * 512 + (s + 1) * 128], rhs=va[:, kt, g * 65:(g + 1) * 65],
                                                     start=(kt == kts[0]), stop=(kt == kts[-1])), reads=[pt, va], writes=[pO[s]])
            if pr is pairs[-1]:
                for s in range(ns):
                    rec = recp.next()
                    p.op(DVE, lambda e, s=s: e.reciprocal(out=rec[:], in_=pO[s][:, 64:65]), reads=[pO[s]], writes=[rec])
                    p.op(DVE, lambda e, s=s: e.tensor_scalar(out=yt[:, s, h * 64:(h + 1) * 64], in0=pO[s][:, 0:64], scalar1=rec[:, 0:1], scalar2=None, op0=ALU.mult),
                         reads=[pO[s], rec], writes=[(yt, (s, h))])

        seq = [(h, pr) for h in range(8) for pr in pairs]
        for i_, (h, pr) in enumerate(seq):
            qk(h, pr)
            if i_ >= 1:
                pv()
        while pend:
            pv()
        yst = ystp.next()
        for s in range(ns):
            for c in range(4):
                p.op(PE, lambda e, s=s, c=c: e.transpose(out=pT_ap[:, c * 128:(c + 1) * 128], in_=yt[:, s, c * 128:(c + 1) * 128], identity=k.identb[:]),
                     reads=[yt, k.identb], writes=[pT_t])
            p.op(DVE, lambda e, s=s: e.tensor_copy(out=yst[:, :, s * 128:(s + 1) * 128], in_=pT_ap[:].rearrange("p (c n) -> p c n", c=4)), reads=[pT_t], writes=[(yst, s)])
        p.dma(POOL, k.yattT[:, tok0:tok0 + W].rearrange("(c p) n -> p c n", p=128), yst[:, :, 0:W], reads=[yst])


def phase_D(k, l, last):
    p, ar = k.p, k.ar
    pus = ar.alloc("pus", [128, NTILE, 512], BF16)
    p.dma(SP, pus[:], k.pu.rearrange("(t p) n -> p t n", p=128), writes=[pus])
    bandb = ar.alloc("bandb", [128, 20, 128], BF16)
    for i in range(2):
        p.dma(POOL, bandb[:, i * 10:(i + 1) * 10, :], k.IN["band"][:, i * 1280:(i + 1) * 1280].rearrange("p (a b) -> p a b", a=10), writes=[(bandb, i)])
    pw = ar.alloc("pw", [128, 4, 128], BF16)
    p.dma(POOL, pw[:], k.IN["pool_w"][l].rearrange("g c e -> c g e"), writes=[pw])
    psc = ar.alloc("psc", [128, 4], F32)
    p.dma(SP, psc[:], k.IN["psc_c"][l], writes=[psc])
    dTp = Rot([ar.alloc("dT%d" % i, [128, 512], BF16) for i in range(3)])
    ystp = Rot([ar.alloc("ystD%d" % i, [128, 4, 512], BF16) for i in range(2)])
    pdp = Rot([k.ps[0], k.ps[1], k.ps[2]])
    pyp = Rot([k.ps[3], k.ps[4], k.ps[5]])
    for (tok0, W, j) in GROUPS:
        if j == 1 and last:
            continue
        first, lastt = (0, 31) if j == 0 else (32, 33)
        t0 = tok0 // 128
        yst = ystp.next()
        for g in range(4):
            pd = pdp.next()
            for i in range(W // 128):
                ti = t0 + i
                srcs = []
                if ti > first:
                    srcs.append((ti - 1, 3))
                srcs.append((ti, 0 if ti == first else (2 if ti == lastt else 1)))
                if ti < lastt:
                    srcs.append((ti + 1, 4))
                for n_, (src, typ) in enumerate(srcs):
                    p.op(PE, lambda e, i=i, src=src, typ=typ, n_=n_: e.matmul(pd[:, i * 128:(i + 1) * 128], lhsT=pus[:, src, g * 128:(g + 1) * 128], rhs=bandb[:, g * 5 + typ, :],
                                                                            start=(n_ == 0), stop=(n_ == len(srcs) - 1)), reads=[pus, bandb], writes=[pd])
            dT = dTp.next()
            p.op(ACT, lambda e: e.activation(out=dT[:, 0:W], in_=pd[:, 0:W], func=AF.Copy), reads=[pd], writes=[dT])
            py = pyp.next()
            p.op(PE, lambda e: e.matmul(py[:, 0:W], lhsT=pw[:, g, :], rhs=dT[:, 0:W], start=True, stop=True), reads=[pw, dT], writes=[py])
            p.op(DVE, lambda e: e.tensor_scalar(out=yst[:, g, 0:W], in0=py[:, 0:W], scalar1=psc[:, g:g + 1], scalar2=None, op0=ALU.mult), reads=[py, psc], writes=[(yst, g)])
        p.dma(POOL, k.ypoolT[:, tok0:tok0 + W].rearrange("(c p) n -> p c n", p=128), yst[:, :, 0:W], reads=[yst])


def prefetch_E(k, l):
    p, ar = k.p, k.ar
    w_in = k.IN["w_in"]
    wg = ar.alloc("wg", [128, 8, 3072], BF16, top=True)
    for i in range(6):
        p.dma(POOL, wg[:, :, i * 512:(i + 1) * 512], w_in[l, :, 2848 + i * 512:2848 + (i + 1) * 512].rearrange("(c p) n -> p c n", p=128), writes=[(wg, i)])
    wb = ar.alloc("wb", [128, 3, 4, 1024], BF16, top=True)
    for br in range(3):
        p.dma(POOL, wb[:, br, :, :], k.IN["w_branch"][l, br].rearrange("(c p) n -> p c n", p=128), writes=[(wb, br)])
    wo = ar.alloc("wo", [128, 8, 1024], BF16, top=True)
    p.dma(POOL, wo[:], k.IN["w_out"][l].rearrange("(c p) n -> p c n", p=128), writes=[wo])
    k.Ew = (wg, wb, wo)


def phase_E(k, l, last):
    p, ar = k.p, k.ar
    AXX = mybir.AxisListType.X
    w_in = k.IN["w_in"]
    wg, wb, wo = k.Ew
    wr = ar.alloc("wr", [128, 8, 36], F32)
    p.dma(SP, wr[:], k.IN["w_rt"][l].rearrange("(c p) n -> p c n", p=128), writes=[wr])
    brt = ar.alloc("brt", [128, 36], F32)
    p.dma(SP, brt[:], k.IN["b_rt"][l], writes=[brt])
    hTb = ar.alloc("hTbE", [128, 8, 512], BF16)
    yT3 = ar.alloc("yT3", [128, 3, 4, 512], BF16)
    xg = ar.alloc("xgE", [128, 8, 512], F32)
    zT = ar.alloc("zT", [128, 8, 512], BF16)
    sgp = Rot([ar.alloc("sg%d" % i, [128, 512], F32) for i in range(2)])
    tp = Rot([ar.alloc("tE%d" % i, [128, 512], F32) for i in range(2)])
    zap = Rot([ar.alloc("za%d" % i, [128, 512], F32) for i in range(2)])
    sqt = ar.alloc("sqtE", [128, 8, 512], F32)
    h2b = ar.alloc("h2b", [128, 8, 512], BF16)
    rs = ar.alloc("rsE", [128, 512], F32)
    rstd = rs
    sm = {n_: Rot([ar.alloc("r_%s%d" % (n_, i), [128, w_], F32) for i in range(4)]) for n_, w_ in
          (("lg", 36), ("gmax", 1), ("ngmax", 1), ("mg", 4), ("eg", 4), ("sge", 1), ("pg", 1), ("pen", 4), ("lem", 32), ("m1", 1), ("mask1", 32),
           ("lem2", 32), ("m2", 1), ("mask2", 32), ("dd", 1), ("w1", 1), ("w2", 1), ("slot", 32), ("ok", 32), ("pr0", 32), ("pr1", 32), ("df0", 1), ("df1", 1))}
    sm["Ab"] = Rot([ar.alloc("r_Ab%d" % i, [128, 32], BF16) for i in range(4)])
    eoff = ar.alloc("eoff", [128, 64], F32)
    p.dma(SP, eoff[:], k.IN["eoff"], writes=[eoff])
    cnt = ar.alloc("cnt", [128, 32], F32)
    p.op(DVE, lambda e: e.tensor_copy(out=cnt[:], in_=eoff[:, 0:32]), reads=[eoff], writes=[cnt])
    h2tp = Rot(k.tokbuf[0:2])
    pTt_ap = k.ps[6].t[:, 0:512].bitcast(BF16)
    pGp = Rot([k.ps[0], k.ps[1]])
    pBp = Rot([k.ps[2], k.ps[3]])
    pYp = Rot([k.ps[4], k.ps[5]])
    pss = k.ps[6]
    pR = k.ps[7]
    srcs = (k.yglaT, k.yattT, k.ypoolT)

    def E_loads(tok0, W, j):
        p.dma(SP, hTb[:, :, 0:W], k.hT[:, tok0:tok0 + W].rearrange("(c p) n -> p c n", p=128), writes=[hTb])
        for br in range(3):
            p.dma(SP, yT3[:, br, :, 0:W], srcs[br][:, tok0:tok0 + W].rearrange("(c p) n -> p c n", p=128), writes=[(yT3, br)])
        p.dma(SP, xg[:, :, 0:W], k.xT[:, tok0:tok0 + W].rearrange("(c p) n -> p c n", p=128), writes=[xg])

    def E_merge(tok0, W, j):
        for ec in range(8):
            za = zap.next()
            for br in range(3):
                pG, pB = pGp.next(), pBp.next()
                for c in range(8):
                    p.op(PE, lambda e, c=c: e.matmul(pG[:, 0:W], lhsT=wg[:, c, br * 1024 + ec * 128:br * 1024 + (ec + 1) * 128], rhs=hTb[:, c, 0:W], start=(c == 0), stop=(c == 7)),
                         reads=[wg, hTb], writes=[pG])
                for c in range(4):
                    p.op(PE, lambda e, c=c: e.matmul(pB[:, 0:W], lhsT=wb[:, br, c, ec * 128:(ec + 1) * 128], rhs=yT3[:, br, c, 0:W], start=(c == 0), stop=(c == 3)),
                         reads=[wb, (yT3, br)], writes=[pB])
                sg = sgp.next()
                p.op(ACT, lambda e: e.activation(out=sg[:, 0:W], in_=pG[:, 0:W], func=AF.Sigmoid), reads=[pG], writes=[sg])
                if br == 0:
                    p.op(DVE, lambda e: e.tensor_tensor(out=za[:, 0:W], in0=sg[:, 0:W], in1=pB[:, 0:W], op=ALU.mult), reads=[sg, pB], writes=[za])
                else:
                    t_ = tp.next()
                    p.op(DVE, lambda e: e.tensor_tensor(out=t_[:, 0:W], in0=sg[:, 0:W], in1=pB[:, 0:W], op=ALU.mult), reads=[sg, pB], writes=[t_])
                    if br == 1:
                        p.op(POOL, lambda e: e.tensor_tensor(out=za[:, 0:W], in0=za[:, 0:W], in1=t_[:, 0:W], op=ALU.add), reads=[za, t_], writes=[za])
                    else:
                        p.op(POOL, lambda e: e.tensor_tensor(out=zT[:, ec, 0:W], in0=za[:, 0:W], in1=t_[:, 0:W], op=ALU.add), reads=[za, t_], writes=[(zT, ec)])
                yield

    def E_out(tok0, W, j):
        for fc in range(8):
            pY = pYp.next()
            for c in range(8):
                p.op(PE, lambda e, c=c: e.matmul(pY[:, 0:W], lhsT=wo[:, c, fc * 128:(fc + 1) * 128], rhs=zT[:, c, 0:W], start=(c == 0), stop=(c == 7)),
                     reads=[wo, zT], writes=[pY])
            p.op(DVE, lambda e: e.scalar_tensor_tensor(out=xg[:, fc, 0:W], in0=pY[:, 0:W], scalar=k.modT[l][:, 16 + fc, j:j + 1], in1=xg[:, fc, 0:W], op0=ALU.mult, op1=ALU.add),
                 reads=[pY, k.modT[l], xg], writes=[xg])
        p.dma(POOL, k.xT[:, tok0:tok0 + W].rearrange("(c p) n -> p c n", p=128), xg[:, :, 0:W], reads=[xg])
        rms_group(k, xg, W, k.A2[l], k.modT[l], 24, j, hT32=sqt, hTb=h2b, sqt=sqt, pss=pss, tmp=(rs, rstd))
        p.dma(POOL, k.h2T[:, tok0:tok0 + W].rearrange("(c p) n -> p c n", p=128), h2b[:, :, 0:W], reads=[h2b])

    def E_router(tok0, W, j):
        ns = W // 128
        RS = [{n_: r_.tiles[s] for n_, r_ in sm.items()} for s in range(ns)]

        def part1(s):
            R = RS[s]
            ts_ = slice(s * 128, (s + 1) * 128)
            lo = s * 40
            lg = R["lg"]
            for c in range(8):
                p.op(PE, lambda e, c=c: e.matmul(pR[:, lo:lo + 36], lhsT=sqt[:, c, ts_], rhs=wr[:, c, :], start=(c == 0), stop=(c == 7)), reads=[sqt, wr], writes=[pR])
            yield
            p.op(DVE, lambda e: e.tensor_tensor(out=lg[:], in0=pR[:, lo:lo + 36], in1=brt[:], op=ALU.add), reads=[pR, brt], writes=[lg])
            yield
            p.op(DVE, lambda e: e.reduce_max(out=R["gmax"][:], in_=lg[:, 0:4], axis=AXX), reads=[lg], writes=[R["gmax"]])
            yield
            p.op(DVE, lambda e: e.tensor_scalar(out=R["mg"][:], in0=lg[:, 0:4], scalar1=R["gmax"][:, 0:1], scalar2=None, op0=ALU.is_ge), reads=[lg, R["gmax"]], writes=[R["mg"]])
            p.op(DVE, lambda e: e.tensor_scalar(out=R["ngmax"][:], in0=R["gmax"][:], scalar1=-1.0, scalar2=None, op0=ALU.mult), reads=[R["gmax"]], writes=[R["ngmax"]])
            yield
            p.op(ACT, lambda e: e.activation(out=R["eg"][:], in_=lg[:, 0:4], func=AF.Exp, bias=R["ngmax"][:, 0:1], scale=1.0, accum_out=R["sge"][:]),
                 reads=[lg, R["ngmax"]], writes=[R["eg"], R["sge"]])
            p.op(DVE, lambda e: e.tensor_scalar(out=R["pen"][:], in0=R["mg"][:], scalar1=-1.0, scalar2=1e30, op0=ALU.add, op1=ALU.mult), reads=[R["mg"]], writes=[R["pen"]])
            yield
            p.op(DVE, lambda e: e.tensor_tensor(out=R["lem"][:].rearrange("p (g x) -> p g x", g=4), in0=lg[:, 4:36].rearrange("p (g x) -> p g x", g=4),
                                                in1=R["pen"][:].unsqueeze(2).broadcast_to([128, 4, 8]), op=ALU.add), reads=[lg, R["pen"]], writes=[R["lem"]])
            yield
            p.op(DVE, lambda e: e.reduce_max(out=R["m1"][:], in_=R["lem"][:], axis=AXX), reads=[R["lem"]], writes=[R["m1"]])
            yield
            p.op(DVE, lambda e: e.tensor_scalar(out=R["mask1"][:], in0=R["lem"][:], scalar1=R["m1"][:, 0:1], scalar2=None, op0=ALU.is_ge), reads=[R["lem"], R["m1"]], writes=[R["mask1"]])
            yield
            p.op(DVE, lambda e: e.scalar_tensor_tensor(out=R["lem2"][:], in0=R["mask1"][:], scalar=-1e30, in1=R["lem"][:], op0=ALU.mult, op1=ALU.add),
                 reads=[R["mask1"], R["lem"]], writes=[R["lem2"]])
            yield
            p.op(DVE, lambda e: e.reduce_max(out=R["m2"][:], in_=R["lem2"][:], axis=AXX), reads=[R["lem2"]], writes=[R["m2"]])
            yield
            p.op(DVE, lambda e: e.tensor_scalar(out=R["mask2"][:], in0=R["lem2"][:], scalar1=R["m2"][:, 0:1], scalar2=None, op0=ALU.is_ge), reads=[R["lem2"], R["m2"]], writes=[R["mask2"]])
            p.op(DVE, lambda e: e.tensor_tensor(out=R["dd"][:], in0=R["m2"][:], in1=R["m1"][:], op=ALU.subtract), reads=[R["m2"], R["m1"]], writes=[R["dd"]])
            yield
            p.op(ACT, lambda e: e.activation(out=R["dd"][:], in_=R["dd"][:], func=AF.Exp), reads=[R["dd"]], writes=[R["dd"]])
            p.op(DVE, lambda e: e.tensor_tensor(out=R["Ab"][:], in0=R["mask1"][:], in1=R["mask2"][:], op=ALU.add), reads=[R["mask1"], R["mask2"]], writes=[R["Ab"]])
            p.op(DVE, lambda e: e.reciprocal(out=R["pg"][:], in_=R["sge"][:]), reads=[R["sge"]], writes=[R["pg"]])
            yield
            p.op(PE, lambda e: e.matmul(pR[:, 192 + s * 64:224 + s * 64], lhsT=k.triub[:], rhs=R["Ab"][:], start=True, stop=True), reads=[R["Ab"], k.triub], writes=[pR])
            p.op(PE, lambda e: e.matmul(pR[:, 224 + s * 64:256 + s * 64], lhsT=k.onesb[:], rhs=R["Ab"][:], start=True, stop=True), reads=[R["Ab"], k.onesb], writes=[pR])
            p.op(DVE, lambda e: e.tensor_scalar(out=R["dd"][:], in0=R["dd"][:], scalar1=1.0, scalar2=None, op0=ALU.add), reads=[R["dd"]], writes=[R["dd"]])
            yield
            p.op(DVE, lambda e: e.reciprocal(out=R["w1"][:], in_=R["dd"][:]), reads=[R["dd"]], writes=[R["w1"]])
            yield
            p.op(DVE, lambda e: e.tensor_tensor(out=R["w1"][:], in0=R["w1"][:], in1=R["pg"][:], op=ALU.mult), reads=[R["w1"], R["pg"]], writes=[R["w1"]])
            yield
            p.op(DVE, lambda e: e.tensor_tensor(out=R["w2"][:], in0=R["pg"][:], in1=R["w1"][:], op=ALU.subtract), reads=[R["w1"], R["pg"]], writes=[R["w2"]])
            yield
            tt = (tok0 + s * 128) // 128
            p.op(DVE, lambda e: e.tensor_copy(out=k.rw[:, tt, 0:1], in_=R["w1"][:]), reads=[R["w1"]], writes=[(k.rw, (tt, 0))])
            p.op(DVE, lambda e: e.tensor_copy(out=k.rw[:, tt, 1:2], in_=R["w2"][:]), reads=[R["w2"]], writes=[(k.rw, (tt, 1))])
            yield

        def part2(s):
            R = RS[s]
            tt = (tok0 + s * 128) // 128
            sl, ok = R["slot"], R["ok"]
            p.op(DVE, lambda e: e.tensor_tensor(out=ok[:], in0=sl[:], in1=eoff[:, 32:64], op=ALU.is_lt), reads=[sl, eoff], writes=[ok])
            yield
            p.op(DVE, lambda e: e.scalar_tensor_tensor(out=sl[:], in0=ok[:], scalar=-1.0e6, in1=sl[:], op0=ALU.mult, op1=ALU.add), reads=[ok, sl], writes=[sl])
            yield
            p.op(DVE, lambda e: e.tensor_scalar(out=sl[:], in0=sl[:], scalar1=1.0e6, scalar2=None, op0=ALU.add), reads=[sl], writes=[sl])
            yield
            for mi, mname in enumerate(("mask1", "mask2")):
                p.op(DVE, lambda e: e.tensor_tensor(out=R["pr%d" % mi][:], in0=R[mname][:], in1=sl[:], op=ALU.mult), reads=[R[mname], sl], writes=[R["pr%d" % mi]])
            yield
            for mi in range(2):
                p.op(DVE, lambda e: e.reduce_sum(out=R["df%d" % mi][:], in_=R["pr%d" % mi][:], axis=AXX), reads=[R["pr%d" % mi]], writes=[R["df%d" % mi]])
            yield
            for mi in range(2):
                p.op(DVE, lambda e: e.tensor_copy(out=k.ridx[:, tt, mi:mi + 1], in_=R["df%d" % mi][:]), reads=[R["df%d" % mi]], writes=[(k.ridx, (tt, mi))])
            yield


        def rest():
            for s in range(ns):
                R = RS[s]
                p.op(DVE, lambda e: e.tensor_tensor(out=R["slot"][:], in0=pR[:, 192 + s * 64:224 + s * 64], in1=cnt[:], op=ALU.add), reads=[pR, cnt], writes=[R["slot"]])
                p.op(DVE, lambda e: e.tensor_tensor(out=cnt[:], in0=pR[:, 224 + s * 64:256 + s * 64], in1=cnt[:], op=ALU.add), reads=[pR, cnt], writes=[cnt])
            roundrobin(part2(s) for s in range(ns))
            for s in range(ns):
                ts_ = slice(s * 128, (s + 1) * 128)
                tt = (tok0 + s * 128) // 128
                for c in range(8):
                    p.op(PE, lambda e, c=c: e.transpose(out=pTt_ap[:, c * 128:(c + 1) * 128], in_=h2b[:, c, ts_], identity=k.identb[:]), reads=[h2b, k.identb], writes=[pss])
                ht = h2tp.next()
                p.op(ACT, lambda e: e.activation(out=ht[:], in_=pTt_ap[:], func=AF.Copy), reads=[pss], writes=[ht])
                for mi in range(2):
                    p.dma_call(POOL, "indirect_dma_start", dict(out=k.Xg[:, :], out_offset=bass.IndirectOffsetOnAxis(ap=k.ridx[:, tt, mi:mi + 1], axis=0),
                                                                in_=ht[:], in_offset=None, bounds_check='NROWS_REG', oob_is_err=False),
                               reads=[ht, (k.ridx, (tt, mi))])


        return [part1(s) for s in range(ns)], rest

    groups = [g_ for g_ in GROUPS if not (g_[2] == 1 and last)]
    prev = None
    for g_ in groups:
        E_loads(*g_)
        if prev is None:
            roundrobin([E_merge(*g_)])
        else:
            roundrobin([E_merge(*g_)] + prev[0])
            prev[1]()
        E_out(*g_)
        prev = E_router(*g_)
    roundrobin(prev[0])
    prev[1]()


def phase_F_dense(k, l, last):
    p, ar = k.p, k.ar
    groups = [g_ for g_ in GROUPS if not (g_[2] == 1 and last)]
    sgs = [groups[0:4], groups[4:]]
    NS = 2304
    h2s = ar.alloc("h2s", [128, 8, NS], BF16)
    yacc = ar.alloc("yacc", [128, 8, NS], F32)
    wgp = Rot([ar.alloc("mwg%d" % i, [128, 8, 512], BF16) for i in range(2)])
    wup = Rot([ar.alloc("mwu%d" % i, [128, 8, 512], BF16) for i in range(2)])
    wdp = Rot([ar.alloc("mwd%d" % i, [128, 4, 1024], BF16) for i in range(2)])
    wrp = Rot([ar.alloc("wrow%d" % i, [128, 512], F32) for i in range(2)])
    sgp = Rot([ar.alloc("msg%d" % i, [128, 512], F32) for i in range(2)])
    hup = Rot([ar.alloc("mhu%d" % i, [128, 512], F32) for i in range(2)])
    HTp = Rot([ar.alloc("mHT%d" % i, [128, 4, 512], BF16) for i in range(2)])
    xg = ar.alloc("xgF", [128, 8, 512], F32)
    pGp = Rot([k.ps[0], k.ps[1]])
    pUp = Rot([k.ps[2], k.ps[3]])
    pYp = Rot([k.ps[4], k.ps[5], k.ps[6]])
    for sg_groups in sgs:
        base = sg_groups[0][0]
        ntok = sum(g_[1] for g_ in sg_groups)
        p.dma(SP, h2s[:, :, 0:ntok], k.h2T[:, base:base + ntok].rearrange("(c p) n -> p c n", p=128), writes=[h2s])
        for ex in range(NE):
            wg_, wu_, wd_ = wgp.next(), wup.next(), wdp.next()
            p.dma(POOL, wg_[:], k.IN["moe_w_gate"][l, ex].rearrange("(c p) n -> p c n", p=128), writes=[wg_])
            p.dma(POOL, wu_[:], k.IN["moe_w_up"][l, ex].rearrange("(c p) n -> p c n", p=128), writes=[wu_])
            p.dma(POOL, wd_[:], k.IN["moe_w_down"][l, ex].rearrange("(c p) n -> p c n", p=128), writes=[wd_])
            for (tok0, W, j) in sg_groups:
                o0 = tok0 - base
                wrow = wrp.next()
                p.dma(SP, wrow[:, 0:W], k.wdT[ex:ex + 1, tok0:tok0 + W].broadcast_to([128, W]), writes=[wrow])
                HT = HTp.next()
                for dc in range(4):
                    pG, pU = pGp.next(), pUp.next()
                    for c in range(8):
                        p.op(PE, lambda e, c=c: e.matmul(pG[:, 0:W], lhsT=wg_[:, c, dc * 128:(dc + 1) * 128], rhs=h2s[:, c, o0:o0 + W], start=(c == 0), stop=(c == 7)),
                             reads=[wg_, h2s], writes=[pG])
                    for c in range(8):
                        p.op(PE, lambda e, c=c: e.matmul(pU[:, 0:W], lhsT=wu_[:, c, dc * 128:(dc + 1) * 128], rhs=h2s[:, c, o0:o0 + W], start=(c == 0), stop=(c == 7)),
                             reads=[wu_, h2s], writes=[pU])
                    sg, hu = sgp.next(), hup.next()
                    p.op(ACT, lambda e: e.activation(out=sg[:, 0:W], in_=pG[:, 0:W], func=AF.Silu), reads=[pG], writes=[sg])
                    p.op(DVE, lambda e: e.tensor_tensor(out=hu[:, 0:W], in0=sg[:, 0:W], in1=pU[:, 0:W], op=ALU.mult), reads=[sg, pU], writes=[hu])
                    p.op(POOL, lambda e: e.tensor_tensor(out=HT[:, dc, 0:W], in0=hu[:, 0:W], in1=wrow[:, 0:W], op=ALU.mult), reads=[hu, wrow], writes=[(HT, dc)])
                for fc in range(8):
                    pY = pYp.next()
                    for c in range(4):
                        p.op(PE, lambda e, c=c: e.matmul(pY[:, 0:W], lhsT=wd_[:, c, fc * 128:(fc + 1) * 128], rhs=HT[:, c, 0:W], start=(c == 0), stop=(c == 3)),
                             reads=[wd_, HT], writes=[pY])
                    ya = yacc[:, fc, o0:o0 + W]
                    if ex == 0:
                        p.op(ACT, lambda e: e.activation(out=ya, in_=pY[:, 0:W], func=AF.Copy), reads=[pY], writes=[(yacc, (fc, tok0))])
                    else:
                        p.op(DVE, lambda e: e.tensor_tensor(out=ya, in0=ya, in1=pY[:, 0:W], op=ALU.add), reads=[pY, (yacc, (fc, tok0))], writes=[(yacc, (fc, tok0))])
            pass
        for (tok0, W, j) in sg_groups:
            o0 = tok0 - base
            p.dma(SP, xg[:, :, 0:W], k.xT[:, tok0:tok0 + W].rearrange("(c p) n -> p c n", p=128), writes=[xg])
            for fc in range(8):
                p.op(DVE, lambda e: e.scalar_tensor_tensor(out=xg[:, fc, 0:W], in0=yacc[:, fc, o0:o0 + W], scalar=k.modT[l][:, 40 + fc, j:j + 1], in1=xg[:, fc, 0:W],
                                                           op0=ALU.mult, op1=ALU.add), reads=[(yacc, (fc, tok0)), k.modT[l], xg], writes=[xg])
            p.dma(POOL, k.xT[:, tok0:tok0 + W].rearrange("(c p) n -> p c n", p=128), xg[:, :, 0:W], reads=[xg])


def phase_F(k, l, last):
    p, ar = k.p, k.ar
    wgp = Rot([ar.alloc("mwg%d" % i, [128, 8, 512], BF16) for i in range(2)])
    wup = Rot([ar.alloc("mwu%d" % i, [128, 8, 512], BF16) for i in range(2)])
    wdp = Rot([ar.alloc("mwd%d" % i, [128, 4, 1024], BF16) for i in range(2)])
    xbp = Rot([ar.alloc("xblk%d" % i, [128, 4, 1024], BF16) for i in range(3)])
    xtp = Rot([ar.alloc("xTs%d" % i, [128, 8, 512], BF16) for i in range(2)])
    sgp = Rot([ar.alloc("msg%d" % i, [128, 512], F32) for i in range(2)])
    HTp = Rot([ar.alloc("mHT%d" % i, [128, 4, 512], BF16) for i in range(2)])
    ysp = Rot([ar.alloc("yst%d" % i, [128, 4, 1024], BF16) for i in range(2)])
    pGp = Rot([k.ps[0], k.ps[1]])
    pUp = Rot([k.ps[2], k.ps[3]])
    pYp = Rot([k.ps[4], k.ps[5]])
    pTp = Rot([k.ps[6], k.ps[7]])
    blocks = [(b0, min(512, CAP - b0)) for b0 in range(0, CAP, 512)]
    stg = ar.alloc("wstg_g", [128, 8, 512], F32)
    stu = ar.alloc("wstg_u", [128, 8, 512], F32)
    std = ar.alloc("wstg_d", [128, 4, 1024], F32)

    def load_w(ex):
        wg_, wu_, wd_ = wgp.next(), wup.next(), wdp.next()
        p.dma(SP, stg[:], k.IN["moe_w_gate"][l, ex].rearrange("(c p) n -> p c n", p=128), writes=[stg])
        p.dma(SP, stu[:], k.IN["moe_w_up"][l, ex].rearrange("(c p) n -> p c n", p=128), writes=[stu])
        p.dma(SP, std[:], k.IN["moe_w_down"][l, ex].rearrange("(c p) n -> p c n", p=128), writes=[std])
        for c in range(8):
            p.op(POOL, lambda e, c=c: e.tensor_copy(out=wg_[:, c, :], in_=stg[:, c, :]), reads=[stg], writes=[(wg_, c)])
            p.op(POOL, lambda e, c=c: e.tensor_copy(out=wu_[:, c, :], in_=stu[:, c, :]), reads=[stu], writes=[(wu_, c)])
        for c in range(4):
            p.op(POOL, lambda e, c=c: e.tensor_copy(out=wd_[:, c, :], in_=std[:, c, :]), reads=[std], writes=[(wd_, c)])
        return wg_, wu_, wd_

    W_ = {}
    XB = {}

    def start_expert(ex):
        for bi, (b0, WB) in enumerate(blocks):
            xb = xbp.next()
            p.dma(SP, xb[:, 0:WB // 128, :], k.Xg[ex * CAP + b0:ex * CAP + b0 + WB, :].rearrange("(s p) n -> p s n", p=128), writes=[xb])
            XB[(ex, bi)] = xb

    def T_(ex, bi):
        b0, WB = blocks[bi]
        xb = XB.pop((ex, bi))
        xT = xtp.next()
        for sub in range(WB // 128):
            pT = pTp.next()
            pT_ap = pT.t[:, 0:512].bitcast(BF16)
            for c in range(8):
                p.op(PE, lambda e, c=c: e.transpose(out=pT_ap[:, c * 128:(c + 1) * 128], in_=xb[:, sub, c * 128:(c + 1) * 128], identity=k.identb[:]),
                     reads=[xb, k.identb], writes=[pT])
            if sub % 2 == 0:
                p.op(ACT, lambda e: e.activation(out=xT[:, :, sub * 128:(sub + 1) * 128], in_=pT_ap[:].rearrange("p (c n) -> p c n", c=8), func=AF.Copy),
                     reads=[pT], writes=[(xT, sub)])
            else:
                p.op(DVE, lambda e: e.tensor_copy(out=xT[:, :, sub * 128:(sub + 1) * 128], in_=pT_ap[:].rearrange("p (c n) -> p c n", c=8)),
                     reads=[pT], writes=[(xT, sub)])
        return xT

    def GU_(ex, bi, xT):
        b0, WB = blocks[bi]
        wg_, wu_, wd_ = W_[ex]
        HT = HTp.next()
        for dc in range(4):
            pG, pU = pGp.next(), pUp.next()
            for c in range(8):
                p.op(PE, lambda e, c=c: e.matmul(pG[:, 0:WB], lhsT=wg_[:, c, dc * 128:(dc + 1) * 128], rhs=xT[:, c, 0:WB], start=(c == 0), stop=(c == 7)),
                     reads=[wg_, xT], writes=[pG])
            for c in range(8):
                p.op(PE, lambda e, c=c: e.matmul(pU[:, 0:WB], lhsT=wu_[:, c, dc * 128:(dc + 1) * 128], rhs=xT[:, c, 0:WB], start=(c == 0), stop=(c == 7)),
                     reads=[wu_, xT], writes=[pU])
            sg = sgp.next()
            p.op(ACT, lambda e: e.activation(out=sg[:, 0:WB], in_=pG[:, 0:WB], func=AF.Silu), reads=[pG], writes=[sg])
            p.op(DVE, lambda e: e.tensor_tensor(out=HT[:, dc, 0:WB], in0=sg[:, 0:WB], in1=pU[:, 0:WB], op=ALU.mult), reads=[sg, pU], writes=[(HT, dc)])
        return HT

    def DN_(ex, bi, HT):
        b0, WB = blocks[bi]
        nsub = WB // 128
        r0 = ex * CAP + b0
        wg_, wu_, wd_ = W_[ex]
        ys = ysp.next()
        for sub in range(nsub):
            for half in range(2):
                pY = pYp.next()
                for c in range(4):
                    p.op(PE, lambda e, c=c: e.matmul(pY[:, 0:512], lhsT=HT[:, c, sub * 128:(sub + 1) * 128], rhs=wd_[:, c, half * 512:(half + 1) * 512], start=(c == 0), stop=(c == 3)),
                         reads=[HT, wd_], writes=[pY])
                if half == 0:
                    p.op(ACT, lambda e: e.activation(out=ys[:, sub, 0:512], in_=pY[:, 0:512], func=AF.Copy), reads=[pY], writes=[(ys, (sub, 0))])
                else:
                    p.op(DVE, lambda e: e.tensor_copy(out=ys[:, sub, 512:1024], in_=pY[:, 0:512]), reads=[pY], writes=[(ys, (sub, 1))])
        p.dma(ACT, k.Yg[r0:r0 + WB, :].rearrange("(s p) n -> p s n", p=128), ys[:, 0:nsub, :], reads=[ys])

    seq = [(ex, bi) for ex in range(NE) for bi in range(len(blocks))]
    W_[0] = load_w(0)
    start_expert(0)
    W_[1] = load_w(1)
    xT_cur = T_(*seq[0])
    for i_, (ex, bi) in enumerate(seq):
        HT = GU_(ex, bi, xT_cur)
        if i_ + 1 < len(seq):
            nex, nbi = seq[i_ + 1]
            if nbi == 0:
                start_expert(nex)
            xT_cur = T_(nex, nbi)
        DN_(ex, bi, HT)
        if i_ + 1 < len(seq) and seq[i_ + 1][1] == 0 and seq[i_ + 1][0] + 1 < NE:
            W_[seq[i_ + 1][0] + 1] = load_w(seq[i_ + 1][0] + 1)
    p.barrier()
    ar.reset()
    y1p = Rot(k.tokbuf[0:2])
    y2p = Rot(k.tokbuf[2:4])
    mp = Rot([ar.alloc("mtok%d" % i, [128, 1024], F32) for i in range(2)])
    xgp = Rot([ar.alloc("xgF%d" % i, [128, 8, 512], F32) for i in range(2)])
    pb = Rot([(k.ps[0], k.ps[1]), (k.ps[2], k.ps[3]), (k.ps[4], k.ps[5])])
    if last:
        fing = ar.alloc("fing", [128, 8, 1], F32)
        p.dma(SP, fing[:].rearrange("p a b -> p (a b)"), k.IN["fing_c"], writes=[fing])
        zb = ar.alloc("zb", [128, 8, 1], F32)
        p.op(DVE, lambda e: e.memset(zb[:], 0.0), writes=[zb])
        sqtZ = ar.alloc("sqtZ", [128, 8, 512], F32)
        rsZ = ar.alloc("rsZ", [128, 512], F32)
        ostp = Rot([ar.alloc("ost%d" % i, [128, 1024], F32) for i in range(2)])
    groups = [g_ for g_ in GROUPS if not (g_[2] == 1 and last)]
    for (tok0, W, j) in groups:
        xg = xgp.next()
        p.dma(SP, xg[:, :, 0:W], k.xT[:, tok0:tok0 + W].rearrange("(c p) n -> p c n", p=128), writes=[xg])
        for s in range(W // 128):
            tt = (tok0 + s * 128) // 128
            ys_ = []
            for mi, yp in enumerate((y1p, y2p)):
                y_ = yp.next()
                p.op(ACT, lambda e: e.activation(out=y_[:], in_=k.zerob[:], func=AF.Copy), reads=[k.zerob], writes=[y_])
                p.dma_call(POOL, "indirect_dma_start", dict(out=y_[:], out_offset=None, in_=k.Yg[:, :],
                                                            in_offset=bass.IndirectOffsetOnAxis(ap=k.ridx[:, tt, mi:mi + 1], axis=0),
                                                            bounds_check='NROWS_REG', oob_is_err=False), reads=[(k.ridx, (tt, mi))], writes=[y_])
                ys_.append(y_)
            m = mp.next()
            p.op(ACT, lambda e: e.activation(out=m[:], in_=ys_[0][:], func=AF.Copy, scale=k.rw[:, tt, 0:1]), reads=[ys_[0], (k.rw, (tt, 0))], writes=[m])
            p.op(DVE, lambda e: e.scalar_tensor_tensor(out=m[:], in0=ys_[1][:], scalar=k.rw[:, tt, 1:2], in1=m[:], op0=ALU.mult, op1=ALU.add),
                 reads=[ys_[1], (k.rw, (tt, 1)), m], writes=[m])
            pa, pbk = pb.next()
            for c in range(8):
                pt = pa if c < 4 else pbk
                p.op(PE, lambda e, c=c: e.transpose(out=pt[:, (c % 4) * 128:(c % 4 + 1) * 128], in_=m[:, c * 128:(c + 1) * 128], identity=k.ident[:]),
                     reads=[m, k.ident], writes=[pt])
            for c in range(8):
                pt = pa if c < 4 else pbk
                p.op(DVE, lambda e, c=c: e.scalar_tensor_tensor(out=xg[:, c, s * 128:(s + 1) * 128], in0=pt[:, (c % 4) * 128:(c % 4 + 1) * 128], scalar=k.modT[l][:, 40 + c, j:j + 1],
                                                               in1=xg[:, c, s * 128:(s + 1) * 128], op0=ALU.mult, op1=ALU.add),
                     reads=[pt, k.modT[l], (xg, (c, s))], writes=[(xg, (c, s))])
        if not last:
            p.dma(SP, k.xT[:, tok0:tok0 + W].rearrange("(c p) n -> p c n", p=128), xg[:, :, 0:W], reads=[xg])
            continue
        rms_group(k, xg, W, fing, zb, 0, 0, hT32=sqtZ, sqt=sqtZ, pss=k.ps[6], tmp=(rsZ, rsZ))
        for s in range(W // 128):
            pa, pbk = pb.next()
            ost = ostp.next()
            for c in range(8):
                pt = pa if c < 4 else pbk
                p.op(PE, lambda e, c=c: e.transpose(out=pt[:, (c % 4) * 128:(c % 4 + 1) * 128], in_=sqtZ[:, c, s * 128:(s + 1) * 128], identity=k.ident[:]),
                     reads=[sqtZ, k.ident], writes=[pt])
            p.op(ACT, lambda e: e.activation(out=ost[:, 0:512], in_=pa[:], func=AF.Copy), reads=[pa], writes=[(ost, 0)])
            p.op(DVE, lambda e: e.tensor_copy(out=ost[:, 512:1024], in_=pbk[:]), reads=[pbk], writes=[(ost, 1)])
            p.dma(SP, k.out[tok0 + s * 128:tok0 + (s + 1) * 128, :], ost[:], reads=[ost], is_output=True)


def phase_Z(k):
    p, ar = k.p, k.ar
    fing = ar.alloc("fing", [128, 8, 1], F32)
    p.dma(SP, fing[:].rearrange("p a b -> p (a b)"), k.IN["fing_c"], writes=[fing])
    zb = ar.alloc("zb", [128, 8, 1], F32)
    p.op(DVE, lambda e: e.memset(zb[:], 0.0), writes=[zb])
    xgp = Rot([ar.alloc("xgZ%d" % i, [128, 8, 512], F32) for i in range(2)])
    sqt = ar.alloc("sqtZ", [128, 8, 512], F32)
    rs = ar.alloc("rsZ", [128, 512], F32)
    rstd = ar.alloc("rstdZ", [128, 512], F32)
    ostp = Rot([ar.alloc("ost%d" % i, [128, 1024], F32) for i in range(2)])
    pss = k.ps[0]
    pb = Rot([(k.ps[1], k.ps[2]), (k.ps[3], k.ps[4]), (k.ps[5], k.ps[6])])
    for (tok0, W, j) in GROUPS[0:8]:
        xg = xgp.next()
        p.dma(SP, xg[:, :, 0:W], k.xT[:, tok0:tok0 + W].rearrange("(c p) n -> p c n", p=128), writes=[xg])
        rms_group(k, xg, W, fing, zb, 0, 0, hT32=sqt, sqt=sqt, pss=pss, tmp=(rs, rstd))
        for s in range(W // 128):
            pa, pbk = pb.next()
            ost = ostp.next()
            for c in range(8):
                pt = pa if c < 4 else pbk
                p.op(PE, lambda e, c=c: e.transpose(out=pt[:, (c % 4) * 128:(c % 4 + 1) * 128], in_=sqt[:, c, s * 128:(s + 1) * 128], identity=k.ident[:]),
                     reads=[sqt, k.ident], writes=[(pt, c % 4)])
            p.op(ACT, lambda e: e.activation(out=ost[:, 0:512], in_=pa[:], func=AF.Copy), reads=[pa], writes=[(ost, 0)])
            p.op(DVE, lambda e: e.tensor_copy(out=ost[:, 512:1024], in_=pbk[:]), reads=[pbk], writes=[(ost, 1)])
            p.dma(POOL, k.out[tok0 + s * 128:tok0 + (s + 1) * 128, :], ost[:], reads=[ost], is_output=True)
```

```python
import os
import numpy as np
import concourse.bass as bass
import concourse.mybir as mybir
from concourse.bass_utils import run_bass_kernel_spmd
from concourse.alu_op_type import AluOpType as ALU
from contextlib import ExitStack

F32 = mybir.dt.float32
BF16 = mybir.dt.bfloat16
AF = mybir.ActivationFunctionType

PE, ACT, DVE, POOL, SP = "tensor", "scalar", "vector", "gpsimd", "sync"
ENGINES = [PE, ACT, DVE, POOL, SP]
DMAQ = (SP, ACT, POOL)
NSLOT = 8

D = 1024
T = 4096
C = 256
NT = T + C
NTILE = NT // 128
DEPTH = 2
INW = 5920
NE = 32
DE = 512
EPS = 1e-6
CAP = 1280
NROWS = NE * CAP
I32 = mybir.dt.int32
GROUPS = [(g * 512, 512, 0) for g in range(8)] + [(4096, 256, 1)]


class Tile:
    def __init__(self, t, name, psum=False):
        self.t = t
        self.name = name
        self.parts = {}
        self.psum = psum

    def __getitem__(self, idx):
        return self.t[idx]


class _Rec:
    def __init__(self):
        self.call = None

    def __getattr__(self, name):
        def f(*a, **kw):
            self.call = (name, a, kw)
            return self
        return f


class Prog:
    def __init__(self, nc):
        self.nc = nc
        self.es = ExitStack()
        self.ops = {e: [] for e in ENGINES}
        self.cnt = {e: 0 for e in ENGINES}
        self.dma_cnt = {e: 0 for e in ENGINES}
        self.known = {e: {} for e in ENGINES}
        self.sem = {}
        self.dsem = {}
        for e in ENGINES:
            self.sem[e] = self.es.enter_context(nc.semaphore("s_" + e))
        for e in DMAQ:
            self.dsem[e] = [self.es.enter_context(nc.semaphore("d_%s%d" % (e, i))) for i in range(NSLOT)]
        self.latest = {}
        self.out_events = []

    def sbuf(self, name, shape, dtype):
        return Tile(self.es.enter_context(self.nc.sbuf_tensor(name, list(shape), dtype)), name)

    def psum(self, name, shape, dtype=F32):
        return Tile(self.es.enter_context(self.nc.psum_tensor(name, list(shape), dtype)), name, psum=True)

    @staticmethod
    def _norm(acc):
        out = []
        for a in acc:
            if a is None:
                continue
            if isinstance(a, Tile):
                out.append((a, "*"))
            elif a[0].psum:
                out.append((a[0], "*"))
            else:
                out.append((a[0], a[1]))
        return out

    def _deps(self, eng, reads, writes):
        deps = []
        for (t, pt) in reads:
            states = list(t.parts.values()) if pt == "*" else [t.parts.get(pt), t.parts.get("*")]
            for s in states:
                if s is not None and s[0] is not None:
                    deps.append(s[0])
        for (t, pt) in writes:
            states = list(t.parts.values()) if pt == "*" else [t.parts.get(pt), t.parts.get("*")]
            for s in states:
                if s is not None:
                    if s[0] is not None:
                        deps.append(s[0])
                    deps.extend(s[1].values())
        need = {}
        for (src, val) in deps:
            if src == PE and eng == PE:
                continue
            if self.known[eng].get(src, 0) >= val:
                continue
            if need.get(src, 0) < val:
                need[src] = val
        for src, val in need.items():
            self.known[eng][src] = val
        return list(need.items())

    def _commit(self, ev, reads, writes):
        self.latest[ev[0]] = ev[1]
        for (t, pt) in reads:
            s = t.parts.setdefault(pt, [None, {}])
            s[1][ev[0]] = ev
        for (t, pt) in writes:
            if pt == "*":
                t.parts = {"*": [ev, {}]}
            else:
                t.parts[pt] = [ev, {}]

    def op(self, eng, fn, reads=(), writes=()):
        reads = self._norm(reads)
        writes = self._norm(writes)
        waits = self._deps(eng, reads, writes)
        self.cnt[eng] += 1
        ev = (eng, self.cnt[eng])
        r = _Rec()
        fn(r)
        self.ops[eng].append((r.call, waits, None))
        self._commit(ev, reads, writes)

    def dma(self, q, out, in_, reads=(), writes=(), is_output=False, **kw):
        reads = self._norm(reads)
        writes = self._norm(writes)
        j = self.dma_cnt[q]
        self.dma_cnt[q] += 1
        slot = j % NSLOT
        val = 16 * (j // NSLOT + 1)
        src = (q, slot)
        waits = self._deps(q, reads, writes)
        if val > 16 and self.known[q].get(src, 0) < val - 16:
            waits.append((src, val - 16))
            self.known[q][src] = val - 16
        ev = (src, val)

        self.ops[q].append((("dma_start", (), dict(out=out, in_=in_, **kw)), waits, slot))
        self._commit(ev, reads, writes)
        if is_output:
            self.out_events.append(ev)
        return ev

    def dma_call(self, q, method, kw, reads=(), writes=()):
        reads = self._norm(reads)
        writes = self._norm(writes)
        j = self.dma_cnt[q]
        self.dma_cnt[q] += 1
        slot = j % NSLOT
        val = 16 * (j // NSLOT + 1)
        src = (q, slot)
        waits = self._deps(q, reads, writes)
        if val > 16 and self.known[q].get(src, 0) < val - 16:
            waits.append((src, val - 16))
            self.known[q][src] = val - 16
        ev = (src, val)
        self.ops[q].append(((method, (), kw), waits, slot))
        self._commit(ev, reads, writes)
        return ev

    def barrier(self):
        for eng in ENGINES:
            waits = []
            for src, val in self.latest.items():
                if src == eng:
                    continue
                if self.known[eng].get(src, 0) < val:
                    waits.append((src, val))
                    self.known[eng][src] = val
            if waits:
                self.ops[eng].append((None, waits, None))

    def _semof(self, src):
        if isinstance(src, tuple):
            return self.dsem[src[0]][src[1]]
        return self.sem[src]

    def emit(self):
        nc = self.nc
        fin = []
        for src, val in self.latest.items():
            if self.known[SP].get(src, 0) < val and src != SP:
                fin.append((src, val))
        self.ops[SP].append((None, fin, None))
        with nc.Block() as block:
            for eng in ENGINES:
                def body(e, ops=self.ops[eng], eng=eng):
                    breg = None
                    for (fn, waits, slot) in ops:
                        for (src, val) in waits:
                            e.wait_ge(self._semof(src), val)
                        if fn is None:
                            continue
                        if fn[2].get('bounds_check') == 'NROWS_REG':
                            if breg is None:
                                breg = e.alloc_register()
                                e.reg_mov(breg, NROWS - 1)
                            fn = (fn[0], fn[1], dict(fn[2], bounds_check=breg))
                        try:
                            inst = getattr(e, fn[0])(*fn[1], **fn[2])
                        except Exception:
                            import traceback
                            traceback.print_exc()
                            for k_, v_ in fn[2].items():
                                if hasattr(v_, 'ap') and hasattr(v_, 'shape'):
                                    print('  AP', k_, v_.shape, v_.offset, v_.ap, v_.dtype, v_.tensor.name if hasattr(v_, 'tensor') else None)
                            print('EMIT FAIL', eng, fn[0], {k_: (getattr(v_, 'shape', v_), getattr(v_, 'dtype', None)) for k_, v_ in fn[2].items()})
                            raise
                        if slot is not None:
                            inst.then_inc(self.dsem[eng][slot], 16)
                        else:
                            inst.then_inc(self.sem[eng], 1)
                getattr(block, eng)(body)
        self.es.close()


class Arena:
    def __init__(self, p, nfloat):
        self.p = p
        self.base = p.sbuf("arena", [128, nfloat], F32)
        self.n = nfloat
        self.n0 = nfloat
        self.off = 0

    def reset(self):
        self.off = 0

    def reset_top(self):
        self.n = self.n0

    def alloc(self, name, shape, dtype, top=False):
        free = int(np.prod(shape[1:]))
        nf = free if dtype == F32 else (free + 1) // 2
        nf = (nf + 7) // 8 * 8
        assert self.off + nf <= self.n, "arena overflow %s %d+%d>%d" % (name, self.off, nf, self.n)
        if top:
            self.n -= nf
            v = self.base.t[0:shape[0], self.n:self.n + nf]
        else:
            v = self.base.t[0:shape[0], self.off:self.off + nf]
            self.off += nf
        if dtype != F32:
            v = v.bitcast(dtype)
        v = v[:, 0:free]
        if len(shape) == 3:
            v = v.rearrange("p (a b) -> p a b", a=shape[1])
        elif len(shape) == 4:
            v = v.rearrange("p (a b c) -> p a b c", a=shape[1], b=shape[2])
        return Tile(v, name)


class Rot:
    def __init__(self, tiles):
        self.tiles = tiles
        self.i = 0

    def next(self):
        t = self.tiles[self.i % len(self.tiles)]
        self.i += 1
        return t


def _consts():
    c = {}
    c["ident"] = np.eye(128, dtype=np.float32)
    c["ones"] = np.ones((128, 128), np.float32)
    blk = np.zeros((128, 128), np.float32)
    blk[:64, :64] = 1
    blk[64:, 64:] = 1
    c["blk64"] = blk
    rm = np.zeros((64, 64), np.float32)
    for a in range(2):
        for f in range(16):
            rm[a * 32 + f, a * 32 + 16 + f] = -1.0
            rm[a * 32 + 16 + f, a * 32 + f] = 1.0
    rot = np.zeros((128, 128), np.float32)
    rot[:64, :64] = rm.T
    rot[64:, 64:] = rm.T
    c["rotT"] = rot
    inv = (np.float32(10000.0) ** (-np.arange(0, 32, 2, dtype=np.float32) / np.float32(32))).astype(np.float32)
    t = np.arange(T)
    row = (t // 64).astype(np.float32)
    col = (t % 64).astype(np.float32)
    cos = np.ones((128, NT), np.float32)
    sin = np.zeros((128, NT), np.float32)
    for pp in range(128):
        d = pp % 64
        a = d // 32
        f = d % 16
        ang = ((row if a == 0 else col) * inv[f]).astype(np.float32)
        cos[pp, :T] = np.cos(ang)
        sin[pp, :T] = np.sin(ang)
    c["cos"] = cos
    c["sin"] = sin
    s = np.arange(128)[:, None]
    tt = np.arange(128)[None, :]
    same = (s // 64) == (tt // 64)
    c["ti_f"] = (same & (s <= tt)).astype(np.float32)
    c["ti_b"] = (same & (s >= tt)).astype(np.float32)
    c["te_f"] = (same & (s > tt)).astype(np.float32)
    c["te_b"] = (same & (s < tt)).astype(np.float32)
    band = np.zeros((4, 5, 128, 128), np.float32)
    wins = (2, 4, 8, 16)
    nseq = 3 * 128
    for gi, w in enumerate(wins):
        tg = np.arange(nseq)
        lo = np.clip(tg - w // 2, 0, nseq)
        hi = np.clip(tg + w // 2, 0, nseq)
        M = np.zeros((nseq, nseq), np.float32)
        for t_ in range(nseq):
            M[lo[t_]:hi[t_], t_] = np.float32(1.0) / np.float32(hi[t_] - lo[t_])
            M[t_, t_] -= 1.0
        band[gi, 0] = M[0:128, 0:128]
        band[gi, 1] = M[128:256, 128:256]
        band[gi, 2] = M[256:384, 256:384]
        band[gi, 3] = M[0:128, 128:256]
        band[gi, 4] = M[256:384, 128:256]
    c["band"] = band.transpose(2, 0, 1, 3).reshape(128, 20 * 128).copy()
    c["triu"] = (s < tt).astype(np.float32)
    eo = np.zeros((128, 64), np.float32)
    eo[:, 0:32] = np.arange(NE, dtype=np.float32)[None, :] * CAP
    eo[:, 32:64] = (np.arange(NE, dtype=np.float32)[None, :] + 1) * CAP
    c["eoff"] = eo
    return c


CONST_SHAPES = {"ident": [128, 128], "ones": [128, 128], "blk64": [128, 128], "rotT": [128, 128],
                "cos": [128, NT], "sin": [128, NT], "ti_f": [128, 128], "ti_b": [128, 128],
                "te_f": [128, 128], "te_b": [128, 128], "band": [128, 2560], "triu": [128, 128], "eoff": [128, 64]}


def _col(v, nch):
    return np.ascontiguousarray(np.asarray(v, np.float32).reshape(nch, 128).T)


def _prep_weights(inp):
    w = {}
    L = DEPTH
    w["w_mod"] = np.ascontiguousarray(inp["w_mod"], np.float32)
    w["w_in"] = np.ascontiguousarray(inp["w_in"], np.float32)
    w["w_branch"] = np.ascontiguousarray(inp["w_branch"], np.float32)
    w["w_out"] = np.ascontiguousarray(inp["w_out"], np.float32)
    w["moe_w_gate"] = np.ascontiguousarray(inp["moe_w_gate"], np.float32)
    w["moe_w_up"] = np.ascontiguousarray(inp["moe_w_up"], np.float32)
    w["moe_w_down"] = np.ascontiguousarray(inp["moe_w_down"], np.float32)
    w["pool_w"] = np.ascontiguousarray(inp["pool_w"], np.float32)
    w["b_mod_c"] = np.stack([_col(inp["b_mod"][l], 48) for l in range(L)])
    w["n1g_c"] = np.stack([_col(inp["norm1_g"][l], 8) for l in range(L)])
    w["n2g_c"] = np.stack([_col(inp["norm2_g"][l], 8) for l in range(L)])
    w["fing_c"] = _col(inp["final_g"], 8)
    w["psc_c"] = np.stack([_col(inp["pool_scale"][l], 4) for l in range(L)])
    up = np.zeros((L, 33, 512), np.float32)
    up[:, 0:16, 0:256] = inp["gla_a_up_f"]
    up[:, 16:32, 256:512] = inp["gla_a_up_b"]
    up[:, 32, 0:256] = inp["gla_a_bias_f"]
    up[:, 32, 256:512] = inp["gla_a_bias_b"]
    w["upcat"] = up
    qk = np.zeros((L, 128, 2), np.float32)
    qk[:, :, 0] = np.tile(np.asarray(inp["att_qn_g"], np.float32), (1, 2))
    qk[:, :, 1] = np.tile(np.asarray(inp["att_kn_g"], np.float32), (1, 2))
    w["qkg_c"] = qk
    w["glag_b"] = np.ascontiguousarray(np.broadcast_to(np.tile(np.asarray(inp["gla_norm_g"], np.float32), (1, 4))[:, None, :], (L, 128, 512)))
    w["w_rt"] = np.ascontiguousarray(np.concatenate([inp["moe_w_group"], inp["moe_w_expert"]], axis=2), np.float32)
    brt = np.concatenate([inp["moe_b_group"], inp["moe_b_expert"]], axis=1).astype(np.float32)
    w["b_rt"] = np.ascontiguousarray(np.broadcast_to(brt[:, None, :], (L, 128, 36)))
    return w


WEIGHT_SHAPES = {"w_mod": [2, 1024, 6144], "w_in": [2, 1024, INW], "w_branch": [2, 3, 512, 1024], "w_out": [2, 1024, 1024],
                 "moe_w_gate": [2, 32, 1024, 512], "moe_w_up": [2, 32, 1024, 512], "moe_w_down": [2, 32, 512, 1024],
                 "pool_w": [2, 4, 128, 128], "b_mod_c": [2, 128, 48], "n1g_c": [2, 128, 8], "n2g_c": [2, 128, 8],
                 "fing_c": [128, 8], "psc_c": [2, 128, 4], "upcat": [2, 33, 512], "qkg_c": [2, 128, 2],
                 "glag_b": [2, 128, 512], "w_rt": [2, 1024, 36], "b_rt": [2, 128, 36]}


def roundrobin(gens):
    gens = list(gens)
    while gens:
        nxt_ = []
        for g_ in gens:
            try:
                next(g_)
                nxt_.append(g_)
            except StopIteration:
                pass
        gens = nxt_


class Ctx:
    pass


def build_program(stop_after=None, dbg_outs=(), layers=DEPTH):
    nc = bass.Bass("TRN2", target_bir_lowering=False)
    p = Prog(nc)
    k = Ctx()
    k.nc, k.p = nc, p
    k.IN = {}

    def din(name, shape):
        k.IN[name] = nc.dram_tensor(name, list(shape), F32, kind="ExternalInput").ap()

    din("xin", [NT, D])
    din("cvec", [128, 8, 2])
    for n_, s_ in CONST_SHAPES.items():
        din(n_, s_)
    for n_, s_ in WEIGHT_SHAPES.items():
        din(n_, s_)
    k.out = nc.dram_tensor("out", [T, D], F32, kind="ExternalOutput").ap()

    def scratch(name, shape, dt):
        kind = "ExternalOutput" if name in dbg_outs else "Internal"
        return nc.dram_tensor(name, list(shape), dt, kind=kind).ap()

    k.xT = scratch("xT", [D, NT], F32)
    k.hT = scratch("hT", [D, NT], BF16)
    k.gqkT = scratch("gqkT", [512, NT], BF16)
    k.gkvr = scratch("gkvr", [NT, 1280], BF16)
    k.la = scratch("la", [NT, 512], F32)
    k.qaT = scratch("qaT", [512, NT], BF16)
    k.kaT = scratch("kaT", [128, NT], BF16)
    k.vaug = scratch("vaug", [NT, 130], BF16)
    k.pu = scratch("pu", [NT, 512], BF16)
    k.of = scratch("of", [NT, 512], F32)
    k.yglaT = scratch("yglaT", [512, NT], BF16)
    k.yattT = scratch("yattT", [512, NT], BF16)
    k.ypoolT = scratch("ypoolT", [512, NT], BF16)
    k.h2T = scratch("h2T", [D, NT], BF16)
    k.wdT = scratch("wdT", [128, NT], F32)
    k.Xg = scratch("Xg", [NROWS, D], BF16)
    k.Yg = scratch("Yg", [NROWS, D], BF16)
    k.dbg = {n_: scratch(n_, s_, F32) for n_, s_ in (("dbg_mod", [128, 96]),) if n_ in dbg_outs}

    k.psw = [p.psum("psw%d" % i, [128, 1024], F32) for i in range(4)]
    k.ps = []
    for i, w_ in enumerate(k.psw):
        k.ps.append(Tile(w_.t[:, 0:512], "ps%d" % (2 * i), psum=True))
        k.ps.append(Tile(w_.t[:, 512:1024], "ps%d" % (2 * i + 1), psum=True))
    k.ident = p.sbuf("c_ident", [128, 128], F32)
    k.identb = p.sbuf("c_identb", [128, 128], BF16)
    k.ones = p.sbuf("c_ones", [128, 128], F32)
    k.blk64 = p.sbuf("c_blk64", [128, 128], F32)
    k.rotTb = p.sbuf("c_rotTb", [128, 128], BF16)
    k.tri = {n_: p.sbuf("c_" + n_, [128, 128], F32) for n_ in ("ti_f", "ti_b", "te_f", "te_b")}
    k.maskb = {n_: p.sbuf("c_m" + n_, [128, 128], BF16) for n_ in ("ti_f", "ti_b")}
    k.cv = p.sbuf("cv", [128, 8, 2], F32)
    k.scv = p.sbuf("scv", [128, 8, 2], F32)
    k.modT = [p.sbuf("modT%d" % l, [128, 48, 2], F32) for l in range(DEPTH)]
    k.A1 = [p.sbuf("A1_%d" % l, [128, 8, 2], F32) for l in range(DEPTH)]
    k.A2 = [p.sbuf("A2_%d" % l, [128, 8, 2], F32) for l in range(DEPTH)]
    k.ngc = p.sbuf("ngc", [128, 8], F32)
    k.bmc = p.sbuf("bmc", [128, 48], F32)
    k.ridx = p.sbuf("ridx", [128, NTILE, 2], I32)
    k.rw = p.sbuf("rw", [128, NTILE, 2], F32)
    k.triub = p.sbuf("c_triub", [128, 128], BF16)
    k.onesb = p.sbuf("c_onesb", [128, 128], BF16)
    k.tokbuf = [p.sbuf("tokbuf%d" % i, [128, 1024], BF16) for i in range(4)]
    k.ar = Arena(p, 46000)

    for n_, t_ in (("ident", k.ident), ("ones", k.ones), ("blk64", k.blk64)):
        p.dma(SP, t_[:], k.IN[n_], writes=[t_])
    for n_ in k.tri:
        p.dma(SP, k.tri[n_][:], k.IN[n_], writes=[k.tri[n_]])
    p.dma(POOL, k.rotTb[:], k.IN["rotT"], writes=[k.rotTb])
    p.dma(POOL, k.identb[:], k.IN["ident"], writes=[k.identb])
    p.dma(POOL, k.triub[:], k.IN["triu"], writes=[k.triub])
    p.dma(POOL, k.onesb[:], k.IN["ones"], writes=[k.onesb])
    for n_ in k.maskb:
        p.dma(POOL, k.maskb[n_][:], k.IN[n_], writes=[k.maskb[n_]])
    p.dma(SP, k.cv[:], k.IN["cvec"], writes=[k.cv])
    p.op(ACT, lambda e: e.activation(out=k.scv[:], in_=k.cv[:], func=AF.Silu), reads=[k.cv], writes=[k.scv])

    phases = []
    for l in range(layers):
        phases.append(("M%d" % l, lambda l=l: phase_M(k, l)))
    for l in range(layers):
        last = (l == DEPTH - 1)
        phases.append(("A%d" % l, lambda l=l: phase_A(k, l)))
        phases.append(("B%d" % l, lambda l=l, last=last: phase_B(k, l, last)))
        phases.append(("C%d" % l, lambda l=l, last=last: phase_C(k, l, last)))
        phases.append(("D%d" % l, lambda l=l, last=last: phase_D(k, l, last)))
        phases.append(("E%d" % l, lambda l=l, last=last: phase_E(k, l, last)))
        phases.append(("F%d" % l, lambda l=l, last=last: phase_F(k, l, last)))
    only = os.environ.get('PHASES')
    for name, fn in phases:
        if only and name not in only.split(','):
            continue
        p.barrier()
        k.ar.reset()
        if name[0] == 'F':
            k.ar.reset_top()
        if name[0] == 'C':
            prefetch_E(k, int(name[1]))
            k.Ew_l = int(name[1])
        if name[0] == 'E' and getattr(k, 'Ew_l', -1) != int(name[1]):
            prefetch_E(k, int(name[1]))
        fn()
        if stop_after == name:
            break
    p.emit()
    return nc


def phase_M(k, l):
    p, ar = k.p, k.ar
    wpool = Rot([ar.alloc("wm%d" % i, [128, 8, 512], F32) for i in range(2)])
    p.dma(SP, k.bmc[:], k.IN["b_mod_c"][l], writes=[k.bmc])
    pm = k.ps[0]
    for fg in range(12):
        wst = wpool.next()
        p.dma(SP if fg % 2 == 0 else ACT, wst[:], k.IN["w_mod"][l, :, fg * 512:(fg + 1) * 512].rearrange("(c p) n -> p c n", p=128), writes=[wst])
        for fc in range(4):
            for c in range(8):
                p.op(PE, lambda e, fc=fc, c=c, wst=wst: e.matmul(pm[:, fc * 2:fc * 2 + 2], lhsT=wst[:, c, fc * 128:(fc + 1) * 128],
                                                               rhs=k.scv[:, c, :], start=(c == 0), stop=(c == 7)),
                     reads=[wst, k.scv], writes=[pm])
        p.op(DVE, lambda e, fg=fg: e.tensor_tensor(out=k.modT[l][:, fg * 4:(fg + 1) * 4, :],
                                                  in0=pm[:, 0:8].rearrange("p (a b) -> p a b", a=4),
                                                  in1=k.bmc[:, fg * 4:(fg + 1) * 4].unsqueeze(2).broadcast_to([128, 4, 2]), op=ALU.add),
             reads=[pm, k.bmc], writes=[(k.modT[l], fg)])
    for (A, gname, c0) in ((k.A1[l], "n1g_c", 8), (k.A2[l], "n2g_c", 32)):
        p.dma(SP, k.ngc[:], k.IN[gname][l], writes=[k.ngc])
        p.op(DVE, lambda e, A=A, c0=c0: e.tensor_scalar(out=A[:], in0=k.modT[l][:, c0:c0 + 8, :], scalar1=1.0, scalar2=None, op0=ALU.add),
             reads=[k.modT[l]], writes=[A])
        p.op(DVE, lambda e, A=A: e.tensor_tensor(out=A[:], in0=A[:], in1=k.ngc[:].unsqueeze(2).broadcast_to([128, 8, 2]), op=ALU.mult),
             reads=[A, k.ngc], writes=[A])
    if "dbg_mod" in k.dbg:
        p.dma(POOL, k.dbg["dbg_mod"], k.modT[l][:].rearrange("p a b -> p (a b)"), reads=[k.modT[l]])


def phase_T0(k):
    p, ar = k.p, k.ar
    xin = k.IN["xin"]
    xl = Rot([ar.alloc("xl%d" % i, [128, 1024], F32) for i in range(3)])
    st = Rot([ar.alloc("xst%d" % i, [128, 8, 512], F32) for i in range(2)])
    pb = Rot([(k.ps[0], k.ps[1]), (k.ps[2], k.ps[3])])
    for (tok0, W, j) in GROUPS:
        stg = st.next()
        for s in range(W // 128):
            xt = xl.next()
            p.dma(SP, xt[:], xin[tok0 + s * 128: tok0 + (s + 1) * 128, :], writes=[xt])
            pa, pbk = pb.next()
            for c in range(8):
                pt = pa if c < 4 else pbk
                p.op(PE, lambda e, c=c, xt=xt, pt=pt: e.transpose(out=pt[:, (c % 4) * 128:(c % 4 + 1) * 128], in_=xt[:, c * 128:(c + 1) * 128], identity=k.ident[:]),
                     reads=[xt, k.ident], writes=[(pt, c % 4)])
            p.op(ACT, lambda e, s=s, stg=stg, pa=pa: e.activation(out=stg[:, 0:4, s * 128:(s + 1) * 128], in_=pa[:].rearrange("p (c n) -> p c n", c=4), func=AF.Copy),
                 reads=[pa], writes=[(stg, (s, 0))])
            p.op(DVE, lambda e, s=s, stg=stg, pbk=pbk: e.tensor_copy(out=stg[:, 4:8, s * 128:(s + 1) * 128], in_=pbk[:].rearrange("p (c n) -> p c n", c=4)),
                 reads=[pbk], writes=[(stg, (s, 1))])
        p.dma(POOL, k.xT[:, tok0:tok0 + W].rearrange("(c p) n -> p c n", p=128), stg[:, :, 0:W], reads=[stg])


def rms_group(k, xg, W, A, B, boff, j, hT32=None, hTb=None, sqt=None, pss=None, tmp=None):
    p = k.p
    p.op(ACT, lambda e: e.activation(out=sqt[:, :, 0:W], in_=xg[:, :, 0:W], func=AF.Square), reads=[xg], writes=[sqt])
    for c in range(8):
        p.op(PE, lambda e, c=c: e.matmul(pss[:, 0:W], lhsT=k.ones[:], rhs=sqt[:, c, 0:W], start=(c == 0), stop=(c == 7)),
             reads=[sqt, k.ones], writes=[pss])
    rs, rstd = tmp
    p.op(ACT, lambda e: e.activation(out=rs[:, 0:W], in_=pss[:, 0:W], func=AF.Sqrt, bias=EPS, scale=1.0 / D), reads=[pss], writes=[rs])
    p.op(DVE, lambda e: e.reciprocal(out=rstd[:, 0:W], in_=rs[:, 0:W]), reads=[rs], writes=[rstd])
    p.op(DVE, lambda e: e.tensor_tensor(out=sqt[:, :, 0:W], in0=xg[:, :, 0:W], in1=rstd[:, 0:W].unsqueeze(1).broadcast_to([128, 8, W]), op=ALU.mult),
         reads=[xg, rstd], writes=[sqt])
    for c in range(8):
        if hT32 is not None:
            p.op(ACT, lambda e, c=c: e.activation(out=hT32[:, c, 0:W], in_=sqt[:, c, 0:W], func=AF.Identity, scale=A[:, c, j:j + 1], bias=B[:, boff + c, j:j + 1]),
                 reads=[sqt, A, B], writes=[(hT32, c)])
            if hTb is not None:
                p.op(POOL, lambda e, c=c: e.tensor_copy(out=hTb[:, c, 0:W], in_=hT32[:, c, 0:W]), reads=[(hT32, c)], writes=[(hTb, c)])
        else:
            p.op(ACT, lambda e, c=c: e.activation(out=hTb[:, c, 0:W], in_=sqt[:, c, 0:W], func=AF.Identity, scale=A[:, c, j:j + 1], bias=B[:, boff + c, j:j + 1]),
                 reads=[sqt, A, B], writes=[(hTb, c)])


def phase_A(k, l):
    p, ar = k.p, k.ar
    w_in = k.IN["w_in"]
    NCOL = 2848
    wA = ar.alloc("wA", [128, 8, NCOL], BF16)
    for c0 in range(0, NCOL, 512):
        c1 = min(NCOL, c0 + 512)
        p.dma(POOL, wA[:, :, c0:c1], w_in[l, :, c0:c1].rearrange("(c p) n -> p c n", p=128), writes=[(wA, c0)])
    upc = ar.alloc("upc", [33, 512], F32)
    p.dma(SP, upc[:], k.IN["upcat"][l], writes=[upc])
    qkg = ar.alloc("qkg", [128, 2], F32)
    p.dma(SP, qkg[:], k.IN["qkg_c"][l], writes=[qkg])
    xgp = Rot([ar.alloc("xg%d" % i, [128, 8, 512], F32) for i in range(2)])
    sqt = ar.alloc("sqt", [128, 8, 512], F32)
    hTp = Rot([ar.alloc("hTb%d" % i, [128, 8, 512], BF16) for i in range(2)])
    rs = ar.alloc("rs", [128, 512], F32)
    rstd = ar.alloc("rstd", [128, 512], F32)
    nb_ = 1 if l == 0 else 2
    if l == 0:
        xlp = Rot([ar.alloc("xl%d" % i, [128, 1024], F32) for i in range(2)])
    cosp = Rot([ar.alloc("cos%d" % i, [128, 512], F32) for i in range(nb_)])
    sinp = Rot([ar.alloc("sin%d" % i, [128, 512], F32) for i in range(nb_)])
    stq = Rot([ar.alloc("stq%d" % i, [128, 4, 512], BF16) for i in range(2)])
    sta = Rot([ar.alloc("sta%d" % i, [128, 5, 512], BF16) for i in range(2)])
    glT = Rot([ar.alloc("glT%d" % i, [33, 512], F32) for i in range(2)])
    for g_ in glT.tiles:
        p.op(DVE, lambda e, g_=g_: e.memset(g_[:], 1.0), writes=[g_])
    sqh = Rot([ar.alloc("sqh%d" % i, [128, 512], F32) for i in range(2)])
    qg = Rot([ar.alloc("qg%d" % i, [128, 512], BF16) for i in range(2)])
    rsh = Rot([ar.alloc("rsh%d" % i, [128, 512], F32) for i in range(2)])
    t1 = Rot([ar.alloc("t1_%d" % i, [128, 512], F32) for i in range(nb_)])
    t2 = Rot([ar.alloc("t2_%d" % i, [128, 512], F32) for i in range(nb_)])
    stkvr = Rot([ar.alloc("stkvr%d" % i, [128, 1280], BF16) for i in range(2)])
    stva = Rot([ar.alloc("stva%d" % i, [128, 2, 65], BF16) for i in range(2)])
    for v_ in stva.tiles:
        p.op(DVE, lambda e, v_=v_: e.memset(v_[:], 1.0), writes=[v_])
    stpu = Rot([ar.alloc("stpu%d" % i, [128, 512], BF16) for i in range(2)])
    e1p = Rot([ar.alloc("e1_%d" % i, [128, 512], F32) for i in range(2)])
    stla = Rot([ar.alloc("stla%d" % i, [128, 512], F32) for i in range(2)])
    pss = k.ps[0]
    pfm = Rot([k.ps[1], k.ps[2]])
    pssh, prq = k.ps[3], k.ps[4]
    ptm = Rot([k.ps[5], k.ps[6], k.ps[7]])

    def fm_proj(col0, M, hTb, W):
        ps_ = pfm.next()
        for c in range(8):
            p.op(PE, lambda e, c=c: e.matmul(ps_[0:M, 0:W], lhsT=wA[:, c, col0:col0 + M], rhs=hTb[:, c, 0:W], start=(c == 0), stop=(c == 7)),
                 reads=[wA, hTb], writes=[ps_])
        return ps_

    def tm_proj(col0, N, hTb, s):
        ps_ = ptm.next()
        for c in range(8):
            p.op(PE, lambda e, c=c: e.matmul(ps_[:, 0:N], lhsT=hTb[:, c, s * 128:(s + 1) * 128], rhs=wA[:, c, col0:col0 + N], start=(c == 0), stop=(c == 7)),
                 reads=[wA, hTb], writes=[ps_])
        return ps_

    def A_prep(tok0, W, j):
        xg = xgp.next()
        if l == 0:
            for s in range(W // 128):
                xt = xlp.next()
                p.dma(SP, xt[:], k.IN["xin"][tok0 + s * 128:tok0 + (s + 1) * 128, :], writes=[xt])
                for half in range(2):
                    pt_ = ptm.next()
                    for c4 in range(4):
                        c = half * 4 + c4
                        p.op(PE, lambda e: e.transpose(out=pt_[:, c4 * 128:(c4 + 1) * 128], in_=xt[:, c * 128:(c + 1) * 128], identity=k.ident[:]),
                             reads=[xt, k.ident], writes=[pt_])
                    if half == 0:
                        p.op(ACT, lambda e: e.activation(out=xg[:, 0:4, s * 128:(s + 1) * 128], in_=pt_[:].rearrange("p (c n) -> p c n", c=4), func=AF.Copy),
                             reads=[pt_], writes=[(xg, (s, 0))])
                    else:
                        p.op(DVE, lambda e: e.tensor_copy(out=xg[:, 4:8, s * 128:(s + 1) * 128], in_=pt_[:].rearrange("p (c n) -> p c n", c=4)),
                             reads=[pt_], writes=[(xg, (s, 1))])
            p.dma(POOL, k.xT[:, tok0:tok0 + W].rearrange("(c p) n -> p c n", p=128), xg[:, :, 0:W], reads=[xg])
        else:
            p.dma(SP, xg[:, :, 0:W], k.xT[:, tok0:tok0 + W].rearrange("(c p) n -> p c n", p=128), writes=[xg])
        hTb = hTp.next()
        rms_group(k, xg, W, k.A1[l], k.modT[l], 0, j, hTb=hTb, sqt=sqt, pss=pss, tmp=(rs, rstd))
        p.dma(POOL, k.hT[:, tok0:tok0 + W].rearrange("(c p) n -> p c n", p=128), hTb[:, :, 0:W], reads=[hTb])
        return hTb

    def A_body(tok0, W, j, hTb):
        cs, sn = cosp.next(), sinp.next()
        p.dma(SP, cs[:, 0:W], k.IN["cos"][:, tok0:tok0 + W], writes=[cs])
        p.dma(SP, sn[:, 0:W], k.IN["sin"][:, tok0:tok0 + W], writes=[sn])
        sq_ = stq.next()
        for i in range(4):
            ps_ = fm_proj(i * 128, 128, hTb, W)
            if i < 2:
                p.op(ACT, lambda e, i=i, ps_=ps_: e.activation(out=sq_[:, i, 0:W], in_=ps_[:, 0:W], func=AF.Copy, scale=0.125), reads=[ps_], writes=[(sq_, i)])
            else:
                p.op(DVE, lambda e, i=i, ps_=ps_: e.tensor_copy(out=sq_[:, i, 0:W], in_=ps_[:, 0:W]), reads=[ps_], writes=[(sq_, i)])
        p.dma(POOL, k.gqkT[:, tok0:tok0 + W].rearrange("(c p) n -> p c n", p=128), sq_[:, :, 0:W], reads=[sq_])
        gl_ = glT.next()
        ps_ = fm_proj(1536, 32, hTb, W)
        p.op(DVE, lambda e, ps_=ps_: e.tensor_copy(out=gl_[0:32, 0:W], in_=ps_[0:32, 0:W]), reads=[ps_], writes=[(gl_, 0)])
        sa_ = sta.next()
        for i in range(5):
            ps_ = fm_proj(1568 + i * 128, 128, hTb, W)
            gcol = qkg[:, 0:1] if i < 4 else qkg[:, 1:2]
            sqh_, qg_, rsh_, t1_, t2_ = sqh.next(), qg.next(), rsh.next(), t1.next(), t2.next()
            p.op(ACT, lambda e, ps_=ps_, sqh_=sqh_: e.activation(out=sqh_[:, 0:W], in_=ps_[:, 0:W], func=AF.Square), reads=[ps_], writes=[sqh_])
            p.op(ACT, lambda e, ps_=ps_, qg_=qg_, gcol=gcol: e.activation(out=qg_[:, 0:W], in_=ps_[:, 0:W], func=AF.Copy, scale=gcol), reads=[ps_, qkg], writes=[qg_])
            p.op(PE, lambda e, sqh_=sqh_: e.matmul(pssh[:, 0:W], lhsT=k.blk64[:], rhs=sqh_[:, 0:W], start=True, stop=True), reads=[sqh_, k.blk64], writes=[pssh])
            p.op(PE, lambda e, qg_=qg_: e.matmul(prq[:, 0:W], lhsT=k.rotTb[:], rhs=qg_[:, 0:W], start=True, stop=True), reads=[qg_, k.rotTb], writes=[prq])
            p.op(ACT, lambda e, rsh_=rsh_: e.activation(out=rsh_[:, 0:W], in_=pssh[:, 0:W], func=AF.Sqrt, bias=EPS, scale=1.0 / 64), reads=[pssh], writes=[rsh_])
            p.op(DVE, lambda e, rsh_=rsh_: e.reciprocal(out=rsh_[:, 0:W], in_=rsh_[:, 0:W]), reads=[rsh_], writes=[rsh_])
            p.op(DVE, lambda e, t2_=t2_: e.tensor_tensor(out=t2_[:, 0:W], in0=prq[:, 0:W], in1=sn[:, 0:W], op=ALU.mult), reads=[prq, sn], writes=[t2_])
            p.op(POOL, lambda e, t1_=t1_, qg_=qg_: e.tensor_tensor(out=t1_[:, 0:W], in0=qg_[:, 0:W], in1=cs[:, 0:W], op=ALU.mult), reads=[qg_, cs], writes=[t1_])
            p.op(POOL, lambda e, t1_=t1_, t2_=t2_: e.tensor_tensor(out=t1_[:, 0:W], in0=t1_[:, 0:W], in1=t2_[:, 0:W], op=ALU.add), reads=[t1_, t2_], writes=[t1_])
            p.op(DVE, lambda e, i=i, t1_=t1_, rsh_=rsh_: e.tensor_tensor(out=sa_[:, i, 0:W], in0=t1_[:, 0:W], in1=rsh_[:, 0:W], op=ALU.mult), reads=[t1_, rsh_], writes=[(sa_, i)])
        p.dma(POOL, k.qaT[:, tok0:tok0 + W].rearrange("(c p) n -> p c n", p=128), sa_[:, 0:4, 0:W], reads=[sa_])
        p.dma(POOL, k.kaT[:, tok0:tok0 + W], sa_[:, 4, 0:W], reads=[sa_])
        for s in range(W // 128):
            r0 = tok0 + s * 128
            sk = stkvr.next()
            psa = tm_proj(256, 512, hTb, s)
            p.op(ACT, lambda e, psa=psa, sk=sk: e.activation(out=sk[:, 0:512], in_=psa[:, 0:512], func=AF.Copy), reads=[psa], writes=[(sk, 0)])
            psb = tm_proj(768, 512, hTb, s)
            p.op(DVE, lambda e, psb=psb, sk=sk: e.tensor_copy(out=sk[:, 512:1024], in_=psb[:, 0:512]), reads=[psb], writes=[(sk, 1)])
            psc = tm_proj(1280, 256, hTb, s)
            p.op(ACT, lambda e, psc=psc, sk=sk: e.activation(out=sk[:, 1024:1280], in_=psc[:, 0:256], func=AF.Copy), reads=[psc], writes=[(sk, 2)])
            p.dma(POOL, k.gkvr[r0:r0 + 128, :], sk[:], reads=[sk])
            sv = stva.next()
            psd = tm_proj(2208, 128, hTb, s)
            p.op(DVE, lambda e, psd=psd, sv=sv: e.tensor_copy(out=sv[:, :, 0:64], in_=psd[:, 0:128].rearrange("p (g d) -> p g d", g=2)), reads=[psd], writes=[(sv, 0)])
            p.dma(POOL, k.vaug[r0:r0 + 128, :], sv[:].rearrange("p g d -> p (g d)"), reads=[sv])
            su = stpu.next()
            pse = tm_proj(2336, 512, hTb, s)
            p.op(ACT, lambda e, pse=pse, su=su: e.activation(out=su[:], in_=pse[:, 0:512], func=AF.Copy), reads=[pse], writes=[su])
            p.dma(POOL, k.pu[r0:r0 + 128, :], su[:], reads=[su])
            psl = ptm.next()
            p.op(PE, lambda e, s=s, psl=psl: e.matmul(psl[:, 0:512], lhsT=gl_[0:33, s * 128:(s + 1) * 128], rhs=upc[0:33, :], start=True, stop=True),
                 reads=[gl_, upc], writes=[psl])
            e1, sl = e1p.next(), stla.next()
            p.op(ACT, lambda e, psl=psl, e1=e1: e.activation(out=e1[:], in_=psl[:, 0:512], func=AF.Exp, scale=-1.0), reads=[psl], writes=[e1])
            p.op(ACT, lambda e, e1=e1: e.activation(out=e1[:], in_=e1[:], func=AF.Ln, bias=1.0, scale=1.0), reads=[e1], writes=[e1])
            p.op(DVE, lambda e, e1=e1, sl=sl: e.tensor_scalar(out=sl[:], in0=e1[:], scalar1=-1.0 / 16, scalar2=None, op0=ALU.mult), reads=[e1], writes=[sl])
            p.dma(POOL, k.la[r0:r0 + 128, :], sl[:], reads=[sl])

    cur = A_prep(*GROUPS[0])
    for gi, g_ in enumerate(GROUPS):
        nxt_h = A_prep(*GROUPS[gi + 1]) if gi + 1 < len(GROUPS) else None
        A_body(*g_, cur)
        cur = nxt_h


def make_in_maps(inputs, cores=range(8)):
    consts = _consts()
    w = _prep_weights(inputs)
    x = np.asarray(inputs["x"], np.float32)
    ctx = np.asarray(inputs["ctx"], np.float32)
    c = np.asarray(inputs["c"], np.float32)
    cc = np.asarray(inputs["c_ctx"], np.float32)
    maps = []
    for b in cores:
        m = {}
        m["xin"] = np.ascontiguousarray(np.concatenate([x[b], ctx[b]], axis=0))
        cv = np.stack([_col(c[b], 8), _col(cc, 8)], axis=2)
        m["cvec"] = np.ascontiguousarray(cv)
        m.update(consts)
        m.update(w)
        maps.append(m)
    return maps


_NC_CACHE = {}


def kernel(**inputs):
    if "nc" not in _NC_CACHE:
        _NC_CACHE["nc"] = build_program()
    nc = _NC_CACHE["nc"]
    maps = make_in_maps(inputs)
    res = run_bass_kernel_spmd(nc, maps, core_ids=list(range(8)))
    return np.stack([np.asarray(r["out"], np.float32) for r in res.results], axis=0)


def phase_B(k, l, last):
    p, ar = k.p, k.ar
    AXX = mybir.AxisListType.X
    glag = ar.alloc("glag", [128, 512], F32)
    p.dma(SP, glag[:], k.IN["glag_b"][l], writes=[glag])
    lap = Rot([ar.alloc("laT%d" % i, [128, 256], F32) for i in range(2)])
    qkp = Rot([ar.alloc("qk%d" % i, [128, 4, 128], BF16) for i in range(2)])
    kvp = Rot([ar.alloc("kvr%d" % i, [128, 1280], BF16) for i in range(2)])
    ofp = Rot([ar.alloc("ofl%d" % i, [128, 512], F32) for i in range(2)])
    Ep = Rot([ar.alloc("E%d" % i, [128, 2, 128], F32) for i in range(2)])
    Eip = Rot([ar.alloc("Ei%d" % i, [128, 2, 128], F32) for i in range(2)])
    Qbp = Rot([ar.alloc("Qb%d" % i, [128, 2, 128], BF16) for i in range(2)])
    Kip = Rot([ar.alloc("Ki%d" % i, [128, 2, 128], BF16) for i in range(2)])
    EXp = Rot([ar.alloc("EX%d" % i, [128, 256], F32) for i in range(2)])
    Klp = Rot([ar.alloc("Kl%d" % i, [128, 256], BF16) for i in range(2)])
    ATp = Rot([ar.alloc("AT%d" % i, [128, 4, 128], BF16) for i in range(2)])
    S32 = [ar.alloc("S32_%d" % h, [128, 128], F32) for h in range(4)]
    Sbp = [Rot([ar.alloc("Sb%d_%d" % (h, i), [128, 128], BF16) for i in range(2)]) for h in range(4)]
    ofst = Rot([ar.alloc("ofst%d" % i, [128, 512], F32) for i in range(2)])
    osum = Rot([ar.alloc("osum%d" % i, [128, 512], F32) for i in range(2)])
    sqo = ar.alloc("sqo", [128, 512], F32)
    ssg = Rot([ar.alloc("ssg%d" % i, [128, 4], F32) for i in range(2)])
    srp = Rot([ar.alloc("sr%d" % i, [128, 512], F32) for i in range(2)])
    ytk = Rot([ar.alloc("ytk%d" % i, [128, 512], BF16) for i in range(2)])
    ystp = Rot([ar.alloc("ystB%d" % i, [128, 4, 512], BF16) for i in range(2)])
    pbT = k.ps[0]
    pA = k.ps[1]
    pAo = k.ps[6]
    pOh = [k.ps[2], k.ps[3], k.ps[4], k.ps[5]]
    pDp = Rot([k.ps[7]])
    pT_ap = k.ps[1].t[:, 0:256].bitcast(BF16)

    for dirn in (0, 1):
        if dirn == 1:
            p.barrier()
        tri_i = k.tri["ti_f" if dirn == 0 else "ti_b"]
        tri_e = k.tri["te_f" if dirn == 0 else "te_b"]
        msk = k.maskb["ti_f" if dirn == 0 else "ti_b"]
        order = [32, 33] + list(range(32)) if dirn == 0 else [33, 32] + list(range(31, -1, -1))
        jorder = (0, 1) if dirn == 0 else (1, 0)
        Sb = []
        for h in range(4):
            p.op(POOL, lambda e, h=h: e.memset(S32[h][:], 0.0), writes=[S32[h]])
            sb = Sbp[h].next()
            p.op(POOL, lambda e, sb=sb: e.memset(sb[:], 0.0), writes=[sb])
            Sb.append(sb)
        yst = None
        for tt in order:
            rows = slice(tt * 128, (tt + 1) * 128)
            is_ctx = tt >= 32
            want_o = not (is_ctx and last)
            laT, qk, kvr = lap.next(), qkp.next(), kvp.next()
            p.dma(SP, laT[:], k.la[rows, dirn * 256:(dirn + 1) * 256], writes=[laT])
            p.dma(SP, qk[:], k.gqkT[:, rows].rearrange("(c p) n -> p c n", p=128), writes=[qk])
            p.dma(SP, kvr[:], k.gkvr[rows, :], writes=[kvr])
            if dirn == 1 and want_o:
                ofl = ofp.next()
                p.dma(SP, ofl[:], k.of[rows, :], writes=[ofl])
            for hp in range(2):
                p.op(PE, lambda e, hp=hp: e.matmul(pbT[:, hp * 128:(hp + 1) * 128], lhsT=laT[:, hp * 128:(hp + 1) * 128], rhs=tri_i[:], start=True, stop=True),
                     reads=[laT, tri_i], writes=[(pbT, hp)])
            p.op(PE, lambda e: e.matmul(pbT[:, 256:512], lhsT=tri_e[:], rhs=laT[:], start=True, stop=True), reads=[laT, tri_e], writes=[pbT])
            E, Ei, Qb, Ki = Ep.next(), Eip.next(), Qbp.next(), Kip.next()
            p.op(ACT, lambda e: e.activation(out=E[:].rearrange("p a b -> p (a b)"), in_=pbT[:, 0:256], func=AF.Exp), reads=[pbT], writes=[E])
            p.op(ACT, lambda e: e.activation(out=Ei[:].rearrange("p a b -> p (a b)"), in_=pbT[:, 0:256], func=AF.Exp, scale=-1.0), reads=[pbT], writes=[Ei])
            p.op(DVE, lambda e: e.tensor_tensor(out=Qb[:], in0=qk[:, 0:2, :], in1=E[:], op=ALU.mult), reads=[qk, E], writes=[Qb])
            p.op(POOL, lambda e: e.tensor_tensor(out=Ki[:], in0=qk[:, 2:4, :], in1=Ei[:], op=ALU.mult), reads=[qk, Ei], writes=[Ki])
            EX, Kl = EXp.next(), Klp.next()
            p.op(ACT, lambda e: e.activation(out=EX[:], in_=pbT[:, 256:512], func=AF.Exp), reads=[pbT], writes=[EX])
            p.op(DVE, lambda e: e.tensor_tensor(out=Kl[:], in0=kvr[:, 0:256], in1=EX[:], op=ALU.mult), reads=[kvr, EX], writes=[Kl])
            if want_o:
                if dirn == 0:
                    ofs = ofst.next()
                else:
                    osm = osum.next()
            HR = [(h // 2, (h % 2) * 64) for h in range(4)]
            if want_o:
                for h in range(4):
                    hp, r0 = HR[h]
                    pAh = pA if r0 == 0 else pAo
                    p.op(PE, lambda e: e.matmul(pAh[:, h * 128:(h + 1) * 128], lhsT=Ki[r0:r0 + 64, hp, :], rhs=Qb[r0:r0 + 64, hp, :], start=True, stop=True),
                         reads=[Ki, Qb], writes=[pAh])
                AT4 = ATp.next()
                for h in range(4):
                    pAh = pA if h % 2 == 0 else pAo
                    p.op(DVE, lambda e: e.tensor_tensor(out=AT4[:, h, :], in0=pAh[:, h * 128:(h + 1) * 128], in1=msk[:], op=ALU.mult), reads=[pAh, msk], writes=[(AT4, h)])
                for h in range(4):
                    p.op(PE, lambda e: e.matmul(pOh[h][:, 0:128], lhsT=AT4[:, h, :], rhs=kvr[:, 256 + h * 128:256 + (h + 1) * 128], start=True, stop=False),
                         reads=[AT4, kvr], writes=[pOh[h]])
            for j in jorder:
                js = slice(j * 64, (j + 1) * 64)
                pD = pDp.next()
                col = j * 64 + 63 if dirn == 0 else j * 64
                for h in range(4):
                    hp, r0 = HR[h]
                    if want_o:
                        p.op(PE, lambda e: e.matmul(pOh[h][js, 0:128], lhsT=Qb[r0:r0 + 64, hp, js], rhs=Sb[h][r0:r0 + 64, :], start=False, stop=True),
                             reads=[Qb, Sb[h]], writes=[pOh[h]])
                    p.op(PE, lambda e: e.matmul(pD[r0:r0 + 64, h * 128:(h + 1) * 128], lhsT=Kl[js, h * 64:(h + 1) * 64], rhs=kvr[js, 256 + h * 128:256 + (h + 1) * 128], start=True, stop=True),
                         reads=[Kl, kvr], writes=[pD])
                for h in range(4):
                    hp, r0 = HR[h]
                    p.op(DVE, lambda e: e.scalar_tensor_tensor(out=S32[h][r0:r0 + 64, :], in0=S32[h][r0:r0 + 64, :], scalar=E[r0:r0 + 64, hp, col:col + 1],
                                                               in1=pD[r0:r0 + 64, h * 128:(h + 1) * 128], op0=ALU.mult, op1=ALU.add),
                         reads=[S32[h], E, pD], writes=[S32[h]])
                for h in range(4):
                    hp, r0 = HR[h]
                    sb = Sbp[h].next()
                    p.op(ACT, lambda e: e.activation(out=sb[r0:r0 + 64, :], in_=S32[h][r0:r0 + 64, :], func=AF.Copy), reads=[S32[h]], writes=[sb])
                    Sb[h] = sb
            if want_o:
                for h in range(4):
                    hs = slice(h * 128, (h + 1) * 128)
                    if dirn == 0:
                        p.op(ACT, lambda e: e.activation(out=ofs[:, hs], in_=pOh[h][:, 0:128], func=AF.Copy), reads=[pOh[h]], writes=[(ofs, h)])
                    else:
                        p.op(DVE, lambda e: e.tensor_tensor(out=osm[:, hs], in0=pOh[h][:, 0:128], in1=ofl[:, hs], op=ALU.add), reads=[pOh[h], ofl], writes=[(osm, h)])
            if not want_o:
                continue
            if dirn == 0:
                p.dma(POOL, k.of[rows, :], ofs[:], reads=[ofs])
                continue
            ss, sr, yt = ssg.next(), srp.next(), ytk.next()
            p.op(POOL, lambda e: e.tensor_tensor(out=sqo[:], in0=osm[:], in1=osm[:], op=ALU.mult), reads=[osm], writes=[sqo])
            p.op(DVE, lambda e: e.reduce_sum(out=ss[:], in_=sqo[:].rearrange("p (h d) -> p h d", h=4), axis=AXX), reads=[sqo], writes=[ss])
            p.op(ACT, lambda e: e.activation(out=ss[:], in_=ss[:], func=AF.Sqrt, bias=EPS, scale=1.0 / 128), reads=[ss], writes=[ss])
            p.op(DVE, lambda e: e.reciprocal(out=ss[:], in_=ss[:]), reads=[ss], writes=[ss])
            p.op(DVE, lambda e: e.tensor_tensor(out=osm[:].rearrange("p (h d) -> p h d", h=4), in0=osm[:].rearrange("p (h d) -> p h d", h=4),
                                                in1=ss[:].unsqueeze(2).broadcast_to([128, 4, 128]), op=ALU.mult), reads=[osm, ss], writes=[osm])
            p.op(POOL, lambda e: e.tensor_tensor(out=osm[:], in0=osm[:], in1=glag[:], op=ALU.mult), reads=[osm, glag], writes=[osm])
            p.op(ACT, lambda e: e.activation(out=sr[:], in_=kvr[:, 768:1280], func=AF.Silu), reads=[kvr], writes=[sr])
            p.op(DVE, lambda e: e.tensor_tensor(out=yt[:], in0=osm[:], in1=sr[:], op=ALU.mult), reads=[osm, sr], writes=[yt])
            grp0 = (tt // 4) * 4
            gsz = 4 if tt < 32 else 2
            pos = tt - grp0
            if yst is None:
                yst = ystp.next()
            for c in range(4):
                p.op(PE, lambda e, c=c: e.transpose(out=pT_ap[:, c * 128:(c + 1) * 128], in_=yt[:, c * 128:(c + 1) * 128], identity=k.identb[:]),
                     reads=[yt, k.identb], writes=[pA])
            p.op(ACT, lambda e: e.activation(out=yst[:, :, pos * 128:(pos + 1) * 128], in_=pT_ap[:].rearrange("p (c n) -> p c n", c=4), func=AF.Copy),
                 reads=[pA], writes=[(yst, pos)])
            done = (pos == 0) if dirn == 1 else (pos == gsz - 1)
            if done:
                p.dma(POOL, k.yglaT[:, grp0 * 128:(grp0 + gsz) * 128].rearrange("(c p) n -> p c n", p=128), yst[:, :, 0:gsz * 128], reads=[yst])
                yst = None


def phase_C(k, l, last):
    p, ar = k.p, k.ar
    kd = ar.alloc("kd", [128, 2, NT], BF16)
    for g in range(2):
        for half in range(2):
            p.dma(SP, kd[half * 64:(half + 1) * 64, g, :], k.kaT[g * 64:(g + 1) * 64, :], writes=[(kd, (g, half))])
    va = ar.alloc("va", [128, NTILE, 130], BF16)
    p.dma(SP, va[:], k.vaug.rearrange("(t p) n -> p t n", p=128), writes=[va])
    qap = Rot([ar.alloc("qa%d" % i, [128, 4, 512], BF16) for i in range(2)])
    ptp = Rot([ar.alloc("pt%d" % i, [128, 1024], BF16) for i in range(3)])
    ytp = Rot([ar.alloc("ytC%d" % i, [128, 4, 512], BF16) for i in range(2)])
    ystp = Rot([ar.alloc("ystC%d" % i, [128, 4, 512], BF16) for i in range(2)])
    recp = Rot([ar.alloc("rec%d" % i, [128, 1], F32) for i in range(4)])
    pSw = Rot([0, 1])
    pO = [k.ps[4], k.ps[5], k.ps[6], k.ps[7]]
    pT_ap = k.psw[1].t[:, 0:256].bitcast(BF16)
    pT_t = k.ps[2]
    for (tok0, W, j) in GROUPS:
        if j == 1 and last:
            continue
        qa = qap.next()
        p.dma(SP, qa[:, :, 0:W], k.qaT[:, tok0:tok0 + W].rearrange("(c p) n -> p c n", p=128), writes=[qa])
        kts = list(range(NTILE)) if j == 0 else [32, 33]
        pairs = [(kts[i], kts[i + 1]) for i in range(0, len(kts), 2)]
        ns = W // 128
        yt = ytp.next()
        pend = []

        def qk(h, pr):
            hp, r0, g = h // 2, (h % 2) * 64, h // 4
            wi = pSw.next()
            banks = (k.ps[2 * wi], k.ps[2 * wi + 1])
            for half, kt in enumerate(pr):
                p.op(PE, lambda e: e.matmul(banks[half][:, 0:W], lhsT=kd[r0:r0 + 64, g, kt * 128:(kt + 1) * 128], rhs=qa[r0:r0 + 64, hp, 0:W], start=True, stop=True),
                     reads=[kd, qa], writes=[banks[half]])
            pt = ptp.next()
            if W == 512:
                p.op(ACT, lambda e: e.activation(out=pt[:, 0:1024], in_=k.psw[wi].t[:, 0:1024], func=AF.Exp, scale=0.125), reads=[banks[0], banks[1]], writes=[pt])
            else:
                for half in range(2):
                    p.op(ACT, lambda e: e.activation(out=pt[:, half * 512:half * 512 + W], in_=banks[half][:, 0:W], func=AF.Exp, scale=0.125), reads=[banks[half]], writes=[(pt, half)])
            pend.append((h, pr, pt))

        def pv():
            h, pr, pt = pend.pop(0)
            g = h // 4
            for half, kt in enumerate(pr):
                for s in range(ns):
                    p.op(PE, lambda e, s=s: e.matmul(pO[s][:, 0:65], lhsT=pt[:, half * 512 + s * 128:half * 512 + (s + 1) * 128], rhs=va[:, kt, g * 65:(g + 1) * 65],
                                                     start=(kt == kts[0]), stop=(kt == kts[-1])), reads=[pt, va], writes=[pO[s]])
            if pr is pairs[-1]:
                for s in range(ns):
                    rec = recp.next()
                    p.op(DVE, lambda e, s=s: e.reciprocal(out=rec[:], in_=pO[s][:, 64:65]), reads=[pO[s]], writes=[rec])
                    p.op(DVE, lambda e, s=s: e.tensor_scalar(out=yt[:, s, h * 64:(h + 1) * 64], in0=pO[s][:, 0:64], scalar1=rec[:, 0:1], scalar2=None, op0=ALU.mult),
                         reads=[pO[s], rec], writes=[(yt, (s, h))])

        seq = [(h, pr) for h in range(8) for pr in pairs]
        for i_, (h, pr) in enumerate(seq):
            qk(h, pr)
            if i_ >= 1:
                pv()
        while pend:
            pv()
        yst = ystp.next()
        for s in range(ns):
            for c in range(4):
                p.op(PE, lambda e, s=s, c=c: e.transpose(out=pT_ap[:, c * 128:(c + 1) * 128], in_=yt[:, s, c * 128:(c + 1) * 128], identity=k.identb[:]),
                     reads=[yt, k.identb], writes=[pT_t])
            p.op(DVE, lambda e, s=s: e.tensor_copy(out=yst[:, :, s * 128:(s + 1) * 128], in_=pT_ap[:].rearrange("p (c n) -> p c n", c=4)), reads=[pT_t], writes=[(yst, s)])
        p.dma(POOL, k.yattT[:, tok0:tok0 + W].rearrange("(c p) n -> p c n", p=128), yst[:, :, 0:W], reads=[yst])


def phase_D(k, l, last):
    p, ar = k.p, k.ar
    pus = ar.alloc("pus", [128, NTILE, 512], BF16)
    p.dma(SP, pus[:], k.pu.rearrange("(t p) n -> p t n", p=128), writes=[pus])
    bandb = ar.alloc("bandb", [128, 20, 128], BF16)
    for i in range(2):
        p.dma(POOL, bandb[:, i * 10:(i + 1) * 10, :], k.IN["band"][:, i * 1280:(i + 1) * 1280].rearrange("p (a b) -> p a b", a=10), writes=[(bandb, i)])
    pw = ar.alloc("pw", [128, 4, 128], BF16)
    p.dma(POOL, pw[:], k.IN["pool_w"][l].rearrange("g c e -> c g e"), writes=[pw])
    psc = ar.alloc("psc", [128, 4], F32)
    p.dma(SP, psc[:], k.IN["psc_c"][l], writes=[psc])
    dTp = Rot([ar.alloc("dT%d" % i, [128, 512], BF16) for i in range(3)])
    ystp = Rot([ar.alloc("ystD%d" % i, [128, 4, 512], BF16) for i in range(2)])
    pdp = Rot([k.ps[0], k.ps[1], k.ps[2]])
    pyp = Rot([k.ps[3], k.ps[4], k.ps[5]])
    for (tok0, W, j) in GROUPS:
        if j == 1 and last:
            continue
        first, lastt = (0, 31) if j == 0 else (32, 33)
        t0 = tok0 // 128
        yst = ystp.next()
        for g in range(4):
            pd = pdp.next()
            for i in range(W // 128):
                ti = t0 + i
                srcs = []
                if ti > first:
                    srcs.append((ti - 1, 3))
                srcs.append((ti, 0 if ti == first else (2 if ti == lastt else 1)))
                if ti < lastt:
                    srcs.append((ti + 1, 4))
                for n_, (src, typ) in enumerate(srcs):
                    p.op(PE, lambda e, i=i, src=src, typ=typ, n_=n_: e.matmul(pd[:, i * 128:(i + 1) * 128], lhsT=pus[:, src, g * 128:(g + 1) * 128], rhs=bandb[:, g * 5 + typ, :],
                                                                            start=(n_ == 0), stop=(n_ == len(srcs) - 1)), reads=[pus, bandb], writes=[pd])
            dT = dTp.next()
            p.op(ACT, lambda e: e.activation(out=dT[:, 0:W], in_=pd[:, 0:W], func=AF.Copy), reads=[pd], writes=[dT])
            py = pyp.next()
            p.op(PE, lambda e: e.matmul(py[:, 0:W], lhsT=pw[:, g, :], rhs=dT[:, 0:W], start=True, stop=True), reads=[pw, dT], writes=[py])
            p.op(DVE, lambda e: e.tensor_scalar(out=yst[:, g, 0:W], in0=py[:, 0:W], scalar1=psc[:, g:g + 1], scalar2=None, op0=ALU.mult), reads=[py, psc], writes=[(yst, g)])
        p.dma(POOL, k.ypoolT[:, tok0:tok0 + W].rearrange("(c p) n -> p c n", p=128), yst[:, :, 0:W], reads=[yst])


def prefetch_E(k, l):
    p, ar = k.p, k.ar
    w_in = k.IN["w_in"]
    wg = ar.alloc("wg", [128, 8, 3072], BF16, top=True)
    for i in range(6):
        p.dma(POOL, wg[:, :, i * 512:(i + 1) * 512], w_in[l, :, 2848 + i * 512:2848 + (i + 1) * 512].rearrange("(c p) n -> p c n", p=128), writes=[(wg, i)])
    wb = ar.alloc("wb", [128, 3, 4, 1024], BF16, top=True)
    for br in range(3):
        p.dma(POOL, wb[:, br, :, :], k.IN["w_branch"][l, br].rearrange("(c p) n -> p c n", p=128), writes=[(wb, br)])
    wo = ar.alloc("wo", [128, 8, 1024], BF16, top=True)
    p.dma(POOL, wo[:], k.IN["w_out"][l].rearrange("(c p) n -> p c n", p=128), writes=[wo])
    k.Ew = (wg, wb, wo)


def phase_E(k, l, last):
    p, ar = k.p, k.ar
    AXX = mybir.AxisListType.X
    w_in = k.IN["w_in"]
    wg, wb, wo = k.Ew
    wr = ar.alloc("wr", [128, 8, 36], F32)
    p.dma(SP, wr[:], k.IN["w_rt"][l].rearrange("(c p) n -> p c n", p=128), writes=[wr])
    brt = ar.alloc("brt", [128, 36], F32)
    p.dma(SP, brt[:], k.IN["b_rt"][l], writes=[brt])
    hTb = ar.alloc("hTbE", [128, 8, 512], BF16)
    yT3 = ar.alloc("yT3", [128, 3, 4, 512], BF16)
    xg = ar.alloc("xgE", [128, 8, 512], F32)
    zT = ar.alloc("zT", [128, 8, 512], BF16)
    sgp = Rot([ar.alloc("sg%d" % i, [128, 512], F32) for i in range(2)])
    tp = Rot([ar.alloc("tE%d" % i, [128, 512], F32) for i in range(2)])
    zap = Rot([ar.alloc("za%d" % i, [128, 512], F32) for i in range(2)])
    sqt = ar.alloc("sqtE", [128, 8, 512], F32)
    h2b = ar.alloc("h2b", [128, 8, 512], BF16)
    rs = ar.alloc("rsE", [128, 512], F32)
    rstd = rs
    sm = {n_: Rot([ar.alloc("r_%s%d" % (n_, i), [128, w_], F32) for i in range(4)]) for n_, w_ in
          (("lg", 36), ("gmax", 1), ("ngmax", 1), ("mg", 4), ("eg", 4), ("sge", 1), ("pg", 1), ("pen", 4), ("lem", 32), ("m1", 1), ("mask1", 32),
           ("lem2", 32), ("m2", 1), ("mask2", 32), ("dd", 1), ("w1", 1), ("w2", 1), ("slot", 32), ("ok", 32), ("pr0", 32), ("pr1", 32), ("df0", 1), ("df1", 1))}
    sm["Ab"] = Rot([ar.alloc("r_Ab%d" % i, [128, 32], BF16) for i in range(4)])
    eoff = ar.alloc("eoff", [128, 64], F32)
    p.dma(SP, eoff[:], k.IN["eoff"], writes=[eoff])
    cnt = ar.alloc("cnt", [128, 32], F32)
    p.op(DVE, lambda e: e.tensor_copy(out=cnt[:], in_=eoff[:, 0:32]), reads=[eoff], writes=[cnt])
    h2tp = Rot(k.tokbuf[0:2])
    pTt_ap = k.ps[6].t[:, 0:512].bitcast(BF16)
    pGp = Rot([k.ps[0], k.ps[1]])
    pBp = Rot([k.ps[2], k.ps[3]])
    pYp = Rot([k.ps[4], k.ps[5]])
    pss = k.ps[6]
    pR = k.ps[7]
    srcs = (k.yglaT, k.yattT, k.ypoolT)

    def E_loads(tok0, W, j):
        p.dma(SP, hTb[:, :, 0:W], k.hT[:, tok0:tok0 + W].rearrange("(c p) n -> p c n", p=128), writes=[hTb])
        for br in range(3):
            p.dma(SP, yT3[:, br, :, 0:W], srcs[br][:, tok0:tok0 + W].rearrange("(c p) n -> p c n", p=128), writes=[(yT3, br)])
        p.dma(SP, xg[:, :, 0:W], k.xT[:, tok0:tok0 + W].rearrange("(c p) n -> p c n", p=128), writes=[xg])

    def E_merge(tok0, W, j):
        for ec in range(8):
            za = zap.next()
            for br in range(3):
                pG, pB = pGp.next(), pBp.next()
                for c in range(8):
                    p.op(PE, lambda e, c=c: e.matmul(pG[:, 0:W], lhsT=wg[:, c, br * 1024 + ec * 128:br * 1024 + (ec + 1) * 128], rhs=hTb[:, c, 0:W], start=(c == 0), stop=(c == 7)),
                         reads=[wg, hTb], writes=[pG])
                for c in range(4):
                    p.op(PE, lambda e, c=c: e.matmul(pB[:, 0:W], lhsT=wb[:, br, c, ec * 128:(ec + 1) * 128], rhs=yT3[:, br, c, 0:W], start=(c == 0), stop=(c == 3)),
                         reads=[wb, (yT3, br)], writes=[pB])
                sg = sgp.next()
                p.op(ACT, lambda e: e.activation(out=sg[:, 0:W], in_=pG[:, 0:W], func=AF.Sigmoid), reads=[pG], writes=[sg])
                if br == 0:
                    p.op(DVE, lambda e: e.tensor_tensor(out=za[:, 0:W], in0=sg[:, 0:W], in1=pB[:, 0:W], op=ALU.mult), reads=[sg, pB], writes=[za])
                else:
                    t_ = tp.next()
                    p.op(DVE, lambda e: e.tensor_tensor(out=t_[:, 0:W], in0=sg[:, 0:W], in1=pB[:, 0:W], op=ALU.mult), reads=[sg, pB], writes=[t_])
                    if br == 1:
                        p.op(POOL, lambda e: e.tensor_tensor(out=za[:, 0:W], in0=za[:, 0:W], in1=t_[:, 0:W], op=ALU.add), reads=[za, t_], writes=[za])
                    else:
                        p.op(POOL, lambda e: e.tensor_tensor(out=zT[:, ec, 0:W], in0=za[:, 0:W], in1=t_[:, 0:W], op=ALU.add), reads=[za, t_], writes=[(zT, ec)])
                yield

    def E_out(tok0, W, j):
        for fc in range(8):
            pY = pYp.next()
            for c in range(8):
                p.op(PE, lambda e, c=c: e.matmul(pY[:, 0:W], lhsT=wo[:, c, fc * 128:(fc + 1) * 128], rhs=zT[:, c, 0:W], start=(c == 0), stop=(c == 7)),
                     reads=[wo, zT], writes=[pY])
            p.op(DVE, lambda e: e.scalar_tensor_tensor(out=xg[:, fc, 0:W], in0=pY[:, 0:W], scalar=k.modT[l][:, 16 + fc, j:j + 1], in1=xg[:, fc, 0:W], op0=ALU.mult, op1=ALU.add),
                 reads=[pY, k.modT[l], xg], writes=[xg])
        p.dma(POOL, k.xT[:, tok0:tok0 + W].rearrange("(c p) n -> p c n", p=128), xg[:, :, 0:W], reads=[xg])
        rms_group(k, xg, W, k.A2[l], k.modT[l], 24, j, hT32=sqt, hTb=h2b, sqt=sqt, pss=pss, tmp=(rs, rstd))
        p.dma(POOL, k.h2T[:, tok0:tok0 + W].rearrange("(c p) n -> p c n", p=128), h2b[:, :, 0:W], reads=[h2b])

    def E_router(tok0, W, j):
        ns = W // 128
        RS = [{n_: r_.tiles[s] for n_, r_ in sm.items()} for s in range(ns)]

        def part1(s):
            R = RS[s]
            ts_ = slice(s * 128, (s + 1) * 128)
            lo = s * 40
            lg = R["lg"]
            for c in range(8):
                p.op(PE, lambda e, c=c: e.matmul(pR[:, lo:lo + 36], lhsT=sqt[:, c, ts_], rhs=wr[:, c, :], start=(c == 0), stop=(c == 7)), reads=[sqt, wr], writes=[pR])
            yield
            p.op(DVE, lambda e: e.tensor_tensor(out=lg[:], in0=pR[:, lo:lo + 36], in1=brt[:], op=ALU.add), reads=[pR, brt], writes=[lg])
            yield
            p.op(DVE, lambda e: e.reduce_max(out=R["gmax"][:], in_=lg[:, 0:4], axis=AXX), reads=[lg], writes=[R["gmax"]])
            yield
            p.op(DVE, lambda e: e.tensor_scalar(out=R["mg"][:], in0=lg[:, 0:4], scalar1=R["gmax"][:, 0:1], scalar2=None, op0=ALU.is_ge), reads=[lg, R["gmax"]], writes=[R["mg"]])
            p.op(DVE, lambda e: e.tensor_scalar(out=R["ngmax"][:], in0=R["gmax"][:], scalar1=-1.0, scalar2=None, op0=ALU.mult), reads=[R["gmax"]], writes=[R["ngmax"]])
            yield
            p.op(ACT, lambda e: e.activation(out=R["eg"][:], in_=lg[:, 0:4], func=AF.Exp, bias=R["ngmax"][:, 0:1], scale=1.0, accum_out=R["sge"][:]),
                 reads=[lg, R["ngmax"]], writes=[R["eg"], R["sge"]])
            p.op(DVE, lambda e: e.tensor_scalar(out=R["pen"][:], in0=R["mg"][:], scalar1=-1.0, scalar2=1e30, op0=ALU.add, op1=ALU.mult), reads=[R["mg"]], writes=[R["pen"]])
            yield
            p.op(DVE, lambda e: e.tensor_tensor(out=R["lem"][:].rearrange("p (g x) -> p g x", g=4), in0=lg[:, 4:36].rearrange("p (g x) -> p g x", g=4),
                                                in1=R["pen"][:].unsqueeze(2).broadcast_to([128, 4, 8]), op=ALU.add), reads=[lg, R["pen"]], writes=[R["lem"]])
            yield
            p.op(DVE, lambda e: e.reduce_max(out=R["m1"][:], in_=R["lem"][:], axis=AXX), reads=[R["lem"]], writes=[R["m1"]])
            yield
            p.op(DVE, lambda e: e.tensor_scalar(out=R["mask1"][:], in0=R["lem"][:], scalar1=R["m1"][:, 0:1], scalar2=None, op0=ALU.is_ge), reads=[R["lem"], R["m1"]], writes=[R["mask1"]])
            yield
            p.op(DVE, lambda e: e.scalar_tensor_tensor(out=R["lem2"][:], in0=R["mask1"][:], scalar=-1e30, in1=R["lem"][:], op0=ALU.mult, op1=ALU.add),
                 reads=[R["mask1"], R["lem"]], writes=[R["lem2"]])
            yield
            p.op(DVE, lambda e: e.reduce_max(out=R["m2"][:], in_=R["lem2"][:], axis=AXX), reads=[R["lem2"]], writes=[R["m2"]])
            yield
            p.op(DVE, lambda e: e.tensor_scalar(out=R["mask2"][:], in0=R["lem2"][:], scalar1=R["m2"][:, 0:1], scalar2=None, op0=ALU.is_ge), reads=[R["lem2"], R["m2"]], writes=[R["mask2"]])
            p.op(DVE, lambda e: e.tensor_tensor(out=R["dd"][:], in0=R["m2"][:], in1=R["m1"][:], op=ALU.subtract), reads=[R["m2"], R["m1"]], writes=[R["dd"]])
            yield
            p.op(ACT, lambda e: e.activation(out=R["dd"][:], in_=R["dd"][:], func=AF.Exp), reads=[R["dd"]], writes=[R["dd"]])
            p.op(DVE, lambda e: e.tensor_tensor(out=R["Ab"][:], in0=R["mask1"][:], in1=R["mask2"][:], op=ALU.add), reads=[R["mask1"], R["mask2"]], writes=[R["Ab"]])
            p.op(DVE, lambda e: e.reciprocal(out=R["pg"][:], in_=R["sge"][:]), reads=[R["sge"]], writes=[R["pg"]])
            yield
            p.op(PE, lambda e: e.matmul(pR[:, 192 + s * 64:224 + s * 64], lhsT=k.triub[:], rhs=R["Ab"][:], start=True, stop=True), reads=[R["Ab"], k.triub], writes=[pR])
            p.op(PE, lambda e: e.matmul(pR[:, 224 + s * 64:256 + s * 64], lhsT=k.onesb[:], rhs=R["Ab"][:], start=True, stop=True), reads=[R["Ab"], k.onesb], writes=[pR])
            p.op(DVE, lambda e: e.tensor_scalar(out=R["dd"][:], in0=R["dd"][:], scalar1=1.0, scalar2=None, op0=ALU.add), reads=[R["dd"]], writes=[R["dd"]])
            yield
            p.op(DVE, lambda e: e.reciprocal(out=R["w1"][:], in_=R["dd"][:]), reads=[R["dd"]], writes=[R["w1"]])
            yield
            p.op(DVE, lambda e: e.tensor_tensor(out=R["w1"][:], in0=R["w1"][:], in1=R["pg"][:], op=ALU.mult), reads=[R["w1"], R["pg"]], writes=[R["w1"]])
            yield
            p.op(DVE, lambda e: e.tensor_tensor(out=R["w2"][:], in0=R["pg"][:], in1=R["w1"][:], op=ALU.subtract), reads=[R["w1"], R["pg"]], writes=[R["w2"]])
            yield
            tt = (tok0 + s * 128) // 128
            p.op(DVE, lambda e: e.tensor_copy(out=k.rw[:, tt, 0:1], in_=R["w1"][:]), reads=[R["w1"]], writes=[(k.rw, (tt, 0))])
            p.op(DVE, lambda e: e.tensor_copy(out=k.rw[:, tt, 1:2], in_=R["w2"][:]), reads=[R["w2"]], writes=[(k.rw, (tt, 1))])
            yield

        def part2(s):
            R = RS[s]
            tt = (tok0 + s * 128) // 128
            sl, ok = R["slot"], R["ok"]
            p.op(DVE, lambda e: e.tensor_tensor(out=ok[:], in0=sl[:], in1=eoff[:, 32:64], op=ALU.is_lt), reads=[sl, eoff], writes=[ok])
            yield
            p.op(DVE, lambda e: e.scalar_tensor_tensor(out=sl[:], in0=ok[:], scalar=-1.0e6, in1=sl[:], op0=ALU.mult, op1=ALU.add), reads=[ok, sl], writes=[sl])
            yield
            p.op(DVE, lambda e: e.tensor_scalar(out=sl[:], in0=sl[:], scalar1=1.0e6, scalar2=None, op0=ALU.add), reads=[sl], writes=[sl])
            yield
            for mi, mname in enumerate(("mask1", "mask2")):
                p.op(DVE, lambda e: e.tensor_tensor(out=R["pr%d" % mi][:], in0=R[mname][:], in1=sl[:], op=ALU.mult), reads=[R[mname], sl], writes=[R["pr%d" % mi]])
            yield
            for mi in range(2):
                p.op(DVE, lambda e: e.reduce_sum(out=R["df%d" % mi][:], in_=R["pr%d" % mi][:], axis=AXX), reads=[R["pr%d" % mi]], writes=[R["df%d" % mi]])
            yield
            for mi in range(2):
                p.op(DVE, lambda e: e.tensor_copy(out=k.ridx[:, tt, mi:mi + 1], in_=R["df%d" % mi][:]), reads=[R["df%d" % mi]], writes=[(k.ridx, (tt, mi))])
            yield


        def rest():
            for s in range(ns):
                R = RS[s]
                p.op(DVE, lambda e: e.tensor_tensor(out=R["slot"][:], in0=pR[:, 192 + s * 64:224 + s * 64], in1=cnt[:], op=ALU.add), reads=[pR, cnt], writes=[R["slot"]])
                p.op(DVE, lambda e: e.tensor_tensor(out=cnt[:], in0=pR[:, 224 + s * 64:256 + s * 64], in1=cnt[:], op=ALU.add), reads=[pR, cnt], writes=[cnt])
            roundrobin(part2(s) for s in range(ns))
            for s in range(ns):
                ts_ = slice(s * 128, (s + 1) * 128)
                tt = (tok0 + s * 128) // 128
                for c in range(8):
                    p.op(PE, lambda e, c=c: e.transpose(out=pTt_ap[:, c * 128:(c + 1) * 128], in_=h2b[:, c, ts_], identity=k.identb[:]), reads=[h2b, k.identb], writes=[pss])
                ht = h2tp.next()
                p.op(ACT, lambda e: e.activation(out=ht[:], in_=pTt_ap[:], func=AF.Copy), reads=[pss], writes=[ht])
                for mi in range(2):
                    p.dma_call(POOL, "indirect_dma_start", dict(out=k.Xg[:, :], out_offset=bass.IndirectOffsetOnAxis(ap=k.ridx[:, tt, mi:mi + 1], axis=0),
                                                                in_=ht[:], in_offset=None, bounds_check='NROWS_REG', oob_is_err=False),
                               reads=[ht, (k.ridx, (tt, mi))])


        return [part1(s) for s in range(ns)], rest

    groups = [g_ for g_ in GROUPS if not (g_[2] == 1 and last)]
    prev = None
    for g_ in groups:
        E_loads(*g_)
        if prev is None:
            roundrobin([E_merge(*g_)])
        else:
            roundrobin([E_merge(*g_)] + prev[0])
            prev[1]()
        E_out(*g_)
        prev = E_router(*g_)
    roundrobin(prev[0])
    prev[1]()


def phase_F_dense(k, l, last):
    p, ar = k.p, k.ar
    groups = [g_ for g_ in GROUPS if not (g_[2] == 1 and last)]
    sgs = [groups[0:4], groups[4:]]
    NS = 2304
    h2s = ar.alloc("h2s", [128, 8, NS], BF16)
    yacc = ar.alloc("yacc", [128, 8, NS], F32)
    wgp = Rot([ar.alloc("mwg%d" % i, [128, 8, 512], BF16) for i in range(2)])
    wup = Rot([ar.alloc("mwu%d" % i, [128, 8, 512], BF16) for i in range(2)])
    wdp = Rot([ar.alloc("mwd%d" % i, [128, 4, 1024], BF16) for i in range(2)])
    wrp = Rot([ar.alloc("wrow%d" % i, [128, 512], F32) for i in range(2)])
    sgp = Rot([ar.alloc("msg%d" % i, [128, 512], F32) for i in range(2)])
    hup = Rot([ar.alloc("mhu%d" % i, [128, 512], F32) for i in range(2)])
    HTp = Rot([ar.alloc("mHT%d" % i, [128, 4, 512], BF16) for i in range(2)])
    xg = ar.alloc("xgF", [128, 8, 512], F32)
    pGp = Rot([k.ps[0], k.ps[1]])
    pUp = Rot([k.ps[2], k.ps[3]])
    pYp = Rot([k.ps[4], k.ps[5], k.ps[6]])
    for sg_groups in sgs:
        base = sg_groups[0][0]
        ntok = sum(g_[1] for g_ in sg_groups)
        p.dma(SP, h2s[:, :, 0:ntok], k.h2T[:, base:base + ntok].rearrange("(c p) n -> p c n", p=128), writes=[h2s])
        for ex in range(NE):
            wg_, wu_, wd_ = wgp.next(), wup.next(), wdp.next()
            p.dma(POOL, wg_[:], k.IN["moe_w_gate"][l, ex].rearrange("(c p) n -> p c n", p=128), writes=[wg_])
            p.dma(POOL, wu_[:], k.IN["moe_w_up"][l, ex].rearrange("(c p) n -> p c n", p=128), writes=[wu_])
            p.dma(POOL, wd_[:], k.IN["moe_w_down"][l, ex].rearrange("(c p) n -> p c n", p=128), writes=[wd_])
            for (tok0, W, j) in sg_groups:
                o0 = tok0 - base
                wrow = wrp.next()
                p.dma(SP, wrow[:, 0:W], k.wdT[ex:ex + 1, tok0:tok0 + W].broadcast_to([128, W]), writes=[wrow])
                HT = HTp.next()
                for dc in range(4):
                    pG, pU = pGp.next(), pUp.next()
                    for c in range(8):
                        p.op(PE, lambda e, c=c: e.matmul(pG[:, 0:W], lhsT=wg_[:, c, dc * 128:(dc + 1) * 128], rhs=h2s[:, c, o0:o0 + W], start=(c == 0), stop=(c == 7)),
                             reads=[wg_, h2s], writes=[pG])
                    for c in range(8):
                        p.op(PE, lambda e, c=c: e.matmul(pU[:, 0:W], lhsT=wu_[:, c, dc * 128:(dc + 1) * 128], rhs=h2s[:, c, o0:o0 + W], start=(c == 0), stop=(c == 7)),
                             reads=[wu_, h2s], writes=[pU])
                    sg, hu = sgp.next(), hup.next()
                    p.op(ACT, lambda e: e.activation(out=sg[:, 0:W], in_=pG[:, 0:W], func=AF.Silu), reads=[pG], writes=[sg])
                    p.op(DVE, lambda e: e.tensor_tensor(out=hu[:, 0:W], in0=sg[:, 0:W], in1=pU[:, 0:W], op=ALU.mult), reads=[sg, pU], writes=[hu])
                    p.op(POOL, lambda e: e.tensor_tensor(out=HT[:, dc, 0:W], in0=hu[:, 0:W], in1=wrow[:, 0:W], op=ALU.mult), reads=[hu, wrow], writes=[(HT, dc)])
                for fc in range(8):
                    pY = pYp.next()
                    for c in range(4):
                        p.op(PE, lambda e, c=c: e.matmul(pY[:, 0:W], lhsT=wd_[:, c, fc * 128:(fc + 1) * 128], rhs=HT[:, c, 0:W], start=(c == 0), stop=(c == 3)),
                             reads=[wd_, HT], writes=[pY])
                    ya = yacc[:, fc, o0:o0 + W]
                    if ex == 0:
                        p.op(ACT, lambda e: e.activation(out=ya, in_=pY[:, 0:W], func=AF.Copy), reads=[pY], writes=[(yacc, (fc, tok0))])
                    else:
                        p.op(DVE, lambda e: e.tensor_tensor(out=ya, in0=ya, in1=pY[:, 0:W], op=ALU.add), reads=[pY, (yacc, (fc, tok0))], writes=[(yacc, (fc, tok0))])
            pass
        for (tok0, W, j) in sg_groups:
            o0 = tok0 - base
            p.dma(SP, xg[:, :, 0:W], k.xT[:, tok0:tok0 + W].rearrange("(c p) n -> p c n", p=128), writes=[xg])
            for fc in range(8):
                p.op(DVE, lambda e: e.scalar_tensor_tensor(out=xg[:, fc, 0:W], in0=yacc[:, fc, o0:o0 + W], scalar=k.modT[l][:, 40 + fc, j:j + 1], in1=xg[:, fc, 0:W],
                                                           op0=ALU.mult, op1=ALU.add), reads=[(yacc, (fc, tok0)), k.modT[l], xg], writes=[xg])
            p.dma(POOL, k.xT[:, tok0:tok0 + W].rearrange("(c p) n -> p c n", p=128), xg[:, :, 0:W], reads=[xg])


def phase_F(k, l, last):
    p, ar = k.p, k.ar
    wgp = Rot([ar.alloc("mwg%d" % i, [128, 8, 512], BF16) for i in range(2)])
    wup = Rot([ar.alloc("mwu%d" % i, [128, 8, 512], BF16) for i in range(2)])
    wdp = Rot([ar.alloc("mwd%d" % i, [128, 4, 1024], BF16) for i in range(2)])
    xbp = Rot([ar.alloc("xblk%d" % i, [128, 4, 1024], BF16) for i in range(3)])
    xtp = Rot([ar.alloc("xTs%d" % i, [128, 8, 512], BF16) for i in range(2)])
    sgp = Rot([ar.alloc("msg%d" % i, [128, 512], F32) for i in range(3)])
    HTp = Rot([ar.alloc("mHT%d" % i, [128, 4, 512], BF16) for i in range(3)])
    ysp = Rot([ar.alloc("yst%d" % i, [128, 4, 1024], BF16) for i in range(3)])
    pGp = Rot([k.ps[0], k.ps[1]])
    pUp = Rot([k.ps[2], k.ps[3]])
    pYp = Rot([k.ps[4], k.ps[5]])
    pTp = Rot([k.ps[6], k.ps[7]])
    blocks = [(b0, min(512, CAP - b0)) for b0 in range(0, CAP, 512)]
    stg = ar.alloc("wstg_g", [128, 8, 512], F32)
    stu = ar.alloc("wstg_u", [128, 8, 512], F32)
    std = ar.alloc("wstg_d", [128, 4, 1024], F32)

    def load_w(ex):
        wg_, wu_, wd_ = wgp.next(), wup.next(), wdp.next()
        p.dma(SP, stg[:], k.IN["moe_w_gate"][l, ex].rearrange("(c p) n -> p c n", p=128), writes=[stg])
        p.dma(SP, stu[:], k.IN["moe_w_up"][l, ex].rearrange("(c p) n -> p c n", p=128), writes=[stu])
        p.dma(SP, std[:], k.IN["moe_w_down"][l, ex].rearrange("(c p) n -> p c n", p=128), writes=[std])
        for c in range(8):
            p.op(POOL, lambda e, c=c: e.tensor_copy(out=wg_[:, c, :], in_=stg[:, c, :]), reads=[stg], writes=[(wg_, c)])
            p.op(POOL, lambda e, c=c: e.tensor_copy(out=wu_[:, c, :], in_=stu[:, c, :]), reads=[stu], writes=[(wu_, c)])
        for c in range(4):
            p.op(POOL, lambda e, c=c: e.tensor_copy(out=wd_[:, c, :], in_=std[:, c, :]), reads=[std], writes=[(wd_, c)])
        return wg_, wu_, wd_

    W_ = {}
    XB = {}

    def start_expert(ex):
        for bi, (b0, WB) in enumerate(blocks):
            xb = xbp.next()
            p.dma(SP, xb[:, 0:WB // 128, :], k.Xg[ex * CAP + b0:ex * CAP + b0 + WB, :].rearrange("(s p) n -> p s n", p=128), writes=[xb])
            XB[(ex, bi)] = xb

    def T_(ex, bi):
        b0, WB = blocks[bi]
        xb = XB.pop((ex, bi))
        xT = xtp.next()
        for sub in range(WB // 128):
            pT = pTp.next()
            pT_ap = pT.t[:, 0:512].bitcast(BF16)
            for c in range(8):
                p.op(PE, lambda e, c=c: e.transpose(out=pT_ap[:, c * 128:(c + 1) * 128], in_=xb[:, sub, c * 128:(c + 1) * 128], identity=k.identb[:]),
                     reads=[xb, k.identb], writes=[pT])
            if sub % 2 == 0:
                p.op(ACT, lambda e: e.activation(out=xT[:, :, sub * 128:(sub + 1) * 128], in_=pT_ap[:].rearrange("p (c n) -> p c n", c=8), func=AF.Copy),
                     reads=[pT], writes=[(xT, sub)])
            else:
                p.op(DVE, lambda e: e.tensor_copy(out=xT[:, :, sub * 128:(sub + 1) * 128], in_=pT_ap[:].rearrange("p (c n) -> p c n", c=8)),
                     reads=[pT], writes=[(xT, sub)])
        return xT

    def GU_(ex, bi, xT):
        b0, WB = blocks[bi]
        wg_, wu_, wd_ = W_[ex]
        HT = HTp.next()
        for dc in range(4):
            pG, pU = pGp.next(), pUp.next()
            for c in range(8):
                p.op(PE, lambda e, c=c: e.matmul(pG[:, 0:WB], lhsT=wg_[:, c, dc * 128:(dc + 1) * 128], rhs=xT[:, c, 0:WB], start=(c == 0), stop=(c == 7)),
                     reads=[wg_, xT], writes=[pG])
            for c in range(8):
                p.op(PE, lambda e, c=c: e.matmul(pU[:, 0:WB], lhsT=wu_[:, c, dc * 128:(dc + 1) * 128], rhs=xT[:, c, 0:WB], start=(c == 0), stop=(c == 7)),
                     reads=[wu_, xT], writes=[pU])
            sg = sgp.next()
            p.op(ACT, lambda e: e.activation(out=sg[:, 0:WB], in_=pG[:, 0:WB], func=AF.Silu), reads=[pG], writes=[sg])
            p.op(DVE, lambda e: e.tensor_tensor(out=HT[:, dc, 0:WB], in0=sg[:, 0:WB], in1=pU[:, 0:WB], op=ALU.mult), reads=[sg, pU], writes=[(HT, dc)])
        return HT

    def DN_(ex, bi, HT):
        b0, WB = blocks[bi]
        nsub = WB // 128
        r0 = ex * CAP + b0
        wg_, wu_, wd_ = W_[ex]
        ys = ysp.next()
        for sub in range(nsub):
            for half in range(2):
                pY = pYp.next()
                for c in range(4):
                    p.op(PE, lambda e, c=c: e.matmul(pY[:, 0:512], lhsT=HT[:, c, sub * 128:(sub + 1) * 128], rhs=wd_[:, c, half * 512:(half + 1) * 512], start=(c == 0), stop=(c == 3)),
                         reads=[HT, wd_], writes=[pY])
                if half == 0:
                    p.op(ACT, lambda e: e.activation(out=ys[:, sub, 0:512], in_=pY[:, 0:512], func=AF.Copy), reads=[pY], writes=[(ys, (sub, 0))])
                else:
                    p.op(DVE, lambda e: e.tensor_copy(out=ys[:, sub, 512:1024], in_=pY[:, 0:512]), reads=[pY], writes=[(ys, (sub, 1))])
        p.dma(ACT, k.Yg[r0:r0 + WB, :].rearrange("(s p) n -> p s n", p=128), ys[:, 0:nsub, :], reads=[ys])

    seq = [(ex, bi) for ex in range(NE) for bi in range(len(blocks))]
    W_[0] = load_w(0)
    start_expert(0)
    W_[1] = load_w(1)
    xT_cur = T_(*seq[0])
    for i_, (ex, bi) in enumerate(seq):
        HT = GU_(ex, bi, xT_cur)
        if i_ + 1 < len(seq):
            nex, nbi = seq[i_ + 1]
            if nbi == 0:
                start_expert(nex)
            xT_cur = T_(nex, nbi)
        DN_(ex, bi, HT)
        if i_ + 1 < len(seq) and seq[i_ + 1][1] == 0 and seq[i_ + 1][0] + 1 < NE:
            W_[seq[i_ + 1][0] + 1] = load_w(seq[i_ + 1][0] + 1)
    p.barrier()
    ar.reset()
    y1p = Rot(k.tokbuf[0:2])
    y2p = Rot(k.tokbuf[2:4])
    mp = Rot([ar.alloc("mtok%d" % i, [128, 1024], F32) for i in range(2)])
    xgp = Rot([ar.alloc("xgF%d" % i, [128, 8, 512], F32) for i in range(2)])
    pb = Rot([(k.ps[0], k.ps[1]), (k.ps[2], k.ps[3]), (k.ps[4], k.ps[5])])
    if last:
        fing = ar.alloc("fing", [128, 8, 1], F32)
        p.dma(SP, fing[:].rearrange("p a b -> p (a b)"), k.IN["fing_c"], writes=[fing])
        zb = ar.alloc("zb", [128, 8, 1], F32)
        p.op(DVE, lambda e: e.memset(zb[:], 0.0), writes=[zb])
        sqtZ = ar.alloc("sqtZ", [128, 8, 512], F32)
        rsZ = ar.alloc("rsZ", [128, 512], F32)
        ostp = Rot([ar.alloc("ost%d" % i, [128, 1024], F32) for i in range(2)])
    groups = [g_ for g_ in GROUPS if not (g_[2] == 1 and last)]
    for (tok0, W, j) in groups:
        xg = xgp.next()
        p.dma(SP, xg[:, :, 0:W], k.xT[:, tok0:tok0 + W].rearrange("(c p) n -> p c n", p=128), writes=[xg])
        for s in range(W // 128):
            tt = (tok0 + s * 128) // 128
            ys_ = []
            for mi, yp in enumerate((y1p, y2p)):
                y_ = yp.next()
                p.op(POOL, lambda e: e.memset(y_[:], 0.0), writes=[y_])
                p.dma_call(POOL, "indirect_dma_start", dict(out=y_[:], out_offset=None, in_=k.Yg[:, :],
                                                            in_offset=bass.IndirectOffsetOnAxis(ap=k.ridx[:, tt, mi:mi + 1], axis=0),
                                                            bounds_check='NROWS_REG', oob_is_err=False), reads=[(k.ridx, (tt, mi))], writes=[y_])
                ys_.append(y_)
            m = mp.next()
            p.op(ACT, lambda e: e.activation(out=m[:], in_=ys_[0][:], func=AF.Copy, scale=k.rw[:, tt, 0:1]), reads=[ys_[0], (k.rw, (tt, 0))], writes=[m])
            p.op(DVE, lambda e: e.scalar_tensor_tensor(out=m[:], in0=ys_[1][:], scalar=k.rw[:, tt, 1:2], in1=m[:], op0=ALU.mult, op1=ALU.add),
                 reads=[ys_[1], (k.rw, (tt, 1)), m], writes=[m])
            pa, pbk = pb.next()
            for c in range(8):
                pt = pa if c < 4 else pbk
                p.op(PE, lambda e, c=c: e.transpose(out=pt[:, (c % 4) * 128:(c % 4 + 1) * 128], in_=m[:, c * 128:(c + 1) * 128], identity=k.ident[:]),
                     reads=[m, k.ident], writes=[pt])
            for c in range(8):
                pt = pa if c < 4 else pbk
                p.op(DVE, lambda e, c=c: e.scalar_tensor_tensor(out=xg[:, c, s * 128:(s + 1) * 128], in0=pt[:, (c % 4) * 128:(c % 4 + 1) * 128], scalar=k.modT[l][:, 40 + c, j:j + 1],
                                                               in1=xg[:, c, s * 128:(s + 1) * 128], op0=ALU.mult, op1=ALU.add),
                     reads=[pt, k.modT[l], (xg, (c, s))], writes=[(xg, (c, s))])
        if not last:
            p.dma(SP, k.xT[:, tok0:tok0 + W].rearrange("(c p) n -> p c n", p=128), xg[:, :, 0:W], reads=[xg])
            continue
        rms_group(k, xg, W, fing, zb, 0, 0, hT32=sqtZ, sqt=sqtZ, pss=k.ps[6], tmp=(rsZ, rsZ))
        for s in range(W // 128):
            pa, pbk = pb.next()
            ost = ostp.next()
            for c in range(8):
                pt = pa if c < 4 else pbk
                p.op(PE, lambda e, c=c: e.transpose(out=pt[:, (c % 4) * 128:(c % 4 + 1) * 128], in_=sqtZ[:, c, s * 128:(s + 1) * 128], identity=k.ident[:]),
                     reads=[sqtZ, k.ident], writes=[pt])
            p.op(ACT, lambda e: e.activation(out=ost[:, 0:512], in_=pa[:], func=AF.Copy), reads=[pa], writes=[(ost, 0)])
            p.op(DVE, lambda e: e.tensor_copy(out=ost[:, 512:1024], in_=pbk[:]), reads=[pbk], writes=[(ost, 1)])
            p.dma(SP, k.out[tok0 + s * 128:tok0 + (s + 1) * 128, :], ost[:], reads=[ost], is_output=True)


def phase_Z(k):
    p, ar = k.p, k.ar
    fing = ar.alloc("fing", [128, 8, 1], F32)
    p.dma(SP, fing[:].rearrange("p a b -> p (a b)"), k.IN["fing_c"], writes=[fing])
    zb = ar.alloc("zb", [128, 8, 1], F32)
    p.op(DVE, lambda e: e.memset(zb[:], 0.0), writes=[zb])
    xgp = Rot([ar.alloc("xgZ%d" % i, [128, 8, 512], F32) for i in range(2)])
    sqt = ar.alloc("sqtZ", [128, 8, 512], F32)
    rs = ar.alloc("rsZ", [128, 512], F32)
    rstd = ar.alloc("rstdZ", [128, 512], F32)
    ostp = Rot([ar.alloc("ost%d" % i, [128, 1024], F32) for i in range(2)])
    pss = k.ps[0]
    pb = Rot([(k.ps[1], k.ps[2]), (k.ps[3], k.ps[4]), (k.ps[5], k.ps[6])])
    for (tok0, W, j) in GROUPS[0:8]:
        xg = xgp.next()
        p.dma(SP, xg[:, :, 0:W], k.xT[:, tok0:tok0 + W].rearrange("(c p) n -> p c n", p=128), writes=[xg])
        rms_group(k, xg, W, fing, zb, 0, 0, hT32=sqt, sqt=sqt, pss=pss, tmp=(rs, rstd))
        for s in range(W // 128):
            pa, pbk = pb.next()
            ost = ostp.next()
            for c in range(8):
                pt = pa if c < 4 else pbk
                p.op(PE, lambda e, c=c: e.transpose(out=pt[:, (c % 4) * 128:(c % 4 + 1) * 128], in_=sqt[:, c, s * 128:(s + 1) * 128], identity=k.ident[:]),
                     reads=[sqt, k.ident], writes=[(pt, c % 4)])
            p.op(ACT, lambda e: e.activation(out=ost[:, 0:512], in_=pa[:], func=AF.Copy), reads=[pa], writes=[(ost, 0)])
            p.op(DVE, lambda e: e.tensor_copy(out=ost[:, 512:1024], in_=pbk[:]), reads=[pbk], writes=[(ost, 1)])
            p.dma(POOL, k.out[tok0 + s * 128:tok0 + (s + 1) * 128, :], ost[:], reads=[ost], is_output=True)
```
